# Optimizing a Trainium2 kernel written in Bass

```python
import math
import jax, jax.numpy as jnp
from jax import lax
import numpy as np

D_MODEL = 1024
BATCH = 8
SEQ = 2048
DEPTH = 1

ATT_HEADS = 8
ATT_KV_HEADS = 2
HEAD_DIM = 64
WINDOW = 128
BLOCK = 128
ATT_Q = ATT_HEADS * HEAD_DIM
ATT_KV = ATT_KV_HEADS * HEAD_DIM
GLA_HEADS = 4
GLA_DK = 64
GLA_DV = 128
GLA_RANK = 16
GLA_NORMALIZER = 16.0
GLA_CHUNK = 16
GLA_K = GLA_HEADS * GLA_DK
GLA_V = GLA_HEADS * GLA_DV
MIX_WIDTH = ATT_Q + GLA_V
IN_COLS = ATT_Q + 2 * ATT_KV + 2 * GLA_K + GLA_V + GLA_RANK + GLA_V
N_GROUPS = 4
EXPERTS_PER_GROUP = 4
N_EXPERTS = N_GROUPS * EXPERTS_PER_GROUP
TOP_K = 2
D_EXPERT = 256
EPS = 1e-6

kernel_name = "hymba_swa_sink_gla_hier_moe_adaln"


def rms_norm(x, g):
    xf = x.astype(jnp.float32)
    y = xf * lax.rsqrt(jnp.mean(xf * xf, axis=-1, keepdims=True) + EPS)
    return (y * g.astype(jnp.float32)).astype(x.dtype)


def alibi_slopes(n_heads):
    h = jnp.arange(1, n_heads + 1, dtype=jnp.float32)
    return jnp.exp2(-8.0 * h / n_heads)


def sliding_window_attention(q, k, v, sinks):
    B, T, Hq, hd = q.shape
    Hkv = k.shape[2]
    G = Hq // Hkv
    nb = T // BLOCK
    f32 = jnp.float32
    qb = q.reshape(B, nb, BLOCK, Hkv, G, hd)

    def banded(t):
        prev = jnp.concatenate([jnp.zeros_like(t[:, :BLOCK]), t[:, :-BLOCK]], axis=1)
        return jnp.concatenate([prev.reshape(B, nb, BLOCK, Hkv, hd),
                                t.reshape(B, nb, BLOCK, Hkv, hd)], axis=2)

    kb, vb = banded(k), banded(v)
    s = jnp.einsum('bnqhgd,bnkhd->bnhgqk', qb, kb).astype(f32) * (hd ** -0.5)
    qi = jnp.arange(BLOCK)[:, None]
    kj = jnp.arange(2 * BLOCK)[None, :]
    dist = qi - kj + BLOCK
    blk = jnp.arange(nb)[:, None, None]
    valid = (dist >= 0) & (dist < WINDOW) & (blk * BLOCK + kj - BLOCK >= 0)
    slopes = alibi_slopes(Hq).reshape(Hkv, G)[:, :, None, None]
    s = s - slopes * dist.astype(f32)
    s = jnp.where(valid[None, :, None, None], s, -jnp.inf)
    sk = sinks.astype(f32).reshape(Hkv, G)[:, :, None, None]
    m = jnp.maximum(jnp.max(s, axis=-1, keepdims=True), sk)
    p = jnp.exp(s - m)
    p = p / (jnp.sum(p, axis=-1, keepdims=True) + jnp.exp(sk - m))
    o = jnp.einsum('bnhgqk,bnkhd->bnqhgd', p.astype(v.dtype), vb)
    return o.reshape(B, T, Hq * hd)


def gla_chunked(q, k, v, log_a):
    B, T, H, dk = q.shape
    dv = v.shape[-1]
    C = GLA_CHUNK
    n = T // C
    f32 = jnp.float32
    q = q.astype(f32).reshape(B, n, C, H, dk) * (dk ** -0.5)
    k = k.astype(f32).reshape(B, n, C, H, dk)
    v = v.astype(f32).reshape(B, n, C, H, dv)
    b = jnp.cumsum(log_a.astype(f32).reshape(B, n, C, H, dk), axis=2)
    causal = jnp.tril(jnp.ones((C, C), dtype=bool))
    diff = b[:, :, :, None] - b[:, :, None, :]
    decay = jnp.where(causal[:, :, None, None], jnp.exp(jnp.minimum(diff, 0.0)), 0.0)
    attn = jnp.einsum('bnijhd,bnjhd->bnhij', q[:, :, :, None] * decay, k)
    o_intra = jnp.einsum('bnhij,bnjhe->bnihe', attn, v)
    b_last = b[:, :, -1]
    q_dec = q * jnp.exp(b)
    k_dec = k * jnp.exp(b_last[:, :, None] - b)
    u = jnp.einsum('bnjhd,bnjhe->bnhde', k_dec, v)

    def step(S, inp):
        a, du = inp
        return a[..., None] * S + du, S

    S0 = jnp.zeros((B, H, dk, dv), f32)
    _, S_prev = lax.scan(step, S0, (jnp.moveaxis(jnp.exp(b_last), 1, 0), jnp.moveaxis(u, 1, 0)))
    S_prev = jnp.moveaxis(S_prev, 0, 1)
    o_inter = jnp.einsum('bnihd,bnhde->bnihe', q_dec, S_prev)
    return (o_intra + o_inter).reshape(B, T, H, dv)


def hier_moe(h, w_group, b_group, w_router, b_router, w1, w3, w2):
    B, T, D = h.shape
    N = B * T
    hf = h.reshape(N, D)
    g_logits = (hf @ w_group + b_group).astype(jnp.float32)
    g_prob = jax.nn.softmax(g_logits, axis=-1)
    g_sel = jnp.argmax(g_logits, axis=-1)
    p_group = jnp.take_along_axis(g_prob, g_sel[:, None], axis=-1)
    e_logits = (hf @ w_router + b_router).astype(jnp.float32).reshape(N, N_GROUPS, EXPERTS_PER_GROUP)
    e_logits = jnp.take_along_axis(e_logits, g_sel[:, None, None], axis=1)[:, 0]
    e_prob = jax.nn.softmax(e_logits, axis=-1)
    top_v, top_i = lax.top_k(e_prob, TOP_K)
    top_v = top_v / jnp.sum(top_v, axis=-1, keepdims=True)
    expert_idx = g_sel[:, None] * EXPERTS_PER_GROUP + top_i
    weights = p_group * top_v
    combine = jnp.sum(jax.nn.one_hot(expert_idx, N_EXPERTS, dtype=jnp.float32) * weights[..., None], axis=1)
    a = jnp.einsum('nd,edf->nef', hf, w1)
    g = jnp.einsum('nd,edf->nef', hf, w3)
    hid = jax.nn.silu(a) * g * combine.astype(hf.dtype)[..., None]
    y = jnp.einsum('nef,efd->nd', hid, w2)
    return y.reshape(B, T, D)


def setup_inputs(seed: int = 0) -> dict:
    key = jax.random.key(seed)
    ks = jax.random.split(key, 24)
    D, L = D_MODEL, DEPTH
    nrm = lambda k, shape, s: jax.random.normal(k, shape, jnp.float32) * s
    return {
        "x": nrm(ks[0], (BATCH, SEQ, D), 1.0),
        "c": nrm(ks[1], (BATCH, D), 1.0),
        "w_ada": nrm(ks[2], (L, D, 6 * D), D ** -0.5),
        "b_ada": nrm(ks[3], (L, 6 * D), 0.02),
        "g_norm1": 1.0 + nrm(ks[4], (L, D), 0.02),
        "w_in": nrm(ks[5], (L, D, IN_COLS), D ** -0.5),
        "q_norm": 1.0 + nrm(ks[6], (L, HEAD_DIM), 0.02),
        "k_norm": 1.0 + nrm(ks[7], (L, HEAD_DIM), 0.02),
        "sinks": nrm(ks[8], (L, ATT_HEADS), 1.0),
        "w_gk2": nrm(ks[9], (L, GLA_RANK, GLA_K), GLA_RANK ** -0.5),
        "b_gk": nrm(ks[10], (L, GLA_K), 0.1),
        "g_gla_out": 1.0 + nrm(ks[11], (L, GLA_DV), 0.02),
        "g_att_out": 1.0 + nrm(ks[12], (L, ATT_Q), 0.02),
        "w_out": nrm(ks[13], (L, MIX_WIDTH, D), MIX_WIDTH ** -0.5),
        "g_norm2": 1.0 + nrm(ks[14], (L, D), 0.02),
        "w_group": nrm(ks[15], (L, D, N_GROUPS), D ** -0.5),
        "b_group": nrm(ks[16], (L, N_GROUPS), 0.01),
        "w_router": nrm(ks[17], (L, D, N_EXPERTS), D ** -0.5),
        "b_router": nrm(ks[18], (L, N_EXPERTS), 0.01),
        "w1": nrm(ks[19], (L, N_EXPERTS, D, D_EXPERT), D ** -0.5),
        "w3": nrm(ks[20], (L, N_EXPERTS, D, D_EXPERT), D ** -0.5),
        "w2": nrm(ks[21], (L, N_EXPERTS, D_EXPERT, D), D_EXPERT ** -0.5),
    }


def reference(x, c, w_ada, b_ada, g_norm1, w_in, q_norm, k_norm, sinks, w_gk2, b_gk,
              g_gla_out, g_att_out, w_out, g_norm2, w_group, b_group, w_router, b_router,
              w1, w3, w2):
    B, T, D = x.shape
    split_points = list(np.cumsum([ATT_Q, ATT_KV, ATT_KV, GLA_K, GLA_K, GLA_V, GLA_RANK]))
    for l in range(DEPTH):
        mod = (jax.nn.silu(c) @ w_ada[l] + b_ada[l]).reshape(B, 6, D)[:, :, None, :]
        shift1, scale1, gate1, shift2, scale2, gate2 = [mod[:, i] for i in range(6)]

        h = rms_norm(x, g_norm1[l]) * (1.0 + scale1) + shift1
        proj = h @ w_in[l]
        q_a, k_a, v_a, q_g, k_g, v_g, lr_g, og_g = jnp.split(proj, split_points, axis=-1)
        q_a = rms_norm(q_a.reshape(B, T, ATT_HEADS, HEAD_DIM), q_norm[l])
        k_a = rms_norm(k_a.reshape(B, T, ATT_KV_HEADS, HEAD_DIM), k_norm[l])
        v_a = v_a.reshape(B, T, ATT_KV_HEADS, HEAD_DIM)
        y_att = rms_norm(sliding_window_attention(q_a, k_a, v_a, sinks[l]), g_att_out[l])
        log_a = jax.nn.log_sigmoid((lr_g @ w_gk2[l] + b_gk[l]).astype(jnp.float32)) / GLA_NORMALIZER
        o_g = gla_chunked(q_g.reshape(B, T, GLA_HEADS, GLA_DK),
                          k_g.reshape(B, T, GLA_HEADS, GLA_DK),
                          v_g.reshape(B, T, GLA_HEADS, GLA_DV),
                          log_a.reshape(B, T, GLA_HEADS, GLA_DK)).astype(x.dtype)
        o_g = rms_norm(o_g, g_gla_out[l]).reshape(B, T, GLA_V)
        y_gla = o_g * jax.nn.silu(og_g)
        mix = jnp.concatenate([y_att, y_gla], axis=-1) @ w_out[l]
        x = x + gate1 * mix

        h = rms_norm(x, g_norm2[l]) * (1.0 + scale2) + shift2
        y = hier_moe(h, w_group[l], b_group[l], w_router[l], b_router[l], w1[l], w3[l], w2[l])
        x = x + gate2 * y
    return x
```

```python
import math
import numpy as np
import concourse.bass as bass
import concourse.mybir as mybir
from concourse.bass_utils import run_bass_kernel_spmd

F32 = mybir.dt.float32
BF16 = mybir.dt.bfloat16
AF = mybir.ActivationFunctionType
ALU = mybir.AluOpType
AX = mybir.AxisListType

T = 2048
D = 1024
NT = 16
EPS = 1e-6
LN8 = math.log(8.0)
NV = 81
NEXP = 16
EB = 2
SAME_ENGINE_FULL_SYNC = True
SEQ_STREAMS = False
GREEDY = False
PIPE_A = True
GROUPS = ((0, 2), (2, 2), (4, 4), (8, 4), (12, 2), (14, 2))
DBG_SKIP = ()
DBG_STAGE = 99
MODCOL = 320
DBG_NT = 4
DBG_SB = (3, 4)
DBG_OB = (5, 4)


class FW:
    def __init__(self, nc):
        self.nc = nc
        self.eng = {"pe": nc.tensor, "act": nc.scalar, "dve": nc.vector, "pool": nc.gpsimd, "sp": nc.sync}
        self.sem = {e: nc.alloc_semaphore("s_" + e) for e in self.eng}
        self.cnt = {e: 0 for e in self.eng}
        self.seen = {e: {} for e in self.eng}
        self.dsem = {q: [nc.alloc_semaphore("d_%s%d" % (q, i)) for i in range(14)] for q in ("sp", "pool")}
        self.dval = {q: [0] * 14 for q in ("sp", "pool")}
        self.drr = {"sp": 0, "pool": 0}
        self.lastw = {}
        self.readers = {}
        self.pe_pending = []
        self.nops = 0
        self.log = None
        self.tags = {}
        self.eng_free = {e: 0.0 for e in self.eng}
        self.tok_end = {}
        self.cur_stream_end = 0.0

    def _deps(self, eng, reads, writes, is_dma):
        toks = []
        for k in reads:
            w = self.lastw.get(k)
            if w is not None:
                toks.append((w, "raw"))
            if isinstance(k, tuple) and k[0] == "ps":
                for r in self.readers.get(k, ()):
                    toks.append((r, "rar"))
        for k in writes:
            w = self.lastw.get(k)
            if w is not None:
                toks.append((w, "waw"))
            for r in self.readers.get(k, ()):
                toks.append((r, "war"))
        need = {}
        for tok, kind in toks:
            if tok[0] == "c":
                src = tok[1]
                if src == eng and not is_dma:
                    if eng == "pe":
                        continue
                    if kind != "raw" and not SAME_ENGINE_FULL_SYNC:
                        continue
                assert tok[2] is not None, "unresolved PE token"
                key = ("c", src)
                need[key] = max(need.get(key, 0), tok[2])
            else:
                key = ("d", tok[1], tok[2])
                need[key] = max(need.get(key, 0), tok[3])
        waits = []
        for key, val in need.items():
            if self.seen[eng].get(key, 0) >= val:
                continue
            self.seen[eng][key] = val
            if key[0] == "c":
                waits.append((self.sem[key[1]], val))
            else:
                waits.append((self.dsem[key[1]][key[2]], val))
        return waits

    def _record(self, tok, reads, writes):
        for k in reads:
            self.readers.setdefault(k, []).append(tok)
        for k in writes:
            self.lastw[k] = tok
            self.readers[k] = []

    def _tagcheck(self, reads, writes, rtag, wtag, keep_tag):
        for k in reads:
            if rtag is not None and k in rtag:
                assert self.tags.get(k) == rtag[k], ("stale read", k, self.tags.get(k), rtag[k])
        if not keep_tag:
            for k in writes:
                self.tags[k] = (wtag or {}).get(k)

    def op(self, eng, fn, reads=(), writes=(), sig=True, rtag=None, wtag=None, keep_tag=False):
        self._tagcheck(reads, writes, rtag, wtag, keep_tag)
        waits = self._deps(eng, reads, writes, False)
        e = self.eng[eng]
        for s, v in waits[1:]:
            e.wait_ge(s, v)
        rec = {}
        ins = fn(_EngProxy(e, rec))
        self._model(eng, rec, reads, writes)
        if waits:
            ins._wait_ge(waits[0][0], waits[0][1])
        tok = ["c", eng, None]
        if eng == "pe" and not sig:
            self.pe_pending.append(tok)
        else:
            self.cnt[eng] += 1
            ins.then_inc(self.sem[eng], 1)
            tok[2] = self.cnt[eng]
            if eng == "pe":
                for p in self.pe_pending:
                    p[2] = tok[2]
                self.pe_pending = []
        self._record(tok, reads, writes)
        self.nops += 1
        if self.log is not None:
            import sys as _s
            fr = _s._getframe(1)
            nm = None
            ln = fr.f_lineno
            while fr is not None:
                if fr.f_code.co_name.startswith(("stage_", "stream_", "mod_chunk")):
                    nm = fr.f_code.co_name
                    break
                fr = fr.f_back
            self.log.append((eng, tok[2], (nm, ln), list(reads), list(writes), [(str(s), v) for s, v in waits]))
        return ins

    def dma(self, q, out, in_, reads=(), writes=()):
        waits = self._deps(q, reads, writes, True)
        i = self.drr[q]
        self.drr[q] = (i + 1) % len(self.dsem[q])
        prev = self.dval[q][i]
        key = ("d", q, i)
        if prev and self.seen[q].get(key, 0) < prev:
            self.seen[q][key] = prev
            waits.append((self.dsem[q][i], prev))
        e = self.eng[q]
        for s, v in waits:
            e.wait_ge(s, v)
        e.dma_start(out=out, in_=in_).then_inc(self.dsem[q][i], 16)
        self.dval[q][i] = prev + 16
        tok = ["d", q, i, prev + 16]
        self._record(tok, reads, writes)
        try:
            nb = 1
            for d_ in in_.shape:
                nb *= d_
            nb *= 4
        except Exception:
            nb = 1 << 20
        st = max(self.eng_free[q], max([self.tok_end.get(("w", k), 0.0) for k in list(reads) + list(writes)] + [self.tok_end.get(("r", k), 0.0) for k in writes] + [0.0]))
        en = st + 2.0 + nb / 180e3
        self.eng_free[q] = st + nb / 180e3
        for k in writes:
            self.tok_end[("w", k)] = en
            self.tok_end[("r", k)] = 0.0
        self.nops += 1

    def _model(self, eng, rec, reads, writes):
        kw = rec.get("kw", {})
        name = rec.get("name", "")

        def fsz(ap):
            try:
                sh = ap.shape
                n = 1
                for d in sh[1:]:
                    n *= d
                return n
            except Exception:
                return 128
        if eng == "pe":
            if name == "transpose":
                n = 128
                dur = 0.07 if kw["in_"].dtype == BF16 else 0.3
            else:
                n = fsz(kw["rhs"])
                dur = max(n, 64) / 2400.0 + 0.02
                if kw["rhs"].dtype == F32:
                    dur *= 4
        elif eng == "act":
            n = fsz(kw.get("out"))
            dur = 0.22 + n / 1200.0 + (0.1 if kw.get("accum_out") is not None else 0.0)
        else:
            n = fsz(kw.get("out")) if kw.get("out") is not None else 128
            dur = 0.12 + n / 960.0
            if name == "tensor_tensor_scan":
                dur = 0.12 + 2 * n / 960.0
            if name == "reciprocal":
                dur = 0.12 + 6 * n / 960.0
        ready = 0.0
        for k in reads:
            ready = max(ready, self.tok_end.get(("w", k), 0.0))
        for k in writes:
            ready = max(ready, self.tok_end.get(("w", k), 0.0), self.tok_end.get(("r", k), 0.0))
        start = max(self.eng_free[eng], ready + 0.15)
        end = start + dur
        self.eng_free[eng] = end
        for k in reads:
            self.tok_end[("r", k)] = max(self.tok_end.get(("r", k), 0.0), end)
        for k in writes:
            self.tok_end[("w", k)] = end
            self.tok_end[("r", k)] = 0.0
        self.cur_stream_end = max(self.cur_stream_end, end)

    def barrier(self):
        for e in self.eng:
            for src in self.eng:
                if src == e:
                    continue
                v = self.cnt[src]
                if v and self.seen[e].get(("c", src), 0) < v:
                    self.seen[e][("c", src)] = v
                    self.eng[e].wait_ge(self.sem[src], v)
            for q in ("sp", "pool"):
                for i, v in enumerate(self.dval[q]):
                    if v and self.seen[e].get(("d", q, i), 0) < v:
                        self.seen[e][("d", q, i)] = v
                        self.eng[e].wait_ge(self.dsem[q][i], v)


class _EngProxy:
    def __init__(self, real, rec):
        self._real = real
        self._rec = rec

    def __getattr__(self, name):
        f = getattr(self._real, name)
        rec = self._rec

        def w(*a, **k):
            rec["name"] = name
            rec["kw"] = k
            return f(*a, **k)
        return w


class Carver:
    def __init__(self, ap, nbytes):
        self.ap = ap
        self.n = nbytes
        self.off = 0

    def get(self, free_shape, dt):
        esz = 4 if dt == F32 else 2
        n = int(np.prod(free_shape)) * esz
        n_al = (n + 63) // 64 * 64
        assert self.off + n_al <= self.n, "carver overflow %d + %d > %d" % (self.off, n_al, self.n)
        v = self.ap[:, self.off // 2:(self.off + n) // 2]
        self.off += n_al
        if dt == F32:
            v = v.bitcast(F32)
        if len(free_shape) == 2:
            v = v.rearrange("p (a b) -> p a b", a=free_shape[0])
        elif len(free_shape) == 3:
            v = v.rearrange("p (a b c) -> p a b c", a=free_shape[0], b=free_shape[1])
        return v

    def reset(self):
        self.off = 0


def build_nc(stage=99, dbg=False):
    nc = bass.Bass("TRN2", target_bir_lowering=False)
    fw = FW(nc)

    def din(name, shape, dt=F32):
        return nc.dram_tensor(name, list(shape), dt, kind="ExternalInput").ap()

    x_d = din("x", [T, D])
    vecs_d = din("vecs", [128, NV])
    sinks_d = din("sinks_b", [128, 8])
    brg_d = din("brg", [128, 20])
    ident_d = din("ident", [128, 128])
    abias_d = din("abias", [128, 4, 512])
    gmask_d = din("gmask", [128, 128])
    sel_d = din("sel", [128, 16, 128])
    wada_d = din("w_ada", [D, 6 * D])
    wintm_d = din("w_in_tm", [D, 1792])
    winfm_d = din("w_in_fm", [D, 640])
    wgk2_d = din("w_gk2", [16, 256])
    wout_d = din("w_out", [D, D])
    wr_d = din("w_rt", [D, 20])
    w1_d = din("w1", [NEXP, D, 256])
    w3_d = din("w3", [NEXP, D, 256])
    w2_d = din("w2", [NEXP, 256, D])
    out_d = nc.dram_tensor("out", [T, D], F32, kind="ExternalOutput").ap()
    dbg_d = nc.dram_tensor("dbg", [128, 4096], F32, kind="ExternalOutput").ap() if dbg else None

    x_v = x_d.rearrange("(t p) d -> p t d", p=128)
    out_v = out_d.rearrange("(t p) d -> p t d", p=128)

    X1 = nc.alloc_sbuf_tensor("X1", [128, NT, D], F32).ap()
    PERS_BYTES = 11776
    pers = Carver(nc.alloc_sbuf_tensor("pers", [128, PERS_BYTES // 2], BF16).ap(), PERS_BYTES)
    REG_BYTES = 135168
    reg = Carver(nc.alloc_sbuf_tensor("reg", [128, REG_BYTES // 2], BF16).ap(), REG_BYTES)

    ident_f = pers.get([128], F32)
    ident_b = pers.get([128], BF16)
    ones_f = pers.get([128], F32)
    ones_b = pers.get([128], BF16)
    abias = pers.get([4, 512], BF16)
    gmask = pers.get([128], BF16)
    sel = pers.get([16, 128], BF16)
    vecs = pers.get([NV], F32)
    es = pers.get([8], F32)
    brg = pers.get([20], F32)
    mod = pers.get([48], F32)
    gs1 = pers.get([8], F32)
    gs2 = pers.get([8], F32)
    nbgk = pers.get([2], F32)
    ssx = pers.get([NT], F32)
    rsx = pers.get([NT], F32)
    sc_b = pers.get([8], BF16)
    tmp8 = pers.get([8], F32)
    dbg_sb = pers.get([16], F32)

    PS = [nc.alloc_psum_tensor("ps%d" % i, [128, 512], F32).ap() for i in range(8)]
    PSB = [p.bitcast(BF16) for p in PS]

    def bk(i):
        return ("ps", i)

    fw.dma("sp", ident_f, ident_d, writes=["ident_f"])
    fw.dma("sp", vecs, vecs_d, writes=["vecs"])
    fw.dma("sp", es, sinks_d, writes=["es"])
    fw.dma("sp", brg, brg_d, writes=["brg"])
    fw.dma("pool", ident_b, ident_d, writes=["ident_b"])
    fw.dma("pool", abias, abias_d, writes=["abias"])
    fw.dma("pool", gmask, gmask_d, writes=["gmask"])
    fw.dma("pool", sel, sel_d, writes=["sel"])
    fw.dma("sp", X1[:, 0:4, :], x_v[:, 0:4, :], writes=[("X1", t) for t in range(4)])
    fw.op("dve", lambda e: e.memset(ones_f, 1.0), writes=["ones_f"])
    fw.op("dve", lambda e: e.memset(ones_b, 1.0), writes=["ones_b"])

    HR = 8
    hT = reg.get([8, HR * 128], BF16)
    win_tm = reg.get([8, 1792], BF16)
    win_fm = reg.get([8, 640], BF16)
    wout_sb = reg.get([8, 1024], BF16)
    wada_buf = [reg.get([8, 128], BF16) for _ in range(2)]
    xn = reg.get([1, 1024], BF16)
    sqq = reg.get([512], F32)
    qn = reg.get([512], BF16)
    kn = reg.get([128], BF16)
    ssq = reg.get([8], F32)
    rq = reg.get([8], F32)
    ssk = reg.get([2], F32)
    rk = reg.get([2], F32)
    qTA = reg.get([2, 512], BF16)
    qTB = reg.get([2, 512], BF16)
    kT = reg.get([4 * 128], BF16)
    vaug = reg.get([4, 2, 65], BF16)
    vg = reg.get([2, 512], BF16)
    sog = reg.get([2, 512], BF16)
    PT = reg.get([4, 512], BF16)
    den = reg.get([8], F32)
    on = reg.get([512], F32)
    ssa = reg.get([1], F32)
    ra = reg.get([1], F32)
    ya = reg.get([512], BF16)
    yA = reg.get([8, 4, 128], BF16)
    yG = reg.get([4, 128], BF16)
    OFF_LBUF = reg.off
    lbuf = reg.get([2, 512], F32)
    Bc = reg.get([2, 512], F32)
    elast = reg.get([2, 4], F32)
    OFF_QDPAD = reg.off
    qd_pad = reg.get([4, 512], BF16)
    kdT = reg.get([2, 512], BF16)
    kdecT = reg.get([2, 512], BF16)
    kdec = reg.get([4, 256], BF16)
    AT = reg.get([4, 128], BF16)
    S = reg.get([2, 128], F32)
    Sbf = reg.get([2, 128], BF16)
    ssg = reg.get([4], F32)
    rg = reg.get([4], F32)
    t1 = reg.get([512], F32)
    sq_b = sqq.bitcast(BF16)
    esc = t1
    sqB = reg.get([512], BF16)
    yg = reg.get([512], BF16)
    lrT_pad = reg.get([512], BF16)
    wgk2_pad = reg.get([256], BF16)
    print("phase1 region bytes", reg.off, "of", reg.n)

    wada_v = wada_d.rearrange("(k p) n -> p k n", p=128)

    ccol = vecs[:, 73:81]
    fw.op("act", lambda e: e.activation(out=tmp8, in_=ccol, func=AF.Exp, scale=-1.0), reads=["vecs"], writes=["tmp8"])
    fw.op("dve", lambda e: e.tensor_scalar_add(out=tmp8, in0=tmp8, scalar1=1.0), reads=["tmp8"], writes=["tmp8"])
    fw.op("dve", lambda e: e.reciprocal(out=tmp8, in_=tmp8), reads=["tmp8"], writes=["tmp8"])
    fw.op("dve", lambda e: e.tensor_tensor(out=sc_b, in0=tmp8, in1=ccol, op=ALU.mult), reads=["tmp8", "vecs"], writes=["sc_b"])
    MODB = 5
    wctr = [0]

    def mod_chunk(c):
        bi = wctr[0] % 2
        wctr[0] += 1
        buf = wada_buf[bi]
        if 'chunkdma' in DBG_SKIP and c >= 16:
            return
        fw.dma("pool", buf, wada_v[:, :, c * 128:(c + 1) * 128], writes=[("wada", bi)])
        for k in range(8):
            fw.op("pe", lambda e, k=k: e.matmul(out=PS[MODB][:, MODCOL:MODCOL + 1], lhsT=buf[:, k, :], rhs=sc_b[:, k:k + 1], start=(k == 0), stop=(k == 7)),
                  reads=[("wada", bi), "sc_b"], writes=[bk(MODB)], sig=(k == 7), keep_tag=True)
        fw.op("dve", lambda e: e.tensor_tensor(out=mod[:, c:c + 1], in0=PS[MODB][:, MODCOL:MODCOL + 1], in1=vecs[:, c:c + 1], op=ALU.add),
              reads=[bk(MODB), "vecs"], writes=[("mod", c // 8)])

    MK = [("mod", i) for i in range(6)]
    big = [reg.ap[:, boff // 2:(boff + 8192) // 2].rearrange("p (k n) -> p k n", k=8) for boff in (OFF_LBUF, OFF_QDPAD)]
    bigkeys = [[("lbuf", 0), ("lbuf", 1), ("Bc", 0), ("Bc", 1)], ["qd_pad", ("kdT", 0), ("kdT", 1), ("kdecT", 0), ("kdecT", 1)]]
    for cc in range(4):
        fw.dma("pool", big[cc % 2], wada_v[:, :, cc * 512:(cc + 1) * 512], writes=bigkeys[cc % 2])
        if cc == 0:
            for g_ in range(1, 4):
                pass
        for j in range(4):
            c = cc * 4 + j
            for k in range(8):
                fw.op("pe", lambda e, k=k, j=j, cc=cc: e.matmul(out=PS[MODB][:, MODCOL:MODCOL + 1], lhsT=big[cc % 2][:, k, j * 128:(j + 1) * 128], rhs=sc_b[:, k:k + 1], start=(k == 0), stop=(k == 7)),
                      reads=bigkeys[cc % 2] + ["sc_b"], writes=[bk(MODB)], sig=(k == 7), keep_tag=True)
            fw.op("dve", lambda e, c=c: e.tensor_tensor(out=mod[:, c:c + 1], in0=PS[MODB][:, MODCOL:MODCOL + 1], in1=vecs[:, c:c + 1], op=ALU.add),
                  reads=[bk(MODB), "vecs"], writes=[("mod", c // 8)])
    fw.op("dve", lambda e: e.scalar_tensor_tensor(out=gs1, in0=mod[:, 8:16], scalar=1.0, in1=vecs[:, 48:56], op0=ALU.add, op1=ALU.mult),
          reads=[("mod", 1), "vecs"], writes=["gs1"])
    fw.op("dve", lambda e: e.tensor_scalar(out=nbgk, in0=vecs[:, 69:71], scalar1=-1.0, scalar2=None, op0=ALU.mult),
          reads=["vecs"], writes=["nbgk"])
    fw.op("act", lambda e: e.activation(out=es, in_=es, func=AF.Exp), reads=["es"], writes=["es"])
    wintm_v = wintm_d.rearrange("(k p) n -> p k n", p=128)
    fw.dma("pool", win_tm[:, :, 0:768], wintm_v[:, :, 0:768], writes=["win_tmA"])
    fw.dma("pool", win_tm[:, :, 768:1792], wintm_v[:, :, 768:1792], writes=["win_tmB"])
    fw.dma("pool", win_fm, winfm_d.rearrange("(k p) n -> p k n", p=128), writes=["win_fm"])
    for g in range(1, 4):
        fw.dma("sp", X1[:, 4 * g:4 * g + 4, :], x_v[:, 4 * g:4 * g + 4, :], reads=["win_tmA"], writes=[("X1", t) for t in range(4 * g, 4 * g + 4)])
    fw.op("dve", lambda e: e.memset(wgk2_pad, 0.0), writes=["wgk2"])
    fw.dma("pool", wgk2_pad[0:16, :], wgk2_d, reads=[], writes=["wgk2"])

    GB = (6, 7)

    def stream_M1():
        for c in range(16, 24):
            mod_chunk(c)
            yield
        fw.dma("pool", wout_sb, wout_d.rearrange("(k p) n -> p k n", p=128), writes=["wout"])
        yield
        for hh in range(2):
            for kk in range(4):
                k = hh * 4 + kk
                fw.op("dve", lambda e, k=k: e.tensor_scalar(out=yg[:, 0:256].bitcast(F32), in0=ident_f, scalar1=mod[:, 16 + k:17 + k], scalar2=None, op0=ALU.mult),
                      reads=["ident_f", ("mod", 2)], writes=["yg"])
                fw.op("pe", lambda e, k=k, hh=hh, kk=kk: e.matmul(out=PS[GB[hh]][:, kk * 128:(kk + 1) * 128], lhsT=ones_f, rhs=yg[:, 0:256].bitcast(F32), start=True, stop=True),
                      reads=["ones_f", "yg"], writes=[bk(GB[hh])])
            for k in range(8):
                fw.op("dve", lambda e, k=k, hh=hh: e.tensor_tensor(out=wout_sb[:, k, hh * 512:(hh + 1) * 512], in0=wout_sb[:, k, hh * 512:(hh + 1) * 512],
                                                               in1=PS[GB[hh]], op=ALU.mult),
                      reads=["wout", bk(GB[hh])], writes=["wout"])
            yield

    def stream_M2():
        for c in range(24, 48):
            mod_chunk(c)
            yield
        fw.op("dve", lambda e: e.scalar_tensor_tensor(out=gs2, in0=mod[:, 32:40], scalar=1.0, in1=vecs[:, 56:64], op0=ALU.add, op1=ALU.mult),
              reads=[("mod", 4), "vecs"], writes=["gs2"])
        yield

    fw.op("dve", lambda e: e.memset(qTA, 0.0), writes=["qTA0", "qTA1"])
    fw.op("dve", lambda e: e.memset(qTB, 0.0), writes=["qTB0", "qTB1"])
    fw.op("dve", lambda e: e.memset(vaug, 1.0), writes=[("vaug", t) for t in range(4)])
    fw.op("dve", lambda e: e.memset(qd_pad, 0.0), writes=["qd_pad"])
    fw.op("dve", lambda e: e.memset(lrT_pad, 0.0), writes=["lrT"])
    fw.op("dve", lambda e: e.memset(S, 0.0), writes=["S"])
    fw.op("dve", lambda e: e.memset(Sbf, 0.0), writes=["Sbf"])

    alt = [0]

    def evac_affine(out, in_, scale, bias, reads, writes, wtag=None, rtag=None):
        alt[0] ^= 1
        if alt[0]:
            if bias is None:
                fw.op("act", lambda e: e.activation(out=out, in_=in_, func=AF.Copy, scale=scale), reads=reads, writes=writes, wtag=wtag, rtag=rtag)
            else:
                fw.op("act", lambda e: e.activation(out=out, in_=in_, func=AF.Identity, scale=scale, bias=bias), reads=reads, writes=writes, wtag=wtag, rtag=rtag)
        else:
            if bias is None:
                fw.op("dve", lambda e: e.tensor_scalar(out=out, in0=in_, scalar1=scale, scalar2=None, op0=ALU.mult), reads=reads, writes=writes, wtag=wtag, rtag=rtag)
            else:
                fw.op("dve", lambda e: e.tensor_scalar(out=out, in0=in_, scalar1=scale, scalar2=bias, op0=ALU.mult, op1=ALU.add), reads=reads, writes=writes, wtag=wtag, rtag=rtag)

    def rstd_act(out, in_, n, extra_bias, rk_, wk_):
        fw.op("act", lambda e: e.activation(out=out, in_=in_, func=AF.Ln, scale=1.0 / n, bias=EPS), reads=rk_, writes=wk_)
        fw.op("act", lambda e: e.activation(out=out, in_=out, func=AF.Exp, scale=-0.5, bias=extra_bias), reads=wk_, writes=wk_)

    TB = 0

    def stage_T1(t):
        hs = t % HR
        xs = 0
        fw.op("act", lambda e: e.activation(out=sq_b, in_=X1[:, t, :], func=AF.Square, accum_out=ssx[:, t:t + 1]),
              reads=[("X1", t)], writes=["sqq", ("ssx", t)])
        rstd_act(rsx[:, t:t + 1], ssx[:, t:t + 1], D, 0.0, [("ssx", t)], [("rsx", t)])
        fw.op("dve", lambda e: e.tensor_scalar(out=xn[:, xs, :], in0=X1[:, t, :], scalar1=rsx[:, t:t + 1], scalar2=None, op0=ALU.mult),
              reads=[("X1", t), ("rsx", t)], writes=[("xn", xs)])
        yield
        T1B = (TB, 3)
        for k in range(8):
            fw.op("pe", lambda e, k=k: e.transpose(out=PSB[T1B[k // 4]][:, (k % 4) * 128:(k % 4 + 1) * 128], in_=xn[:, xs, k * 128:(k + 1) * 128], identity=ident_b),
                  reads=[("xn", xs), "ident_b"], writes=[bk(T1B[k // 4])], sig=(k % 4 == 3), wtag={bk(T1B[k // 4]): ("T1", t)})
        for k in range(4):
            fw.op("act", lambda e, k=k: e.activation(out=hT[:, k, hs * 128:(hs + 1) * 128], in_=PSB[TB][:, k * 128:(k + 1) * 128], func=AF.Identity,
                                                     scale=gs1[:, k:k + 1], bias=mod[:, k:k + 1]),
                  reads=[bk(TB), "gs1", ("mod", 0)], writes=[("hT", hs)], wtag={("hT", hs): t}, rtag={bk(TB): ("T1", t)})
        hv = hT[:, 4:8, hs * 128:(hs + 1) * 128]
        fw.op("dve", lambda e: e.tensor_tensor(out=hv, in0=PSB[3][:, 0:512].rearrange("p (k n) -> p k n", k=4),
                                               in1=gs1[:, 4:8].unsqueeze(2).to_broadcast([128, 4, 128]), op=ALU.mult),
              reads=[bk(3), "gs1"], writes=[("hT", hs)], wtag={("hT", hs): t}, rtag={bk(3): ("T1", t)})
        fw.op("dve", lambda e: e.tensor_tensor(out=hv, in0=hv, in1=mod[:, 4:8].unsqueeze(2).to_broadcast([128, 4, 128]), op=ALU.add),
              reads=[("hT", hs), ("mod", 0)], writes=[("hT", hs)], wtag={("hT", hs): t})
        yield

    QB, KVB, VGB, OGB = 1, 2, 6, 7

    def stage_T2(t):
        hs = t % HR
        for bank, c0, n in ((QB, 0, 512), (KVB, 512, 256)):
            for k in range(8):
                fw.op("pe", lambda e, bank=bank, c0=c0, n=n, k=k: e.matmul(
                    out=PS[bank][:, 0:n], lhsT=hT[:, k, hs * 128:(hs + 1) * 128], rhs=win_tm[:, k, c0:c0 + n],
                    start=(k == 0), stop=(k == 7)),
                    reads=[("hT", hs), "win_tmA"], writes=[bk(bank)], sig=(k == 7), rtag={("hT", hs): t}, wtag={bk(bank): ("proj", t)})
            yield

    def stage_T3(t):
        qs = t % 2
        vs = t % 4
        fw.op("act", lambda e: e.activation(out=sqq, in_=PS[QB], func=AF.Square), reads=[bk(QB)], writes=["sqq"], rtag={bk(QB): ("proj", t)})
        fw.op("dve", lambda e: e.tensor_reduce(out=ssq, in_=sqq.rearrange("p (h d) -> p h d", h=8), axis=AX.X, op=ALU.add),
              reads=["sqq"], writes=["ssq"])
        rstd_act(rq, ssq, 64, -LN8, ["ssq"], ["rq"])
        fw.op("dve", lambda e: e.tensor_tensor(out=qn.rearrange("p (h d) -> p h d", h=8), in0=PS[QB].rearrange("p (h d) -> p h d", h=8),
                                               in1=rq.unsqueeze(2).to_broadcast([128, 8, 64]), op=ALU.mult),
              reads=[bk(QB), "rq"], writes=["qn"], rtag={bk(QB): ("proj", t)})
        yield
        fw.op("act", lambda e: e.activation(out=sqq[:, 0:128], in_=PS[KVB][:, 0:128], func=AF.Square), reads=[bk(KVB)], writes=["sqq"])
        fw.op("dve", lambda e: e.tensor_reduce(out=ssk, in_=sqq[:, 0:128].rearrange("p (h d) -> p h d", h=2), axis=AX.X, op=ALU.add),
              reads=["sqq"], writes=["ssk"])
        rstd_act(rk, ssk, 64, 0.0, ["ssk"], ["rk"])
        fw.op("dve", lambda e: e.tensor_tensor(out=kn.rearrange("p (h d) -> p h d", h=2), in0=PS[KVB][:, 0:128].rearrange("p (h d) -> p h d", h=2),
                                               in1=rk.unsqueeze(2).to_broadcast([128, 2, 64]), op=ALU.mult),
              reads=[bk(KVB), "rk"], writes=["kn"], rtag={bk(KVB): ("proj", t)})
        yield
        if 'T3v' in DBG_SKIP:
            return
        fw.op("act", lambda e: e.activation(out=vaug[:, t % 4, :, 0:64], in_=PS[KVB][:, 128:256].rearrange("p (h d) -> p h d", h=2), func=AF.Copy),
              reads=[bk(KVB)], writes=[("vaug", t % 4)])
        if 'T3t' in DBG_SKIP:
            return
        for i in range(4):
            fw.op("pe", lambda e, i=i: e.transpose(out=PSB[TB][:, i * 128:(i + 1) * 128], in_=qn[:, i * 128:(i + 1) * 128], identity=ident_b),
                  reads=["qn", "ident_b"], writes=[bk(TB)], sig=False)
        fw.op("pe", lambda e: e.transpose(out=PSB[TB][:, 512:640], in_=kn, identity=ident_b),
              reads=["kn", "ident_b"], writes=[bk(TB)], sig=True, wtag={bk(TB): ("T3", t)})
        if 'T3e' in DBG_SKIP:
            return
        gq = vecs[:, 71:72]
        gk = vecs[:, 72:73]
        fw.op("act", lambda e: e.activation(out=qTA[0:64, qs, :], in_=PSB[TB][0:64, 0:512], func=AF.Copy, scale=gq[0:64, :]),
              reads=[bk(TB), "vecs"], writes=["qTA%d" % qs])
        fw.op("dve", lambda e: e.tensor_scalar(out=qTB[64:128, qs, :], in0=PSB[TB][64:128, 0:512], scalar1=gq[64:128, :], scalar2=None, op0=ALU.mult),
              reads=[bk(TB), "vecs"], writes=["qTB%d" % qs])
        fw.op("dve", lambda e: e.tensor_scalar(out=kT[:, (t % 4) * 128:(t % 4 + 1) * 128], in0=PSB[TB][:, 512:640], scalar1=gk, scalar2=None, op0=ALU.mult),
              reads=[bk(TB), "vecs"], writes=[("kT", t % 4)], rtag={bk(TB): ("T3", t)})
        yield

    SB = DBG_SB
    OB = DBG_OB

    def stage_T4(t):
        qs = t % 2
        ys = t % 8
        blocks = [(t, 0)] + ([(t - 1, 1)] if t > 0 else [])
        si = 0
        allpts = []
        for kv in range(2):
            qT = qTA if kv == 0 else qTB
            qkey = ("qTA%d" if kv == 0 else "qTB%d") % qs
            pts = []
            for (blk, which) in blocks:
                bank = SB[si % 2]
                pslot = si % 4
                si += 1
                fw.op("pe", lambda e, bank=bank, blk=blk, qT=qT: e.matmul(out=PS[bank], lhsT=kT[:, (blk % 4) * 128:(blk % 4 + 1) * 128], rhs=qT[:, qs, :], start=True, stop=False),
                      reads=[("kT", blk % 4), qkey], writes=[bk(bank)], sig=False)
                fw.op("pe", lambda e, bank=bank, which=which, kv=kv: e.matmul(out=PS[bank], lhsT=ident_b, rhs=abias[:, kv * 2 + which, :], start=False, stop=True),
                      reads=["ident_b", "abias"], writes=[bk(bank)], sig=True)
                fw.op("act", lambda e, bank=bank, pslot=pslot: e.activation(out=PT[:, pslot, :], in_=PS[bank], func=AF.Exp),
                      reads=[bk(bank)], writes=[("PT", pslot)])
                pts.append((pslot, blk))
                yield
            allpts.append(pts)
        for kv in range(2):
            pts = allpts[kv]
            for g in range(4):
                h = kv * 4 + g
                ob = OB[kv]
                for pi, (pslot, blk) in enumerate(pts):
                    fw.op("pe", lambda e, ob=ob, g=g, pslot=pslot, blk=blk, pi=pi, kv=kv, pts=pts: e.matmul(
                        out=PS[ob][:, g * 65:(g + 1) * 65], lhsT=PT[:, pslot, g * 128:(g + 1) * 128], rhs=vaug[:, blk % 4, kv, :],
                        start=(pi == 0), stop=(pi == len(pts) - 1)),
                        reads=[("PT", pslot), ("vaug", blk % 4)], writes=[bk(ob)], sig=(g == 3 and pi == len(pts) - 1))
            yield
        for kv in range(2):
            ov = PS[OB[kv]][:, 0:260].rearrange("p (h d) -> p h d", h=4)
            fw.op("dve", lambda e, ov=ov, kv=kv: e.tensor_tensor(out=den[:, kv * 4:(kv + 1) * 4], in0=ov[:, :, 64], in1=es[:, kv * 4:(kv + 1) * 4], op=ALU.add),
                  reads=[bk(OB[kv]), "es"], writes=["den"])
        fw.op("dve", lambda e: e.reciprocal(out=den, in_=den), reads=["den"], writes=["den"])
        yield
        for kv in range(2):
            ov = PS[OB[kv]][:, 0:260].rearrange("p (h d) -> p h d", h=4)
            fw.op("dve", lambda e, ov=ov, kv=kv: e.tensor_tensor(
                out=on[:, kv * 256:(kv + 1) * 256].rearrange("p (h d) -> p h d", h=4), in0=ov[:, :, 0:64],
                in1=den[:, kv * 4:(kv + 1) * 4].unsqueeze(2).to_broadcast([128, 4, 64]), op=ALU.mult),
                reads=[bk(OB[kv]), "den"], writes=["on"])
        yield
        fw.op("act", lambda e: e.activation(out=sqq, in_=on, func=AF.Square, accum_out=ssa), reads=["on"], writes=["sqq", "ssa"])
        rstd_act(ra, ssa, 512, 0.0, ["ssa"], ["ra"])
        fw.op("dve", lambda e: e.tensor_scalar(out=ya, in0=on, scalar1=ra, scalar2=None, op0=ALU.mult), reads=["on", "ra"], writes=["ya"])
        yield
        for i in range(4):
            fw.op("pe", lambda e, i=i: e.transpose(out=PSB[TB][:, i * 128:(i + 1) * 128], in_=ya[:, i * 128:(i + 1) * 128], identity=ident_b),
                  reads=["ya", "ident_b"], writes=[bk(TB)], sig=(i == 3), wtag={bk(TB): ("T4", t)})
        for i in range(4):
            evac_affine(yA[:, ys, i, :], PSB[TB][:, i * 128:(i + 1) * 128], vecs[:, 64 + i:65 + i], None, [bk(TB), "vecs"], [("yA", ys)], wtag={("yA", ys): t}, rtag={bk(TB): ("T4", t)})
        yield

    FB = (6, 7)

    def stage_G1(T0, GT):
        hcol = (T0 % HR) * 128
        NG_ = GT * 128
        hkeys = [("hT", (T0 + i) % HR) for i in range(GT)]

        def fm_proj(m, bank):
            for k in range(8):
                fw.op("pe", lambda e, k=k: e.matmul(out=PS[bank][:, 0:NG_], lhsT=win_fm[:, k, m * 128:(m + 1) * 128], rhs=hT[:, k, hcol:hcol + NG_],
                                                    start=(k == 0), stop=(k == 7)),
                      reads=hkeys + ["win_fm"], writes=[bk(bank)], sig=(k == 7), rtag={("hT", (T0 + i) % HR): T0 + i for i in range(GT)})
        fm_proj(4, FB[0])
        fw.op("act", lambda e: e.activation(out=lrT_pad[0:16, 0:NG_], in_=PS[FB[0]][0:16, 0:NG_], func=AF.Copy), reads=[bk(FB[0])], writes=["lrT"])
        yield
        for c in range(2):
            bank = FB[(c + 1) % 2]
            fw.op("pe", lambda e, c=c, bank=bank: e.matmul(out=PS[bank][:, 0:NG_], lhsT=wgk2_pad[:, c * 128:(c + 1) * 128], rhs=lrT_pad[:, 0:NG_], start=True, stop=True),
                  reads=["wgk2", "lrT"], writes=[bk(bank)])
            fw.op("act", lambda e, c=c, bank=bank: e.activation(out=lbuf[:, c, 0:NG_], in_=PS[bank][:, 0:NG_], func=AF.Exp, scale=-1.0, bias=nbgk[:, c:c + 1]),
                  reads=[bk(bank), "nbgk"], writes=[("lbuf", c)])
            fw.op("act", lambda e, c=c: e.activation(out=lbuf[:, c, 0:NG_], in_=lbuf[:, c, 0:NG_], func=AF.Ln, bias=1.0), reads=[("lbuf", c)], writes=[("lbuf", c)])
            yield
            for j in range(GT):
                fw.op("dve", lambda e, c=c, j=j: e.tensor_tensor_scan(out=Bc[:, c, j * 128:(j + 1) * 128], data0=ones_f, data1=lbuf[:, c, j * 128:(j + 1) * 128],
                                                                      initial=0.0, op0=ALU.mult, op1=ALU.add),
                      reads=[("lbuf", c), "ones_f"], writes=[("Bc", c)])
            yield
            fw.op("act", lambda e, c=c: e.activation(out=lbuf[:, c, 0:NG_], in_=Bc[:, c, 0:NG_], func=AF.Exp, scale=-1.0 / 16, bias=-LN8), reads=[("Bc", c)], writes=[("lbuf", c)])
            fw.op("act", lambda e, c=c: e.activation(out=elast[:, c, 0:GT], in_=Bc[:, c, 0:NG_].rearrange("p (j i) -> p j i", j=GT)[:, :, 127], func=AF.Exp, scale=-1.0 / 16),
                  reads=[("Bc", c)], writes=[("elast", c)])
            fw.op("act", lambda e, c=c: e.activation(out=Bc[:, c, 0:NG_], in_=Bc[:, c, 0:NG_], func=AF.Exp, scale=1.0 / 16), reads=[("Bc", c)], writes=[("Bc", c)])
            yield
        for c in range(2):
            bq = FB[0]
            fm_proj(c, bq)
            for hh in range(2):
                fw.op("dve", lambda e, c=c, hh=hh: e.tensor_tensor(out=qd_pad[hh * 64:(hh + 1) * 64, 2 * c + hh, 0:NG_], in0=PS[bq][hh * 64:(hh + 1) * 64, 0:NG_],
                                                                   in1=lbuf[hh * 64:(hh + 1) * 64, c, 0:NG_], op=ALU.mult),
                      reads=[bk(bq), ("lbuf", c)], writes=["qd_pad"])
            yield
            bkk = FB[1]
            fm_proj(2 + c, bkk)
            fw.op("dve", lambda e, c=c: e.tensor_tensor(out=kdT[:, c, 0:NG_], in0=PS[bkk][:, 0:NG_], in1=Bc[:, c, 0:NG_], op=ALU.mult),
                  reads=[bk(bkk), ("Bc", c)], writes=[("kdT", c)])
            yield
            for j in range(GT):
                fw.op("dve", lambda e, c=c, j=j: e.tensor_scalar(out=kdecT[:, c, j * 128:(j + 1) * 128], in0=kdT[:, c, j * 128:(j + 1) * 128],
                                                                 scalar1=elast[:, c, j:j + 1], scalar2=None, op0=ALU.mult),
                      reads=[("kdT", c), ("elast", c)], writes=[("kdecT", c)])
            yield
        for j in range(GT):
            for c in range(2):
                fw.op("pe", lambda e, c=c, j=j: e.transpose(out=PSB[6][:, (j * 2 + c) * 128:(j * 2 + c + 1) * 128], in_=kdecT[:, c, j * 128:(j + 1) * 128], identity=ident_b),
                      reads=[("kdecT", c), "ident_b"], writes=[bk(6)], sig=(j == GT - 1 and c == 1))
        fw.op("act", lambda e: e.activation(out=kdec[:, 0:GT, :].rearrange("p j c -> p (j c)"), in_=PSB[6][:, 0:GT * 256], func=AF.Copy), reads=[bk(6)], writes=["kdec"])
        yield

    def stage_B0(t):
        hs = t % HR
        vs = t % 2
        for bank, c0, n in ((VGB, 768, 512), (OGB, 1280, 512)):
            for k in range(8):
                fw.op("pe", lambda e, bank=bank, c0=c0, n=n, k=k: e.matmul(
                    out=PS[bank][:, 0:n], lhsT=hT[:, k, hs * 128:(hs + 1) * 128], rhs=win_tm[:, k, c0:c0 + n],
                    start=(k == 0), stop=(k == 7)),
                    reads=[("hT", hs), "win_tmB"], writes=[bk(bank)], sig=(k == 7), rtag={("hT", hs): t})
            yield
        fw.op("act", lambda e: e.activation(out=vg[:, vs, :], in_=PS[VGB], func=AF.Copy), reads=[bk(VGB)], writes=[("vg", vs)])
        yield
        fw.op("act", lambda e: e.activation(out=esc, in_=PS[OGB], func=AF.Exp, scale=-1.0), reads=[bk(OGB)], writes=["t1"])
        fw.op("act", lambda e: e.activation(out=esc, in_=esc, func=AF.Ln, bias=1.0), reads=["t1"], writes=["t1"])
        fw.op("act", lambda e: e.activation(out=esc, in_=esc, func=AF.Exp, scale=-1.0), reads=["t1"], writes=["t1"])
        fw.op("dve", lambda e: e.tensor_tensor(out=sog[:, vs, :], in0=PS[OGB], in1=esc, op=ALU.mult), reads=[bk(OGB), "t1"], writes=[("sog", vs)])
        yield

    AB, OGB2, UB = 6, 7, 6

    def stage_G2(t, j):
        vs = t % 2
        ys = t % 8
        for h in range(4):
            c = h // 2
            fw.op("pe", lambda e, h=h, c=c: e.matmul(out=PS[AB][:, h * 128:(h + 1) * 128], lhsT=kdT[:, c, j * 128:(j + 1) * 128],
                                                     rhs=qd_pad[:, h, j * 128:(j + 1) * 128], start=True, stop=True),
                  reads=[("kdT", c), "qd_pad"], writes=[bk(AB)], sig=(h == 3))
        yield
        fw.op("dve", lambda e: e.tensor_tensor(out=AT, in0=PS[AB].rearrange("p (h i) -> p h i", h=4), in1=gmask.unsqueeze(1).to_broadcast([128, 4, 128]), op=ALU.mult),
              reads=[bk(AB), "gmask"], writes=["AT"])
        yield
        for h in range(4):
            c = h // 2
            fw.op("pe", lambda e, h=h: e.matmul(out=PS[OGB2][:, h * 128:(h + 1) * 128], lhsT=AT[:, h, :], rhs=vg[:, vs, h * 128:(h + 1) * 128], start=True, stop=False),
                  reads=["AT", ("vg", vs)], writes=[bk(OGB2)], sig=False)
            fw.op("pe", lambda e, h=h, c=c: e.matmul(out=PS[OGB2][:, h * 128:(h + 1) * 128], lhsT=qd_pad[:, h, j * 128:(j + 1) * 128], rhs=Sbf[:, c, :], start=False, stop=True),
                  reads=["qd_pad", "Sbf"], writes=[bk(OGB2)], sig=(h == 3))
        yield
        for c in range(2):
            fw.op("pe", lambda e, c=c: e.matmul(out=PS[UB][:, c * 256:(c + 1) * 256], lhsT=kdec[:, j, c * 128:(c + 1) * 128], rhs=vg[:, vs, c * 256:(c + 1) * 256], start=True, stop=True),
                  reads=["kdec", ("vg", vs)], writes=[bk(UB)], sig=(c == 1))
        yield
        for c in range(2):
            for hh in range(2):
                fw.op("dve", lambda e, c=c, hh=hh: e.scalar_tensor_tensor(
                    out=S[hh * 64:(hh + 1) * 64, c, :], in0=S[hh * 64:(hh + 1) * 64, c, :], scalar=elast[hh * 64:(hh + 1) * 64, c, j:j + 1],
                    in1=PS[UB][hh * 64:(hh + 1) * 64, c * 256 + hh * 128:c * 256 + (hh + 1) * 128], op0=ALU.mult, op1=ALU.add),
                    reads=["S", ("elast", c), bk(UB)], writes=["S"])
        fw.op("act", lambda e: e.activation(out=Sbf, in_=S, func=AF.Copy), reads=["S"], writes=["Sbf"])
        yield
        fw.op("act", lambda e: e.activation(out=sqB, in_=PS[OGB2], func=AF.Square), reads=[bk(OGB2)], writes=["sqB"])
        fw.op("dve", lambda e: e.tensor_reduce(out=ssg, in_=sqB.rearrange("p (h d) -> p h d", h=4), axis=AX.X, op=ALU.add), reads=["sqB"], writes=["ssg"])
        rstd_act(rg, ssg, 128, 0.0, ["ssg"], ["rg"])
        fw.op("dve", lambda e: e.tensor_tensor(out=t1.rearrange("p (h d) -> p h d", h=4), in0=PS[OGB2].rearrange("p (h d) -> p h d", h=4),
                                               in1=rg.unsqueeze(2).to_broadcast([128, 4, 128]), op=ALU.mult),
              reads=[bk(OGB2), "rg"], writes=["t1"])
        yield
        fw.op("dve", lambda e: e.tensor_tensor(out=yg, in0=t1, in1=sog[:, vs, :], op=ALU.mult), reads=["t1", ("sog", vs)], writes=["yg"])
        yield
        for i in range(4):
            fw.op("pe", lambda e, i=i: e.transpose(out=PSB[6][:, i * 128:(i + 1) * 128], in_=yg[:, i * 128:(i + 1) * 128], identity=ident_b),
                  reads=["yg", "ident_b"], writes=[bk(6)], sig=(i == 3))
        yield
        fw.op("act", lambda e: e.activation(out=yG.rearrange("p a b -> p (a b)"), in_=PSB[6][:, 0:512], func=AF.Copy, scale=vecs[:, 68:69]),
              reads=[bk(6), "vecs"], writes=["yG"], wtag={"yG": t})
        yield

    WB = (6, 7)

    def stage_W(t):
        ys = t % 8
        for dh in range(2):
            for k in range(8):
                fw.op("pe", lambda e, dh=dh, k=k: e.matmul(out=PS[WB[dh]], lhsT=(yA[:, ys, k, :] if k < 4 else yG[:, k - 4, :]), rhs=wout_sb[:, k, dh * 512:(dh + 1) * 512], start=(k == 0), stop=(k == 7)),
                      reads=[("yA", ys), "yG", "wout"], writes=[bk(WB[dh])], sig=(k == 7), rtag={("yA", ys): t, "yG": t})
            fw.op("dve", lambda e, dh=dh: e.tensor_tensor(out=X1[:, t, dh * 512:(dh + 1) * 512], in0=PS[WB[dh]], in1=X1[:, t, dh * 512:(dh + 1) * 512], op=ALU.add),
                  reads=[bk(WB[dh]), ("X1", t)], writes=[("X1", t)])
            yield

    def chain(*gens):
        for g_ in gens:
            yield from g_

    def run_streams(streams):
        streams = [s for s in streams if s is not None]
        if SEQ_STREAMS:
            for s in streams:
                for _ in s:
                    pass
            return
        if not GREEDY:
            while streams:
                nxt = []
                for s in streams:
                    try:
                        next(s)
                        nxt.append(s)
                    except StopIteration:
                        pass
                streams = nxt
            return
        ready = [0.0] * len(streams)
        alive = list(range(len(streams)))
        while alive:
            i = min(alive, key=lambda j: ready[j])
            fw.cur_stream_end = 0.0
            try:
                next(streams[i])
                ready[i] = max(ready[i], fw.cur_stream_end)
            except StopIteration:
                alive.remove(i)

    def interleave(a, b):
        gens = [a, b]
        while gens:
            nxt = []
            for g_ in gens:
                try:
                    next(g_)
                    nxt.append(g_)
                    yield
                except StopIteration:
                    pass
            gens = nxt

    def stream_A(g):
        ts = list(range(GROUPS[g][0], GROUPS[g][0] + GROUPS[g][1]))
        parts = [stage_T1(ts[0]), stage_T2(ts[0])]
        for i, t in enumerate(ts):
            parts.append(stage_T3(t))
            if i + 1 < len(ts) and PIPE_A:
                parts.append(interleave(stage_T4(t), chain(stage_T1(ts[i + 1]), stage_T2(ts[i + 1]))))
            else:
                parts.append(stage_T4(t))
                if i + 1 < len(ts):
                    parts.append(stage_T1(ts[i + 1]))
                    parts.append(stage_T2(ts[i + 1]))
        return chain(*parts)

    def stream_B(g):
        T0, n_ = GROUPS[g]
        return chain(stage_G1(T0, n_), *[chain(stage_B0(t), stage_G2(t, t - T0), stage_W(t)) for t in range(T0, T0 + n_)])

    def early_exit():
        fw.barrier()
        for g_ in range(4):
            fw.dma("sp", out_v[:, 4 * g_:4 * g_ + 4, :], X1[:, 4 * g_:4 * g_ + 4, :], reads=[("X1", t) for t in range(4 * g_, 4 * g_ + 4)], writes=[("out", g_)])
        fw.barrier()
        return nc, fw, None
    if DBG_STAGE == 10:
        return early_exit()
    run_streams([stream_A(0) if 'A0' not in DBG_SKIP else None, stream_M1() if 'M1' not in DBG_SKIP else None])
    if DBG_STAGE == 11:
        return early_exit()
    m2 = stream_M2()
    NGRP = len(GROUPS)
    for g in range(NGRP):
        run_streams([stream_A(g + 1) if g < NGRP - 1 else None, stream_B(g) if 'B' not in DBG_SKIP else None, m2 if (g == 0 and 'M2' not in DBG_SKIP) else None])
        if DBG_STAGE == 12 + g:
            return early_exit()

    if stage == 1:
        fw.barrier()
        for g in range(4):
            fw.dma("sp", out_v[:, 4 * g:4 * g + 4, :], X1[:, 4 * g:4 * g + 4, :], reads=[("X1", t) for t in range(4 * g, 4 * g + 4)], writes=[("out", g)])
        fw.barrier()
        return nc, fw, None

    fw.barrier()
    reg.reset()
    h2T = reg.get([8, T], BF16)
    wb = [dict(w1=reg.get([EB, 8, 256], BF16), w3=reg.get([EB, 8, 256], BF16), w2=reg.get([EB, 2, 1024], BF16)) for _ in range(2)]
    xn2 = reg.get([2, 1024], F32)
    xT32 = reg.get([2, 1024], F32)
    wr_sb = reg.get([8, 20], F32)
    shrep = reg.get([128], F32)
    cb_sb = reg.get([20], F32)
    L = reg.get([NT, 20], F32)
    g2b = reg.get([1024], F32)
    comb_pad = reg.get([NT, 128], BF16)
    cT_sb = reg.get([T], BF16)
    Cb = reg.get([2, 512], BF16)
    s_sb = reg.get([2, 512], BF16)
    gc_sb = reg.get([2, 512], BF16)
    hid = reg.get([2, EB * 2, 512], BF16)
    sq2 = reg.get([1024], BF16)
    gmax = reg.get([NT], F32)
    goh = reg.get([NT, 4], F32)
    gex = reg.get([NT, 4], F32)
    pg = reg.get([NT], F32)
    tmp16 = reg.get([NT, 16], F32)
    esel = reg.get([NT, 4], F32)
    esel2 = reg.get([NT, 4], F32)
    m1 = reg.get([NT], F32)
    m2 = reg.get([NT], F32)
    oh1 = reg.get([NT, 4], F32)
    oh2 = reg.get([NT, 4], F32)
    rr = reg.get([NT], F32)
    wa = reg.get([NT], F32)
    wb2 = reg.get([NT], F32)
    wsel = reg.get([NT, 4], F32)
    print("phase2 region bytes", reg.off, "of", reg.n)

    def load_expert_batch(bi):
        buf = wb[bi % 2]
        for j in range(EB):
            e_ = bi * EB + j
            fw.dma("pool", buf["w1"][:, j, :, :], w1_d[e_].rearrange("(k p) f -> p k f", p=128), writes=[("w1", bi % 2, j)])
            fw.dma("pool", buf["w3"][:, j, :, :], w3_d[e_].rearrange("(k p) f -> p k f", p=128), writes=[("w3", bi % 2, j)])
            fw.dma("pool", buf["w2"][:, j, :, :], w2_d[e_].rearrange("(k p) d -> p k d", p=128), writes=[("w2", bi % 2, j)])

    fw.dma("sp", wr_sb, wr_d.rearrange("(k p) n -> p k n", p=128), writes=["wr"])
    load_expert_batch(0)
    load_expert_batch(1)

    for k in range(8):
        fw.op("dve", lambda e, k=k: e.tensor_scalar(out=shrep, in0=ident_f, scalar1=mod[:, 40 + k:41 + k], scalar2=None, op0=ALU.mult),
              reads=["ident_f", ("mod", 5)], writes=["shrep"])
        fw.op("pe", lambda e, k=k: e.matmul(out=PS[k // 4][:, (k % 4) * 128:(k % 4 + 1) * 128], lhsT=ones_f, rhs=shrep, start=True, stop=True),
              reads=["ones_f", "shrep"], writes=[bk(k // 4)])
    for hh in range(2):
        fw.op("act", lambda e, hh=hh: e.activation(out=g2b[:, hh * 512:(hh + 1) * 512], in_=PS[hh], func=AF.Copy), reads=[bk(hh)], writes=["g2b"])
    for k in range(8):
        fw.op("dve", lambda e, k=k: e.tensor_copy(out=shrep, in_=mod[:, 24 + k:25 + k].to_broadcast([128, 128])),
              reads=[("mod", 3)], writes=["shrep"])
        fw.op("pe", lambda e, k=k: e.matmul(out=PS[2][:, 0:20], lhsT=shrep, rhs=wr_sb[:, k, :], start=(k == 0), stop=(k == 7)),
              reads=["shrep", "wr"], writes=[bk(2)])
    fw.op("dve", lambda e: e.tensor_tensor(out=cb_sb, in0=PS[2][:, 0:20], in1=brg, op=ALU.add), reads=[bk(2), "brg"], writes=["cb"])
    for k in range(8):
        fw.op("dve", lambda e, k=k: e.tensor_scalar(out=wr_sb[:, k, :], in0=wr_sb[:, k, :], scalar1=gs2[:, k:k + 1], scalar2=None, op0=ALU.mult),
              reads=["wr", "gs2", bk(2)], writes=["wr"])
    fw.op("dve", lambda e: e.memset(comb_pad, 0.0), writes=["comb_pad"])
    fw.op("dve", lambda e: e.memset(cT_sb, 0.0), writes=["cT"])

    def stage_N(t):
        p_ = t % 2
        xs = p_
        b0_, b1_, rb = 2 * p_, 2 * p_ + 1, 4 + p_
        tb = (b0_, b1_)
        fw.op("act", lambda e: e.activation(out=sq2, in_=X1[:, t, :], func=AF.Square, accum_out=ssx[:, t:t + 1]),
              reads=[("X1", t)], writes=["sq2", ("ssx", t)])
        rstd_act(rsx[:, t:t + 1], ssx[:, t:t + 1], D, 0.0, [("ssx", t)], [("rsx", t)])
        fw.op("dve", lambda e: e.tensor_scalar(out=xn2[:, xs, :], in0=X1[:, t, :], scalar1=rsx[:, t:t + 1], scalar2=None, op0=ALU.mult),
              reads=[("X1", t), ("rsx", t)], writes=[("xn2", xs)])
        yield
        for k in range(8):
            fw.op("pe", lambda e, k=k: e.transpose(out=PS[tb[k // 4]][:, (k % 4) * 128:(k % 4 + 1) * 128], in_=xn2[:, xs, k * 128:(k + 1) * 128], identity=ident_f),
                  reads=[("xn2", xs), "ident_f"], writes=[bk(tb[k // 4])], sig=(k % 4 == 3))
        yield
        for hh in range(2):
            fw.op("dve", lambda e, hh=hh: e.tensor_copy(out=xT32[:, xs, hh * 512:(hh + 1) * 512], in_=PS[tb[hh]]), reads=[bk(tb[hh])], writes=[("xT32", xs)])
        yield
        for k in range(8):
            evac_affine(h2T[:, k, t * 128:(t + 1) * 128], PS[tb[k // 4]][:, (k % 4) * 128:(k % 4 + 1) * 128], gs2[:, k:k + 1], mod[:, 24 + k:25 + k],
                        [bk(tb[k // 4]), "gs2", ("mod", 3)], [("h2T", t // 4)])
        yield
        for k in range(8):
            fw.op("pe", lambda e, k=k: e.matmul(out=PS[rb][:, 0:20], lhsT=xT32[:, xs, k * 128:(k + 1) * 128], rhs=wr_sb[:, k, :], start=(k == 0), stop=(k == 7)),
                  reads=[("xT32", xs), "wr"], writes=[bk(rb)], sig=(k == 7))
        yield
        fw.op("dve", lambda e: e.tensor_tensor(out=L[:, t, :], in0=PS[rb][:, 0:20], in1=cb_sb, op=ALU.add), reads=[bk(rb), "cb"], writes=["L"])
        yield

    run_streams([chain(*[stage_N(t) for t in range(0, NT, 2)]), chain(*[stage_N(t) for t in range(1, NT, 2)])])

    def dv(fn, reads, writes):
        fw.op("dve", fn, reads=reads, writes=writes)
    gl = L[:, :, 0:4]
    el = L[:, :, 4:20].rearrange("p t (g i) -> p t g i", g=4)
    dv(lambda e: e.tensor_reduce(out=gmax, in_=gl, axis=AX.X, op=ALU.max), ["L"], ["gmax"])
    dv(lambda e: e.tensor_tensor(out=goh, in0=gl, in1=gmax.unsqueeze(2).to_broadcast([128, NT, 4]), op=ALU.is_equal), ["L", "gmax"], ["goh"])
    dv(lambda e: e.tensor_tensor(out=gex, in0=gl, in1=gmax.unsqueeze(2).to_broadcast([128, NT, 4]), op=ALU.subtract), ["L", "gmax"], ["gex"])
    fw.op("act", lambda e: e.activation(out=gex, in_=gex, func=AF.Exp), reads=["gex"], writes=["gex"])
    dv(lambda e: e.tensor_reduce(out=pg, in_=gex, axis=AX.X, op=ALU.add), ["gex"], ["pg"])
    dv(lambda e: e.reciprocal(out=pg, in_=pg), ["pg"], ["pg"])
    dv(lambda e: e.tensor_tensor(out=tmp16.rearrange("p t (g i) -> p t g i", g=4), in0=el, in1=goh.unsqueeze(3).to_broadcast([128, NT, 4, 4]), op=ALU.mult),
       ["L", "goh"], ["tmp16"])
    dv(lambda e: e.tensor_reduce(out=esel, in_=tmp16.rearrange("p t (g i) -> p t i g", g=4), axis=AX.X, op=ALU.add), ["tmp16"], ["esel"])
    dv(lambda e: e.tensor_reduce(out=m1, in_=esel, axis=AX.X, op=ALU.max), ["esel"], ["m1"])
    dv(lambda e: e.tensor_tensor(out=oh1, in0=esel, in1=m1.unsqueeze(2).to_broadcast([128, NT, 4]), op=ALU.is_equal), ["esel", "m1"], ["oh1"])
    dv(lambda e: e.scalar_tensor_tensor(out=esel2, in0=oh1, scalar=-1e30, in1=esel, op0=ALU.mult, op1=ALU.add), ["oh1", "esel"], ["esel2"])
    dv(lambda e: e.tensor_reduce(out=m2, in_=esel2, axis=AX.X, op=ALU.max), ["esel2"], ["m2"])
    dv(lambda e: e.tensor_tensor(out=oh2, in0=esel2, in1=m2.unsqueeze(2).to_broadcast([128, NT, 4]), op=ALU.is_equal), ["esel2", "m2"], ["oh2"])
    dv(lambda e: e.tensor_tensor(out=rr, in0=m2, in1=m1, op=ALU.subtract), ["m1", "m2"], ["rr"])
    fw.op("act", lambda e: e.activation(out=rr, in_=rr, func=AF.Exp), reads=["rr"], writes=["rr"])
    dv(lambda e: e.tensor_scalar_add(out=wa, in0=rr, scalar1=1.0), ["rr"], ["wa"])
    dv(lambda e: e.reciprocal(out=wa, in_=wa), ["wa"], ["wa"])
    dv(lambda e: e.tensor_tensor(out=wa, in0=wa, in1=pg, op=ALU.mult), ["wa", "pg"], ["wa"])
    dv(lambda e: e.tensor_tensor(out=wb2, in0=wa, in1=rr, op=ALU.mult), ["wa", "rr"], ["wb2"])
    dv(lambda e: e.tensor_tensor(out=wsel, in0=oh1, in1=wa.unsqueeze(2).to_broadcast([128, NT, 4]), op=ALU.mult), ["oh1", "wa"], ["wsel"])
    dv(lambda e: e.tensor_tensor(out=oh2, in0=oh2, in1=wb2.unsqueeze(2).to_broadcast([128, NT, 4]), op=ALU.mult), ["oh2", "wb2"], ["oh2"])
    dv(lambda e: e.tensor_tensor(out=wsel, in0=wsel, in1=oh2, op=ALU.add), ["wsel", "oh2"], ["wsel"])
    dv(lambda e: e.tensor_tensor(out=comb_pad[:, :, 0:16].rearrange("p t (g i) -> p t g i", g=4), in0=goh.unsqueeze(3).to_broadcast([128, NT, 4, 4]),
                                 in1=wsel.unsqueeze(2).to_broadcast([128, NT, 4, 4]), op=ALU.mult), ["goh", "wsel"], ["comb_pad"])
    for half in range(2):
        for i in range(8):
            t = half * 8 + i
            fw.op("pe", lambda e, t=t, i=i: e.transpose(out=PSB[3][:, i * 128:(i + 1) * 128], in_=comb_pad[:, t, :], identity=ident_b),
                  reads=["comb_pad", "ident_b"], writes=[bk(3)], sig=(i == 7))
        fw.op("act", lambda e, half=half: e.activation(out=cT_sb[0:16, half * 1024:(half + 1) * 1024], in_=PSB[3][0:16, :], func=AF.Copy), reads=[bk(3)], writes=["cT"])

    NB = NEXP // EB
    AG = (0, 1, 2, 3)
    CBK = 4
    YB = (5, 6, 7)
    yi = [0]
    ui = [0]
    for bi in range(NB):
        buf = wb[bi % 2]
        for j in range(EB):
            for fc in range(2):
                fw.op("dve", lambda e, j=j, fc=fc: e.tensor_tensor(out=buf["w2"][:, j, fc, :], in0=buf["w2"][:, j, fc, :], in1=g2b, op=ALU.mult),
                      reads=[("w2", bi % 2, j), "g2b"], writes=[("w2", bi % 2, j)])
        for tg in range(4):
            hs = tg % 2
            for j in range(EB):
                e_ = bi * EB + j
                fw.op("pe", lambda e, e_=e_, tg=tg: e.matmul(out=PS[CBK], lhsT=sel[:, e_, :], rhs=cT_sb[:, tg * 512:(tg + 1) * 512], start=True, stop=True),
                      reads=["sel", "cT"], writes=[bk(CBK)])
                cs = ui[0] % 2
                fw.op("act", lambda e, cs=cs: e.activation(out=Cb[:, cs, :], in_=PS[CBK], func=AF.Copy), reads=[bk(CBK)], writes=[("Cb", cs)])
                for fc in range(2):
                    u = ui[0] % 2
                    ab, gb = AG[2 * u], AG[2 * u + 1]
                    ui[0] += 1
                    for (bank, wname) in ((ab, "w1"), (gb, "w3")):
                        for k in range(8):
                            fw.op("pe", lambda e, bank=bank, wname=wname, j=j, fc=fc, k=k, tg=tg: e.matmul(
                                out=PS[bank], lhsT=buf[wname][:, j, k, fc * 128:(fc + 1) * 128], rhs=h2T[:, k, tg * 512:(tg + 1) * 512],
                                start=(k == 0), stop=(k == 7)),
                                reads=[(wname, bi % 2, j), ("h2T", tg)], writes=[bk(bank)], sig=(k == 7))
                    fw.op("act", lambda e, ab=ab, u=u: e.activation(out=s_sb[:, u, :], in_=PS[ab], func=AF.Silu), reads=[bk(ab)], writes=[("s", u)])
                    fw.op("dve", lambda e, gb=gb, u=u, cs=cs: e.tensor_tensor(out=gc_sb[:, u, :], in0=PS[gb], in1=Cb[:, cs, :], op=ALU.mult),
                          reads=[bk(gb), ("Cb", cs)], writes=[("gc", u)])
                    fw.op("dve", lambda e, u=u, j=j, fc=fc, hs=hs: e.tensor_tensor(out=hid[:, hs, j * 2 + fc, :], in0=s_sb[:, u, :], in1=gc_sb[:, u, :], op=ALU.mult),
                          reads=[("s", u), ("gc", u)], writes=[("hid", hs)])
                ui[0] += 0
            for tt in range(4):
                t = tg * 4 + tt
                for dh in range(2):
                    yb = YB[yi[0] % 3]
                    yi[0] += 1
                    n = EB * 2
                    for q in range(n):
                        j, fc = q // 2, q % 2
                        fw.op("pe", lambda e, yb=yb, q=q, j=j, fc=fc, tt=tt, dh=dh, hs=hs: e.matmul(
                            out=PS[yb], lhsT=hid[:, hs, q, tt * 128:(tt + 1) * 128], rhs=buf["w2"][:, j, fc, dh * 512:(dh + 1) * 512],
                            start=(q == 0), stop=(q == n - 1)),
                            reads=[("hid", hs), ("w2", bi % 2, j)], writes=[bk(yb)], sig=(q == n - 1))
                    fw.op("dve", lambda e, yb=yb, t=t, dh=dh: e.tensor_tensor(out=X1[:, t, dh * 512:(dh + 1) * 512], in0=PS[yb], in1=X1[:, t, dh * 512:(dh + 1) * 512], op=ALU.add),
                          reads=[bk(yb), ("X1", t)], writes=[("X1", t)])
            if bi == NB - 1:
                fw.dma("sp", out_v[:, 4 * tg:4 * tg + 4, :], X1[:, 4 * tg:4 * tg + 4, :], reads=[("X1", t) for t in range(4 * tg, 4 * tg + 4)], writes=[("out", tg)])
        if bi + 2 < NB:
            load_expert_batch(bi + 2)
    fw.barrier()
    print("ops", fw.nops, "sem counts", fw.cnt)
    return nc, fw, None


def host_inputs(inputs, b):
    f = np.float32
    g = lambda k: np.asarray(inputs[k], dtype=f)
    P = [0, 4, 1, 5, 2, 6, 3, 7]
    w_in = g("w_in")[0]
    qcols = np.concatenate([np.arange(h * 64, (h + 1) * 64) for h in P])
    w_in_tm = np.concatenate([w_in[:, qcols], w_in[:, 512:768], w_in[:, 1280:1792], w_in[:, 1808:2320]], axis=1)
    w_in_fm = np.concatenate([w_in[:, 768:1280], w_in[:, 1792:1920]], axis=1)
    col = lambda v: np.ascontiguousarray(v.reshape(-1, 128).T)
    vecs = np.concatenate([
        col(g("b_ada")[0]), col(g("g_norm1")[0]), col(g("g_norm2")[0]), col(g("g_att_out")[0]), col(g("g_gla_out")[0]),
        col(g("b_gk")[0]), col(np.tile(g("q_norm")[0], 2)), col(np.tile(g("k_norm")[0], 2)), col(g("c")[b]),
    ], axis=1)
    assert vecs.shape == (128, NV)
    sinks_b = np.ascontiguousarray(np.broadcast_to(g("sinks")[0][None, :], (128, 8)))
    brg = np.ascontiguousarray(np.broadcast_to(np.concatenate([g("b_group")[0], g("b_router")[0]])[None, :], (128, 20)))
    slopes = np.exp2(-8.0 * np.arange(1, 9) / 8).astype(f)
    kj = np.arange(128)[:, None]
    qi = np.arange(128)[None, :]
    abias = np.zeros((128, 4, 512), f)
    for kv in range(2):
        for gg in range(4):
            h = kv * 4 + gg
            dcur = (qi - kj).astype(f)
            abias[:, kv * 2 + 0, gg * 128:(gg + 1) * 128] = np.where(dcur >= 0, -slopes[h] * dcur, -30000.0)
            dprev = (qi - kj + 128).astype(f)
            abias[:, kv * 2 + 1, gg * 128:(gg + 1) * 128] = np.where(dprev < 128, -slopes[h] * dprev, -30000.0)
    gmask = (kj <= qi).astype(f)
    sel = np.zeros((128, 16, 128), f)
    for e in range(16):
        sel[e, e, :] = 1.0
    w_rt = np.concatenate([g("w_group")[0], g("w_router")[0]], axis=1)
    return {
        "x": np.ascontiguousarray(g("x")[b]), "vecs": vecs, "sinks_b": sinks_b, "brg": brg,
        "ident": np.eye(128, dtype=f), "abias": abias, "gmask": gmask, "sel": sel,
        "w_ada": g("w_ada")[0], "w_in_tm": np.ascontiguousarray(w_in_tm), "w_in_fm": np.ascontiguousarray(w_in_fm),
        "w_gk2": g("w_gk2")[0], "w_out": g("w_out")[0], "w_rt": np.ascontiguousarray(w_rt),
        "w1": g("w1")[0], "w3": g("w3")[0], "w2": g("w2")[0],
    }


def kernel(**inputs):
    nc, fw, _ = build_nc()
    in_maps = [host_inputs(inputs, b) for b in range(8)]
    res = run_bass_kernel_spmd(nc, in_maps, core_ids=list(range(8)))
    return np.stack([r["out"] for r in res.results], axis=0).astype(np.float32)
```

```python
import math
import numpy as np
import concourse.bass as bass
import concourse.mybir as mybir
from concourse.bass_utils import run_bass_kernel_spmd

F32 = mybir.dt.float32
BF16 = mybir.dt.bfloat16
AF = mybir.ActivationFunctionType
ALU = mybir.AluOpType
AX = mybir.AxisListType

T = 2048
D = 1024
NT = 16
EPS = 1e-6
LN8 = math.log(8.0)
NV = 81
NEXP = 16
EB = 2
SAME_ENGINE_FULL_SYNC = True
SEQ_STREAMS = False
GREEDY = True
PE_SLOW = 1.0
PIPE_A = True
GROUPS = ((0, 2), (2, 2), (4, 4), (8, 4), (12, 2), (14, 2))
DBG_SKIP = ()
DBG_STAGE = 99
MODCOL = 320
DBG_NT = 4
DBG_SB = (3, 4)
DBG_OB = (5, 4)


class FW:
    def __init__(self, nc):
        self.nc = nc
        self.eng = {"pe": nc.tensor, "act": nc.scalar, "dve": nc.vector, "pool": nc.gpsimd, "sp": nc.sync}
        self.sem = {e: nc.alloc_semaphore("s_" + e) for e in self.eng}
        self.cnt = {e: 0 for e in self.eng}
        self.seen = {e: {} for e in self.eng}
        self.dsem = {q: [nc.alloc_semaphore("d_%s%d" % (q, i)) for i in range(14)] for q in ("sp", "pool")}
        self.dval = {q: [0] * 14 for q in ("sp", "pool")}
        self.drr = {"sp": 0, "pool": 0}
        self.lastw = {}
        self.readers = {}
        self.pe_pending = []
        self.nops = 0
        self.log = None
        self.tags = {}
        self.eng_free = {e: 0.0 for e in self.eng}
        self.tok_end = {}
        self.cur_stream_end = 0.0

    def _deps(self, eng, reads, writes, is_dma):
        toks = []
        for k in reads:
            w = self.lastw.get(k)
            if w is not None:
                toks.append((w, "raw"))
            if isinstance(k, tuple) and k[0] == "ps":
                for r in self.readers.get(k, ()):
                    toks.append((r, "rar"))
        for k in writes:
            w = self.lastw.get(k)
            if w is not None:
                toks.append((w, "waw"))
            for r in self.readers.get(k, ()):
                toks.append((r, "war"))
        need = {}
        for tok, kind in toks:
            if tok[0] == "c":
                src = tok[1]
                if src == eng and not is_dma:
                    if eng == "pe":
                        continue
                    if kind != "raw" and not SAME_ENGINE_FULL_SYNC:
                        continue
                assert tok[2] is not None, "unresolved PE token"
                key = ("c", src)
                need[key] = max(need.get(key, 0), tok[2])
            else:
                key = ("d", tok[1], tok[2])
                need[key] = max(need.get(key, 0), tok[3])
        waits = []
        for key, val in need.items():
            if self.seen[eng].get(key, 0) >= val:
                continue
            self.seen[eng][key] = val
            if key[0] == "c":
                waits.append((self.sem[key[1]], val))
            else:
                waits.append((self.dsem[key[1]][key[2]], val))
        return waits

    def _record(self, tok, reads, writes):
        for k in reads:
            self.readers.setdefault(k, []).append(tok)
        for k in writes:
            self.lastw[k] = tok
            self.readers[k] = []

    def _tagcheck(self, reads, writes, rtag, wtag, keep_tag):
        for k in reads:
            if rtag is not None and k in rtag:
                assert self.tags.get(k) == rtag[k], ("stale read", k, self.tags.get(k), rtag[k])
        if not keep_tag:
            for k in writes:
                self.tags[k] = (wtag or {}).get(k)

    def op(self, eng, fn, reads=(), writes=(), sig=True, rtag=None, wtag=None, keep_tag=False):
        self._tagcheck(reads, writes, rtag, wtag, keep_tag)
        waits = self._deps(eng, reads, writes, False)
        e = self.eng[eng]
        for s, v in waits[1:]:
            e.wait_ge(s, v)
        rec = {}
        ins = fn(_EngProxy(e, rec))
        self._model(eng, rec, reads, writes)
        if waits:
            ins._wait_ge(waits[0][0], waits[0][1])
        tok = ["c", eng, None]
        if eng == "pe" and not sig:
            self.pe_pending.append(tok)
        else:
            self.cnt[eng] += 1
            ins.then_inc(self.sem[eng], 1)
            tok[2] = self.cnt[eng]
            if eng == "pe":
                for p in self.pe_pending:
                    p[2] = tok[2]
                self.pe_pending = []
        self._record(tok, reads, writes)
        self.nops += 1
        if self.log is not None:
            import sys as _s
            fr = _s._getframe(1)
            nm = None
            ln = fr.f_lineno
            while fr is not None:
                if fr.f_code.co_name.startswith(("stage_", "stream_", "mod_chunk")):
                    nm = fr.f_code.co_name
                    break
                fr = fr.f_back
            self.log.append((eng, tok[2], (nm, ln), list(reads), list(writes), [(str(s), v) for s, v in waits]))
        return ins

    def dma(self, q, out, in_, reads=(), writes=()):
        waits = self._deps(q, reads, writes, True)
        i = self.drr[q]
        self.drr[q] = (i + 1) % len(self.dsem[q])
        prev = self.dval[q][i]
        key = ("d", q, i)
        if prev and self.seen[q].get(key, 0) < prev:
            self.seen[q][key] = prev
            waits.append((self.dsem[q][i], prev))
        e = self.eng[q]
        for s, v in waits:
            e.wait_ge(s, v)
        e.dma_start(out=out, in_=in_).then_inc(self.dsem[q][i], 16)
        self.dval[q][i] = prev + 16
        tok = ["d", q, i, prev + 16]
        self._record(tok, reads, writes)
        try:
            nb = 1
            for d_ in in_.shape:
                nb *= d_
            nb *= 4
        except Exception:
            nb = 1 << 20
        st = max(self.eng_free[q], max([self.tok_end.get(("w", k), 0.0) for k in list(reads) + list(writes)] + [self.tok_end.get(("r", k), 0.0) for k in writes] + [0.0]))
        en = st + 2.0 + nb / 180e3
        self.eng_free[q] = st + nb / 180e3
        for k in writes:
            self.tok_end[("w", k)] = en
            self.tok_end[("r", k)] = 0.0
        self.nops += 1

    def _model(self, eng, rec, reads, writes):
        kw = rec.get("kw", {})
        name = rec.get("name", "")

        def fsz(ap):
            try:
                sh = ap.shape
                n = 1
                for d in sh[1:]:
                    n *= d
                return n
            except Exception:
                return 128
        if eng == "pe":
            if name == "transpose":
                n = 128
                dur = 0.07 if kw["in_"].dtype == BF16 else 0.3
            else:
                n = fsz(kw["rhs"])
                dur = (max(n, 64) / 2400.0 + 0.02) * PE_SLOW
                if kw["rhs"].dtype == F32:
                    dur *= 4
        elif eng == "act":
            n = fsz(kw.get("out"))
            dur = 0.22 + n / 1200.0 + (0.1 if kw.get("accum_out") is not None else 0.0)
        else:
            n = fsz(kw.get("out")) if kw.get("out") is not None else 128
            dur = 0.12 + n / 960.0
            if name == "tensor_tensor_scan":
                dur = 0.12 + 2 * n / 960.0
            if name == "reciprocal":
                dur = 0.12 + 6 * n / 960.0
        ready = 0.0
        for k in reads:
            ready = max(ready, self.tok_end.get(("w", k), 0.0))
        for k in writes:
            ready = max(ready, self.tok_end.get(("w", k), 0.0), self.tok_end.get(("r", k), 0.0))
        start = max(self.eng_free[eng], ready + 0.15)
        end = start + dur
        self.eng_free[eng] = end
        for k in reads:
            self.tok_end[("r", k)] = max(self.tok_end.get(("r", k), 0.0), end)
        for k in writes:
            self.tok_end[("w", k)] = end
            self.tok_end[("r", k)] = 0.0
        self.cur_stream_end = max(self.cur_stream_end, end)

    def barrier(self):
        for e in self.eng:
            for src in self.eng:
                if src == e:
                    continue
                v = self.cnt[src]
                if v and self.seen[e].get(("c", src), 0) < v:
                    self.seen[e][("c", src)] = v
                    self.eng[e].wait_ge(self.sem[src], v)
            for q in ("sp", "pool"):
                for i, v in enumerate(self.dval[q]):
                    if v and self.seen[e].get(("d", q, i), 0) < v:
                        self.seen[e][("d", q, i)] = v
                        self.eng[e].wait_ge(self.dsem[q][i], v)


class _EngProxy:
    def __init__(self, real, rec):
        self._real = real
        self._rec = rec

    def __getattr__(self, name):
        f = getattr(self._real, name)
        rec = self._rec

        def w(*a, **k):
            rec["name"] = name
            rec["kw"] = k
            return f(*a, **k)
        return w


class Carver:
    def __init__(self, ap, nbytes):
        self.ap = ap
        self.n = nbytes
        self.off = 0

    def get(self, free_shape, dt):
        esz = 4 if dt == F32 else 2
        n = int(np.prod(free_shape)) * esz
        n_al = (n + 63) // 64 * 64
        assert self.off + n_al <= self.n, "carver overflow %d + %d > %d" % (self.off, n_al, self.n)
        v = self.ap[:, self.off // 2:(self.off + n) // 2]
        self.off += n_al
        if dt == F32:
            v = v.bitcast(F32)
        if len(free_shape) == 2:
            v = v.rearrange("p (a b) -> p a b", a=free_shape[0])
        elif len(free_shape) == 3:
            v = v.rearrange("p (a b c) -> p a b c", a=free_shape[0], b=free_shape[1])
        return v

    def reset(self):
        self.off = 0


def build_nc(stage=99, dbg=False):
    nc = bass.Bass("TRN2", target_bir_lowering=False)
    fw = FW(nc)

    def din(name, shape, dt=F32):
        return nc.dram_tensor(name, list(shape), dt, kind="ExternalInput").ap()

    x_d = din("x", [T, D])
    vecs_d = din("vecs", [128, NV])
    sinks_d = din("sinks_b", [128, 8])
    brg_d = din("brg", [128, 20])
    ident_d = din("ident", [128, 128])
    abias_d = din("abias", [128, 4, 512])
    gmask_d = din("gmask", [128, 128])
    sel_d = din("sel", [128, 16, 128])
    wada_d = din("w_ada", [D, 6 * D])
    wintm_d = din("w_in_tm", [D, 1792])
    winfm_d = din("w_in_fm", [D, 640])
    wgk2_d = din("w_gk2", [16, 256])
    wout_d = din("w_out", [D, D])
    wr_d = din("w_rt", [D, 20])
    w1_d = din("w1", [NEXP, D, 256])
    w3_d = din("w3", [NEXP, D, 256])
    w2_d = din("w2", [NEXP, 256, D])
    out_d = nc.dram_tensor("out", [T, D], F32, kind="ExternalOutput").ap()
    dbg_d = nc.dram_tensor("dbg", [128, 4096], F32, kind="ExternalOutput").ap() if dbg else None

    x_v = x_d.rearrange("(t p) d -> p t d", p=128)
    out_v = out_d.rearrange("(t p) d -> p t d", p=128)

    X1 = nc.alloc_sbuf_tensor("X1", [128, NT, D], F32).ap()
    PERS_BYTES = 11776
    pers = Carver(nc.alloc_sbuf_tensor("pers", [128, PERS_BYTES // 2], BF16).ap(), PERS_BYTES)
    REG_BYTES = 135168
    reg = Carver(nc.alloc_sbuf_tensor("reg", [128, REG_BYTES // 2], BF16).ap(), REG_BYTES)

    ident_f = pers.get([128], F32)
    ident_b = pers.get([128], BF16)
    ones_f = pers.get([128], F32)
    ones_b = pers.get([128], BF16)
    abias = pers.get([4, 512], BF16)
    gmask = pers.get([128], BF16)
    sel = pers.get([16, 128], BF16)
    vecs = pers.get([NV], F32)
    es = pers.get([8], F32)
    brg = pers.get([20], F32)
    mod = pers.get([48], F32)
    gs1 = pers.get([8], F32)
    gs2 = pers.get([8], F32)
    nbgk = pers.get([2], F32)
    ssx = pers.get([NT], F32)
    rsx = pers.get([NT], F32)
    sc_b = pers.get([8], BF16)
    tmp8 = pers.get([8], F32)
    dbg_sb = pers.get([16], F32)

    PS = [nc.alloc_psum_tensor("ps%d" % i, [128, 512], F32).ap() for i in range(8)]
    PSB = [p.bitcast(BF16) for p in PS]

    def bk(i):
        return ("ps", i)

    fw.dma("sp", ident_f, ident_d, writes=["ident_f"])
    fw.dma("sp", vecs, vecs_d, writes=["vecs"])
    fw.dma("sp", es, sinks_d, writes=["es"])
    fw.dma("sp", brg, brg_d, writes=["brg"])
    fw.dma("pool", ident_b, ident_d, writes=["ident_b"])
    fw.dma("pool", abias, abias_d, writes=["abias"])
    fw.dma("pool", gmask, gmask_d, writes=["gmask"])
    fw.dma("pool", sel, sel_d, writes=["sel"])
    fw.dma("sp", X1[:, 0:4, :], x_v[:, 0:4, :], writes=[("X1", t) for t in range(4)])
    fw.op("dve", lambda e: e.memset(ones_f, 1.0), writes=["ones_f"])
    fw.op("dve", lambda e: e.memset(ones_b, 1.0), writes=["ones_b"])

    HR = 8
    hT = reg.get([8, HR * 128], BF16)
    win_tm = reg.get([8, 1792], BF16)
    win_fm = reg.get([8, 640], BF16)
    wout_sb = reg.get([8, 1024], BF16)
    wada_buf = [reg.get([8, 128], BF16) for _ in range(2)]
    xn = reg.get([1, 1024], BF16)
    sqq = reg.get([512], F32)
    qn = reg.get([512], BF16)
    kn = reg.get([128], BF16)
    ssq = reg.get([8], F32)
    rq = reg.get([8], F32)
    ssk = reg.get([2], F32)
    rk = reg.get([2], F32)
    qTA = reg.get([2, 512], BF16)
    qTB = reg.get([2, 512], BF16)
    kT = reg.get([4 * 128], BF16)
    vaug = reg.get([4, 2, 65], BF16)
    vg = reg.get([2, 512], BF16)
    sog = reg.get([2, 512], BF16)
    PT = reg.get([4, 512], BF16)
    den = reg.get([8], F32)
    on = reg.get([512], F32)
    ssa = reg.get([1], F32)
    ra = reg.get([1], F32)
    ya = reg.get([512], BF16)
    yA = reg.get([8, 4, 128], BF16)
    yG = reg.get([4, 128], BF16)
    OFF_LBUF = reg.off
    lbuf = reg.get([2, 512], F32)
    Bc = reg.get([2, 512], F32)
    elast = reg.get([2, 4], F32)
    OFF_QDPAD = reg.off
    qd_pad = reg.get([4, 512], BF16)
    kdT = reg.get([2, 512], BF16)
    kdecT = reg.get([2, 512], BF16)
    kdec = reg.get([4, 256], BF16)
    AT = reg.get([4, 128], BF16)
    S = reg.get([2, 128], F32)
    Sbf = reg.get([2, 128], BF16)
    ssg = reg.get([4], F32)
    rg = reg.get([4], F32)
    t1 = reg.get([512], F32)
    sq_b = sqq.bitcast(BF16)
    esc = t1
    sqB = reg.get([512], BF16)
    yg = reg.get([512], BF16)
    lrT_pad = reg.get([512], BF16)
    wgk2_pad = reg.get([256], BF16)
    print("phase1 region bytes", reg.off, "of", reg.n)

    wada_v = wada_d.rearrange("(k p) n -> p k n", p=128)

    ccol = vecs[:, 73:81]
    fw.op("act", lambda e: e.activation(out=tmp8, in_=ccol, func=AF.Exp, scale=-1.0), reads=["vecs"], writes=["tmp8"])
    fw.op("dve", lambda e: e.tensor_scalar_add(out=tmp8, in0=tmp8, scalar1=1.0), reads=["tmp8"], writes=["tmp8"])
    fw.op("dve", lambda e: e.reciprocal(out=tmp8, in_=tmp8), reads=["tmp8"], writes=["tmp8"])
    fw.op("dve", lambda e: e.tensor_tensor(out=sc_b, in0=tmp8, in1=ccol, op=ALU.mult), reads=["tmp8", "vecs"], writes=["sc_b"])
    MODB = 5
    wctr = [0]

    def mod_chunk(c):
        bi = wctr[0] % 2
        wctr[0] += 1
        buf = wada_buf[bi]
        if 'chunkdma' in DBG_SKIP and c >= 16:
            return
        fw.dma("pool", buf, wada_v[:, :, c * 128:(c + 1) * 128], writes=[("wada", bi)])
        for k in range(8):
            fw.op("pe", lambda e, k=k: e.matmul(out=PS[MODB][:, MODCOL:MODCOL + 1], lhsT=buf[:, k, :], rhs=sc_b[:, k:k + 1], start=(k == 0), stop=(k == 7)),
                  reads=[("wada", bi), "sc_b"], writes=[bk(MODB)], sig=(k == 7), keep_tag=True)
        fw.op("dve", lambda e: e.tensor_tensor(out=mod[:, c:c + 1], in0=PS[MODB][:, MODCOL:MODCOL + 1], in1=vecs[:, c:c + 1], op=ALU.add),
              reads=[bk(MODB), "vecs"], writes=[("mod", c // 8)])

    MK = [("mod", i) for i in range(6)]
    big = [reg.ap[:, boff // 2:(boff + 8192) // 2].rearrange("p (k n) -> p k n", k=8) for boff in (OFF_LBUF, OFF_QDPAD)]
    bigkeys = [[("lbuf", 0), ("lbuf", 1), ("Bc", 0), ("Bc", 1)], ["qd_pad", ("kdT", 0), ("kdT", 1), ("kdecT", 0), ("kdecT", 1)]]
    for cc in range(4):
        fw.dma("pool", big[cc % 2], wada_v[:, :, cc * 512:(cc + 1) * 512], writes=bigkeys[cc % 2])
        if cc == 0:
            for g_ in range(1, 4):
                pass
        for j in range(4):
            c = cc * 4 + j
            for k in range(8):
                fw.op("pe", lambda e, k=k, j=j, cc=cc: e.matmul(out=PS[MODB][:, MODCOL:MODCOL + 1], lhsT=big[cc % 2][:, k, j * 128:(j + 1) * 128], rhs=sc_b[:, k:k + 1], start=(k == 0), stop=(k == 7)),
                      reads=bigkeys[cc % 2] + ["sc_b"], writes=[bk(MODB)], sig=(k == 7), keep_tag=True)
            fw.op("dve", lambda e, c=c: e.tensor_tensor(out=mod[:, c:c + 1], in0=PS[MODB][:, MODCOL:MODCOL + 1], in1=vecs[:, c:c + 1], op=ALU.add),
                  reads=[bk(MODB), "vecs"], writes=[("mod", c // 8)])
    fw.op("dve", lambda e: e.scalar_tensor_tensor(out=gs1, in0=mod[:, 8:16], scalar=1.0, in1=vecs[:, 48:56], op0=ALU.add, op1=ALU.mult),
          reads=[("mod", 1), "vecs"], writes=["gs1"])
    fw.op("dve", lambda e: e.tensor_scalar(out=nbgk, in0=vecs[:, 69:71], scalar1=-1.0, scalar2=None, op0=ALU.mult),
          reads=["vecs"], writes=["nbgk"])
    fw.op("act", lambda e: e.activation(out=es, in_=es, func=AF.Exp), reads=["es"], writes=["es"])
    wintm_v = wintm_d.rearrange("(k p) n -> p k n", p=128)
    fw.dma("pool", win_tm[:, :, 0:768], wintm_v[:, :, 0:768], writes=["win_tmA"])
    fw.dma("pool", win_tm[:, :, 768:1792], wintm_v[:, :, 768:1792], writes=["win_tmB"])
    fw.dma("pool", win_fm, winfm_d.rearrange("(k p) n -> p k n", p=128), writes=["win_fm"])
    for g in range(1, 4):
        fw.dma("sp", X1[:, 4 * g:4 * g + 4, :], x_v[:, 4 * g:4 * g + 4, :], reads=["win_tmA"], writes=[("X1", t) for t in range(4 * g, 4 * g + 4)])
    fw.op("dve", lambda e: e.memset(wgk2_pad, 0.0), writes=["wgk2"])
    fw.dma("pool", wgk2_pad[0:16, :], wgk2_d, reads=[], writes=["wgk2"])

    GB = (6, 7)

    def stream_M1():
        for c in range(16, 24):
            mod_chunk(c)
            yield
        fw.dma("pool", wout_sb, wout_d.rearrange("(k p) n -> p k n", p=128), writes=["wout"])
        yield
        for hh in range(2):
            for kk in range(4):
                k = hh * 4 + kk
                fw.op("dve", lambda e, k=k: e.tensor_scalar(out=yg[:, 0:256].bitcast(F32), in0=ident_f, scalar1=mod[:, 16 + k:17 + k], scalar2=None, op0=ALU.mult),
                      reads=["ident_f", ("mod", 2)], writes=["yg"])
                fw.op("pe", lambda e, k=k, hh=hh, kk=kk: e.matmul(out=PS[GB[hh]][:, kk * 128:(kk + 1) * 128], lhsT=ones_f, rhs=yg[:, 0:256].bitcast(F32), start=True, stop=True),
                      reads=["ones_f", "yg"], writes=[bk(GB[hh])])
            for k in range(8):
                fw.op("dve", lambda e, k=k, hh=hh: e.tensor_tensor(out=wout_sb[:, k, hh * 512:(hh + 1) * 512], in0=wout_sb[:, k, hh * 512:(hh + 1) * 512],
                                                               in1=PS[GB[hh]], op=ALU.mult),
                      reads=["wout", bk(GB[hh])], writes=["wout"])
            yield

    def stream_M2():
        for c in range(24, 48):
            mod_chunk(c)
            yield
        fw.op("dve", lambda e: e.scalar_tensor_tensor(out=gs2, in0=mod[:, 32:40], scalar=1.0, in1=vecs[:, 56:64], op0=ALU.add, op1=ALU.mult),
              reads=[("mod", 4), "vecs"], writes=["gs2"])
        yield

    fw.op("dve", lambda e: e.memset(qTA, 0.0), writes=["qTA0", "qTA1"])
    fw.op("dve", lambda e: e.memset(qTB, 0.0), writes=["qTB0", "qTB1"])
    fw.op("dve", lambda e: e.memset(vaug, 1.0), writes=[("vaug", t) for t in range(4)])
    fw.op("dve", lambda e: e.memset(qd_pad, 0.0), writes=["qd_pad"])
    fw.op("dve", lambda e: e.memset(lrT_pad, 0.0), writes=["lrT"])
    fw.op("dve", lambda e: e.memset(S, 0.0), writes=["S"])
    fw.op("dve", lambda e: e.memset(Sbf, 0.0), writes=["Sbf"])

    alt = [0]

    def evac_affine(out, in_, scale, bias, reads, writes, wtag=None, rtag=None):
        alt[0] ^= 1
        if alt[0]:
            if bias is None:
                fw.op("act", lambda e: e.activation(out=out, in_=in_, func=AF.Copy, scale=scale), reads=reads, writes=writes, wtag=wtag, rtag=rtag)
            else:
                fw.op("act", lambda e: e.activation(out=out, in_=in_, func=AF.Identity, scale=scale, bias=bias), reads=reads, writes=writes, wtag=wtag, rtag=rtag)
        else:
            if bias is None:
                fw.op("dve", lambda e: e.tensor_scalar(out=out, in0=in_, scalar1=scale, scalar2=None, op0=ALU.mult), reads=reads, writes=writes, wtag=wtag, rtag=rtag)
            else:
                fw.op("dve", lambda e: e.tensor_scalar(out=out, in0=in_, scalar1=scale, scalar2=bias, op0=ALU.mult, op1=ALU.add), reads=reads, writes=writes, wtag=wtag, rtag=rtag)

    def rstd_act(out, in_, n, extra_bias, rk_, wk_):
        fw.op("act", lambda e: e.activation(out=out, in_=in_, func=AF.Ln, scale=1.0 / n, bias=EPS), reads=rk_, writes=wk_)
        fw.op("act", lambda e: e.activation(out=out, in_=out, func=AF.Exp, scale=-0.5, bias=extra_bias), reads=wk_, writes=wk_)

    TB = 0

    def stage_T1(t):
        hs = t % HR
        xs = 0
        fw.op("act", lambda e: e.activation(out=sq_b, in_=X1[:, t, :], func=AF.Square, accum_out=ssx[:, t:t + 1]),
              reads=[("X1", t)], writes=["sqq", ("ssx", t)])
        rstd_act(rsx[:, t:t + 1], ssx[:, t:t + 1], D, 0.0, [("ssx", t)], [("rsx", t)])
        fw.op("dve", lambda e: e.tensor_scalar(out=xn[:, xs, :], in0=X1[:, t, :], scalar1=rsx[:, t:t + 1], scalar2=None, op0=ALU.mult),
              reads=[("X1", t), ("rsx", t)], writes=[("xn", xs)])
        yield
        T1B = (TB, 3)
        for k in range(8):
            fw.op("pe", lambda e, k=k: e.transpose(out=PSB[T1B[k // 4]][:, (k % 4) * 128:(k % 4 + 1) * 128], in_=xn[:, xs, k * 128:(k + 1) * 128], identity=ident_b),
                  reads=[("xn", xs), "ident_b"], writes=[bk(T1B[k // 4])], sig=(k % 4 == 3), wtag={bk(T1B[k // 4]): ("T1", t)})
        for k in range(4):
            fw.op("act", lambda e, k=k: e.activation(out=hT[:, k, hs * 128:(hs + 1) * 128], in_=PSB[TB][:, k * 128:(k + 1) * 128], func=AF.Identity,
                                                     scale=gs1[:, k:k + 1], bias=mod[:, k:k + 1]),
                  reads=[bk(TB), "gs1", ("mod", 0)], writes=[("hT", hs)], wtag={("hT", hs): t}, rtag={bk(TB): ("T1", t)})
        hv = hT[:, 4:8, hs * 128:(hs + 1) * 128]
        fw.op("dve", lambda e: e.tensor_tensor(out=hv, in0=PSB[3][:, 0:512].rearrange("p (k n) -> p k n", k=4),
                                               in1=gs1[:, 4:8].unsqueeze(2).to_broadcast([128, 4, 128]), op=ALU.mult),
              reads=[bk(3), "gs1"], writes=[("hT", hs)], wtag={("hT", hs): t}, rtag={bk(3): ("T1", t)})
        fw.op("dve", lambda e: e.tensor_tensor(out=hv, in0=hv, in1=mod[:, 4:8].unsqueeze(2).to_broadcast([128, 4, 128]), op=ALU.add),
              reads=[("hT", hs), ("mod", 0)], writes=[("hT", hs)], wtag={("hT", hs): t})
        yield

    QB, KVB, VGB, OGB = 1, 2, 6, 7

    def stage_T2(t):
        hs = t % HR
        for bank, c0, n in ((QB, 0, 512), (KVB, 512, 256)):
            for k in range(8):
                fw.op("pe", lambda e, bank=bank, c0=c0, n=n, k=k: e.matmul(
                    out=PS[bank][:, 0:n], lhsT=hT[:, k, hs * 128:(hs + 1) * 128], rhs=win_tm[:, k, c0:c0 + n],
                    start=(k == 0), stop=(k == 7)),
                    reads=[("hT", hs), "win_tmA"], writes=[bk(bank)], sig=(k == 7), rtag={("hT", hs): t}, wtag={bk(bank): ("proj", t)})
            yield

    def stage_T3(t):
        qs = t % 2
        vs = t % 4
        fw.op("act", lambda e: e.activation(out=sqq, in_=PS[QB], func=AF.Square), reads=[bk(QB)], writes=["sqq"], rtag={bk(QB): ("proj", t)})
        fw.op("dve", lambda e: e.tensor_reduce(out=ssq, in_=sqq.rearrange("p (h d) -> p h d", h=8), axis=AX.X, op=ALU.add),
              reads=["sqq"], writes=["ssq"])
        rstd_act(rq, ssq, 64, -LN8, ["ssq"], ["rq"])
        fw.op("dve", lambda e: e.tensor_tensor(out=qn.rearrange("p (h d) -> p h d", h=8), in0=PS[QB].rearrange("p (h d) -> p h d", h=8),
                                               in1=rq.unsqueeze(2).to_broadcast([128, 8, 64]), op=ALU.mult),
              reads=[bk(QB), "rq"], writes=["qn"], rtag={bk(QB): ("proj", t)})
        yield
        fw.op("act", lambda e: e.activation(out=sqq[:, 0:128], in_=PS[KVB][:, 0:128], func=AF.Square), reads=[bk(KVB)], writes=["sqq"])
        fw.op("dve", lambda e: e.tensor_reduce(out=ssk, in_=sqq[:, 0:128].rearrange("p (h d) -> p h d", h=2), axis=AX.X, op=ALU.add),
              reads=["sqq"], writes=["ssk"])
        rstd_act(rk, ssk, 64, 0.0, ["ssk"], ["rk"])
        fw.op("dve", lambda e: e.tensor_tensor(out=kn.rearrange("p (h d) -> p h d", h=2), in0=PS[KVB][:, 0:128].rearrange("p (h d) -> p h d", h=2),
                                               in1=rk.unsqueeze(2).to_broadcast([128, 2, 64]), op=ALU.mult),
              reads=[bk(KVB), "rk"], writes=["kn"], rtag={bk(KVB): ("proj", t)})
        yield
        if 'T3v' in DBG_SKIP:
            return
        fw.op("act", lambda e: e.activation(out=vaug[:, t % 4, :, 0:64], in_=PS[KVB][:, 128:256].rearrange("p (h d) -> p h d", h=2), func=AF.Copy),
              reads=[bk(KVB)], writes=[("vaug", t % 4)])
        if 'T3t' in DBG_SKIP:
            return
        for i in range(4):
            fw.op("pe", lambda e, i=i: e.transpose(out=PSB[TB][:, i * 128:(i + 1) * 128], in_=qn[:, i * 128:(i + 1) * 128], identity=ident_b),
                  reads=["qn", "ident_b"], writes=[bk(TB)], sig=False)
        fw.op("pe", lambda e: e.transpose(out=PSB[TB][:, 512:640], in_=kn, identity=ident_b),
              reads=["kn", "ident_b"], writes=[bk(TB)], sig=True, wtag={bk(TB): ("T3", t)})
        if 'T3e' in DBG_SKIP:
            return
        gq = vecs[:, 71:72]
        gk = vecs[:, 72:73]
        fw.op("act", lambda e: e.activation(out=qTA[0:64, qs, :], in_=PSB[TB][0:64, 0:512], func=AF.Copy, scale=gq[0:64, :]),
              reads=[bk(TB), "vecs"], writes=["qTA%d" % qs])
        fw.op("dve", lambda e: e.tensor_scalar(out=qTB[64:128, qs, :], in0=PSB[TB][64:128, 0:512], scalar1=gq[64:128, :], scalar2=None, op0=ALU.mult),
              reads=[bk(TB), "vecs"], writes=["qTB%d" % qs])
        fw.op("dve", lambda e: e.tensor_scalar(out=kT[:, (t % 4) * 128:(t % 4 + 1) * 128], in0=PSB[TB][:, 512:640], scalar1=gk, scalar2=None, op0=ALU.mult),
              reads=[bk(TB), "vecs"], writes=[("kT", t % 4)], rtag={bk(TB): ("T3", t)})
        yield

    SB = DBG_SB
    OB = DBG_OB

    def stage_T4(t):
        qs = t % 2
        ys = t % 8
        blocks = [(t, 0)] + ([(t - 1, 1)] if t > 0 else [])
        si = 0
        allpts = []
        for kv in range(2):
            qT = qTA if kv == 0 else qTB
            qkey = ("qTA%d" if kv == 0 else "qTB%d") % qs
            pts = []
            for (blk, which) in blocks:
                bank = SB[si % 2]
                pslot = si % 4
                si += 1
                fw.op("pe", lambda e, bank=bank, blk=blk, qT=qT: e.matmul(out=PS[bank], lhsT=kT[:, (blk % 4) * 128:(blk % 4 + 1) * 128], rhs=qT[:, qs, :], start=True, stop=False),
                      reads=[("kT", blk % 4), qkey], writes=[bk(bank)], sig=False)
                fw.op("pe", lambda e, bank=bank, which=which, kv=kv: e.matmul(out=PS[bank], lhsT=ident_b, rhs=abias[:, kv * 2 + which, :], start=False, stop=True),
                      reads=["ident_b", "abias"], writes=[bk(bank)], sig=True)
                fw.op("act", lambda e, bank=bank, pslot=pslot: e.activation(out=PT[:, pslot, :], in_=PS[bank], func=AF.Exp),
                      reads=[bk(bank)], writes=[("PT", pslot)])
                pts.append((pslot, blk))
                yield
            allpts.append(pts)
        for kv in range(2):
            pts = allpts[kv]
            for g in range(4):
                h = kv * 4 + g
                ob = OB[kv]
                for pi, (pslot, blk) in enumerate(pts):
                    fw.op("pe", lambda e, ob=ob, g=g, pslot=pslot, blk=blk, pi=pi, kv=kv, pts=pts: e.matmul(
                        out=PS[ob][:, g * 65:(g + 1) * 65], lhsT=PT[:, pslot, g * 128:(g + 1) * 128], rhs=vaug[:, blk % 4, kv, :],
                        start=(pi == 0), stop=(pi == len(pts) - 1)),
                        reads=[("PT", pslot), ("vaug", blk % 4)], writes=[bk(ob)], sig=(g == 3 and pi == len(pts) - 1))
            yield
        for kv in range(2):
            ov = PS[OB[kv]][:, 0:260].rearrange("p (h d) -> p h d", h=4)
            fw.op("dve", lambda e, ov=ov, kv=kv: e.tensor_tensor(out=den[:, kv * 4:(kv + 1) * 4], in0=ov[:, :, 64], in1=es[:, kv * 4:(kv + 1) * 4], op=ALU.add),
                  reads=[bk(OB[kv]), "es"], writes=["den"])
        fw.op("dve", lambda e: e.reciprocal(out=den, in_=den), reads=["den"], writes=["den"])
        yield
        for kv in range(2):
            ov = PS[OB[kv]][:, 0:260].rearrange("p (h d) -> p h d", h=4)
            fw.op("dve", lambda e, ov=ov, kv=kv: e.tensor_tensor(
                out=on[:, kv * 256:(kv + 1) * 256].rearrange("p (h d) -> p h d", h=4), in0=ov[:, :, 0:64],
                in1=den[:, kv * 4:(kv + 1) * 4].unsqueeze(2).to_broadcast([128, 4, 64]), op=ALU.mult),
                reads=[bk(OB[kv]), "den"], writes=["on"])
        yield
        fw.op("act", lambda e: e.activation(out=sqq, in_=on, func=AF.Square, accum_out=ssa), reads=["on"], writes=["sqq", "ssa"])
        rstd_act(ra, ssa, 512, 0.0, ["ssa"], ["ra"])
        fw.op("dve", lambda e: e.tensor_scalar(out=ya, in0=on, scalar1=ra, scalar2=None, op0=ALU.mult), reads=["on", "ra"], writes=["ya"])
        yield
        for i in range(4):
            fw.op("pe", lambda e, i=i: e.transpose(out=PSB[TB][:, i * 128:(i + 1) * 128], in_=ya[:, i * 128:(i + 1) * 128], identity=ident_b),
                  reads=["ya", "ident_b"], writes=[bk(TB)], sig=(i == 3), wtag={bk(TB): ("T4", t)})
        for i in range(4):
            evac_affine(yA[:, ys, i, :], PSB[TB][:, i * 128:(i + 1) * 128], vecs[:, 64 + i:65 + i], None, [bk(TB), "vecs"], [("yA", ys)], wtag={("yA", ys): t}, rtag={bk(TB): ("T4", t)})
        yield

    FB = (6, 7)

    def stage_G1(T0, GT):
        hcol = (T0 % HR) * 128
        NG_ = GT * 128
        hkeys = [("hT", (T0 + i) % HR) for i in range(GT)]

        def fm_proj(m, bank):
            for k in range(8):
                fw.op("pe", lambda e, k=k: e.matmul(out=PS[bank][:, 0:NG_], lhsT=win_fm[:, k, m * 128:(m + 1) * 128], rhs=hT[:, k, hcol:hcol + NG_],
                                                    start=(k == 0), stop=(k == 7)),
                      reads=hkeys + ["win_fm"], writes=[bk(bank)], sig=(k == 7), rtag={("hT", (T0 + i) % HR): T0 + i for i in range(GT)})
        fm_proj(4, FB[0])
        fw.op("act", lambda e: e.activation(out=lrT_pad[0:16, 0:NG_], in_=PS[FB[0]][0:16, 0:NG_], func=AF.Copy), reads=[bk(FB[0])], writes=["lrT"])
        yield
        for c in range(2):
            bank = FB[(c + 1) % 2]
            fw.op("pe", lambda e, c=c, bank=bank: e.matmul(out=PS[bank][:, 0:NG_], lhsT=wgk2_pad[:, c * 128:(c + 1) * 128], rhs=lrT_pad[:, 0:NG_], start=True, stop=True),
                  reads=["wgk2", "lrT"], writes=[bk(bank)])
            fw.op("act", lambda e, c=c, bank=bank: e.activation(out=lbuf[:, c, 0:NG_], in_=PS[bank][:, 0:NG_], func=AF.Exp, scale=-1.0, bias=nbgk[:, c:c + 1]),
                  reads=[bk(bank), "nbgk"], writes=[("lbuf", c)])
            fw.op("act", lambda e, c=c: e.activation(out=lbuf[:, c, 0:NG_], in_=lbuf[:, c, 0:NG_], func=AF.Ln, bias=1.0), reads=[("lbuf", c)], writes=[("lbuf", c)])
            yield
            for j in range(GT):
                fw.op("dve", lambda e, c=c, j=j: e.tensor_tensor_scan(out=Bc[:, c, j * 128:(j + 1) * 128], data0=ones_f, data1=lbuf[:, c, j * 128:(j + 1) * 128],
                                                                      initial=0.0, op0=ALU.mult, op1=ALU.add),
                      reads=[("lbuf", c), "ones_f"], writes=[("Bc", c)])
            yield
            fw.op("act", lambda e, c=c: e.activation(out=lbuf[:, c, 0:NG_], in_=Bc[:, c, 0:NG_], func=AF.Exp, scale=-1.0 / 16, bias=-LN8), reads=[("Bc", c)], writes=[("lbuf", c)])
            fw.op("act", lambda e, c=c: e.activation(out=elast[:, c, 0:GT], in_=Bc[:, c, 0:NG_].rearrange("p (j i) -> p j i", j=GT)[:, :, 127], func=AF.Exp, scale=-1.0 / 16),
                  reads=[("Bc", c)], writes=[("elast", c)])
            fw.op("act", lambda e, c=c: e.activation(out=Bc[:, c, 0:NG_], in_=Bc[:, c, 0:NG_], func=AF.Exp, scale=1.0 / 16), reads=[("Bc", c)], writes=[("Bc", c)])
            yield
        for c in range(2):
            bq = FB[0]
            fm_proj(c, bq)
            for hh in range(2):
                fw.op("dve", lambda e, c=c, hh=hh: e.tensor_tensor(out=qd_pad[hh * 64:(hh + 1) * 64, 2 * c + hh, 0:NG_], in0=PS[bq][hh * 64:(hh + 1) * 64, 0:NG_],
                                                                   in1=lbuf[hh * 64:(hh + 1) * 64, c, 0:NG_], op=ALU.mult),
                      reads=[bk(bq), ("lbuf", c)], writes=["qd_pad"])
            yield
            bkk = FB[1]
            fm_proj(2 + c, bkk)
            fw.op("dve", lambda e, c=c: e.tensor_tensor(out=kdT[:, c, 0:NG_], in0=PS[bkk][:, 0:NG_], in1=Bc[:, c, 0:NG_], op=ALU.mult),
                  reads=[bk(bkk), ("Bc", c)], writes=[("kdT", c)])
            yield
            for j in range(GT):
                fw.op("dve", lambda e, c=c, j=j: e.tensor_scalar(out=kdecT[:, c, j * 128:(j + 1) * 128], in0=kdT[:, c, j * 128:(j + 1) * 128],
                                                                 scalar1=elast[:, c, j:j + 1], scalar2=None, op0=ALU.mult),
                      reads=[("kdT", c), ("elast", c)], writes=[("kdecT", c)])
            yield
        for j in range(GT):
            for c in range(2):
                fw.op("pe", lambda e, c=c, j=j: e.transpose(out=PSB[6][:, (j * 2 + c) * 128:(j * 2 + c + 1) * 128], in_=kdecT[:, c, j * 128:(j + 1) * 128], identity=ident_b),
                      reads=[("kdecT", c), "ident_b"], writes=[bk(6)], sig=(j == GT - 1 and c == 1))
        fw.op("act", lambda e: e.activation(out=kdec[:, 0:GT, :].rearrange("p j c -> p (j c)"), in_=PSB[6][:, 0:GT * 256], func=AF.Copy), reads=[bk(6)], writes=["kdec"])
        yield

    def stage_B0(t):
        hs = t % HR
        vs = t % 2
        for bank, c0, n in ((VGB, 768, 512), (OGB, 1280, 512)):
            for k in range(8):
                fw.op("pe", lambda e, bank=bank, c0=c0, n=n, k=k: e.matmul(
                    out=PS[bank][:, 0:n], lhsT=hT[:, k, hs * 128:(hs + 1) * 128], rhs=win_tm[:, k, c0:c0 + n],
                    start=(k == 0), stop=(k == 7)),
                    reads=[("hT", hs), "win_tmB"], writes=[bk(bank)], sig=(k == 7), rtag={("hT", hs): t})
            yield
        fw.op("act", lambda e: e.activation(out=vg[:, vs, :], in_=PS[VGB], func=AF.Copy), reads=[bk(VGB)], writes=[("vg", vs)])
        yield
        fw.op("act", lambda e: e.activation(out=esc, in_=PS[OGB], func=AF.Exp, scale=-1.0), reads=[bk(OGB)], writes=["t1"])
        fw.op("act", lambda e: e.activation(out=esc, in_=esc, func=AF.Ln, bias=1.0), reads=["t1"], writes=["t1"])
        fw.op("act", lambda e: e.activation(out=esc, in_=esc, func=AF.Exp, scale=-1.0), reads=["t1"], writes=["t1"])
        fw.op("dve", lambda e: e.tensor_tensor(out=sog[:, vs, :], in0=PS[OGB], in1=esc, op=ALU.mult), reads=[bk(OGB), "t1"], writes=[("sog", vs)])
        yield

    AB, OGB2, UB = 6, 7, 6

    def stage_G2(t, j):
        vs = t % 2
        ys = t % 8
        for h in range(4):
            c = h // 2
            fw.op("pe", lambda e, h=h, c=c: e.matmul(out=PS[AB][:, h * 128:(h + 1) * 128], lhsT=kdT[:, c, j * 128:(j + 1) * 128],
                                                     rhs=qd_pad[:, h, j * 128:(j + 1) * 128], start=True, stop=True),
                  reads=[("kdT", c), "qd_pad"], writes=[bk(AB)], sig=(h == 3))
        yield
        fw.op("dve", lambda e: e.tensor_tensor(out=AT, in0=PS[AB].rearrange("p (h i) -> p h i", h=4), in1=gmask.unsqueeze(1).to_broadcast([128, 4, 128]), op=ALU.mult),
              reads=[bk(AB), "gmask"], writes=["AT"])
        yield
        for h in range(4):
            c = h // 2
            fw.op("pe", lambda e, h=h: e.matmul(out=PS[OGB2][:, h * 128:(h + 1) * 128], lhsT=AT[:, h, :], rhs=vg[:, vs, h * 128:(h + 1) * 128], start=True, stop=False),
                  reads=["AT", ("vg", vs)], writes=[bk(OGB2)], sig=False)
            fw.op("pe", lambda e, h=h, c=c: e.matmul(out=PS[OGB2][:, h * 128:(h + 1) * 128], lhsT=qd_pad[:, h, j * 128:(j + 1) * 128], rhs=Sbf[:, c, :], start=False, stop=True),
                  reads=["qd_pad", "Sbf"], writes=[bk(OGB2)], sig=(h == 3))
        yield
        for c in range(2):
            fw.op("pe", lambda e, c=c: e.matmul(out=PS[UB][:, c * 256:(c + 1) * 256], lhsT=kdec[:, j, c * 128:(c + 1) * 128], rhs=vg[:, vs, c * 256:(c + 1) * 256], start=True, stop=True),
                  reads=["kdec", ("vg", vs)], writes=[bk(UB)], sig=(c == 1))
        yield
        for c in range(2):
            for hh in range(2):
                fw.op("dve", lambda e, c=c, hh=hh: e.scalar_tensor_tensor(
                    out=S[hh * 64:(hh + 1) * 64, c, :], in0=S[hh * 64:(hh + 1) * 64, c, :], scalar=elast[hh * 64:(hh + 1) * 64, c, j:j + 1],
                    in1=PS[UB][hh * 64:(hh + 1) * 64, c * 256 + hh * 128:c * 256 + (hh + 1) * 128], op0=ALU.mult, op1=ALU.add),
                    reads=["S", ("elast", c), bk(UB)], writes=["S"])
        fw.op("act", lambda e: e.activation(out=Sbf, in_=S, func=AF.Copy), reads=["S"], writes=["Sbf"])
        yield
        fw.op("act", lambda e: e.activation(out=sqB, in_=PS[OGB2], func=AF.Square), reads=[bk(OGB2)], writes=["sqB"])
        fw.op("dve", lambda e: e.tensor_reduce(out=ssg, in_=sqB.rearrange("p (h d) -> p h d", h=4), axis=AX.X, op=ALU.add), reads=["sqB"], writes=["ssg"])
        rstd_act(rg, ssg, 128, 0.0, ["ssg"], ["rg"])
        fw.op("dve", lambda e: e.tensor_tensor(out=t1.rearrange("p (h d) -> p h d", h=4), in0=PS[OGB2].rearrange("p (h d) -> p h d", h=4),
                                               in1=rg.unsqueeze(2).to_broadcast([128, 4, 128]), op=ALU.mult),
              reads=[bk(OGB2), "rg"], writes=["t1"])
        yield
        fw.op("dve", lambda e: e.tensor_tensor(out=yg, in0=t1, in1=sog[:, vs, :], op=ALU.mult), reads=["t1", ("sog", vs)], writes=["yg"])
        yield
        for i in range(4):
            fw.op("pe", lambda e, i=i: e.transpose(out=PSB[6][:, i * 128:(i + 1) * 128], in_=yg[:, i * 128:(i + 1) * 128], identity=ident_b),
                  reads=["yg", "ident_b"], writes=[bk(6)], sig=(i == 3))
        yield
        fw.op("act", lambda e: e.activation(out=yG.rearrange("p a b -> p (a b)"), in_=PSB[6][:, 0:512], func=AF.Copy, scale=vecs[:, 68:69]),
              reads=[bk(6), "vecs"], writes=["yG"], wtag={"yG": t})
        yield

    WB = (6, 7)

    def stage_W(t):
        ys = t % 8
        for dh in range(2):
            for k in range(8):
                fw.op("pe", lambda e, dh=dh, k=k: e.matmul(out=PS[WB[dh]], lhsT=(yA[:, ys, k, :] if k < 4 else yG[:, k - 4, :]), rhs=wout_sb[:, k, dh * 512:(dh + 1) * 512], start=(k == 0), stop=(k == 7)),
                      reads=[("yA", ys), "yG", "wout"], writes=[bk(WB[dh])], sig=(k == 7), rtag={("yA", ys): t, "yG": t})
            fw.op("dve", lambda e, dh=dh: e.tensor_tensor(out=X1[:, t, dh * 512:(dh + 1) * 512], in0=PS[WB[dh]], in1=X1[:, t, dh * 512:(dh + 1) * 512], op=ALU.add),
                  reads=[bk(WB[dh]), ("X1", t)], writes=[("X1", t)])
            yield

    def chain(*gens):
        for g_ in gens:
            yield from g_

    def run_streams(streams):
        streams = [s for s in streams if s is not None]
        if SEQ_STREAMS:
            for s in streams:
                for _ in s:
                    pass
            return
        if not GREEDY:
            while streams:
                nxt = []
                for s in streams:
                    try:
                        next(s)
                        nxt.append(s)
                    except StopIteration:
                        pass
                streams = nxt
            return
        ready = [0.0] * len(streams)
        alive = list(range(len(streams)))
        while alive:
            i = min(alive, key=lambda j: ready[j])
            fw.cur_stream_end = 0.0
            try:
                next(streams[i])
                ready[i] = max(ready[i], fw.cur_stream_end)
            except StopIteration:
                alive.remove(i)

    def interleave(a, b):
        gens = [a, b]
        while gens:
            nxt = []
            for g_ in gens:
                try:
                    next(g_)
                    nxt.append(g_)
                    yield
                except StopIteration:
                    pass
            gens = nxt

    def stream_A(g):
        ts = list(range(GROUPS[g][0], GROUPS[g][0] + GROUPS[g][1]))
        parts = [stage_T1(ts[0]), stage_T2(ts[0])]
        for i, t in enumerate(ts):
            parts.append(stage_T3(t))
            if i + 1 < len(ts) and PIPE_A:
                parts.append(interleave(stage_T4(t), chain(stage_T1(ts[i + 1]), stage_T2(ts[i + 1]))))
            else:
                parts.append(stage_T4(t))
                if i + 1 < len(ts):
                    parts.append(stage_T1(ts[i + 1]))
                    parts.append(stage_T2(ts[i + 1]))
        return chain(*parts)

    def stream_B(g):
        T0, n_ = GROUPS[g]
        return chain(stage_G1(T0, n_), *[chain(stage_B0(t), stage_G2(t, t - T0), stage_W(t)) for t in range(T0, T0 + n_)])

    def early_exit():
        fw.barrier()
        for g_ in range(4):
            fw.dma("sp", out_v[:, 4 * g_:4 * g_ + 4, :], X1[:, 4 * g_:4 * g_ + 4, :], reads=[("X1", t) for t in range(4 * g_, 4 * g_ + 4)], writes=[("out", g_)])
        fw.barrier()
        return nc, fw, None
    if DBG_STAGE == 10:
        return early_exit()
    run_streams([stream_A(0) if 'A0' not in DBG_SKIP else None, stream_M1() if 'M1' not in DBG_SKIP else None])
    if DBG_STAGE == 11:
        return early_exit()
    m2 = stream_M2()
    NGRP = len(GROUPS)
    for g in range(NGRP):
        run_streams([stream_A(g + 1) if g < NGRP - 1 else None, stream_B(g) if 'B' not in DBG_SKIP else None, m2 if (g == 0 and 'M2' not in DBG_SKIP) else None])
        if DBG_STAGE == 12 + g:
            return early_exit()

    if stage == 1:
        fw.barrier()
        for g in range(4):
            fw.dma("sp", out_v[:, 4 * g:4 * g + 4, :], X1[:, 4 * g:4 * g + 4, :], reads=[("X1", t) for t in range(4 * g, 4 * g + 4)], writes=[("out", g)])
        fw.barrier()
        return nc, fw, None

    fw.barrier()
    reg.reset()
    h2T = reg.get([8, T], BF16)
    wb = [dict(w1=reg.get([EB, 8, 256], BF16), w3=reg.get([EB, 8, 256], BF16), w2=reg.get([EB, 2, 1024], BF16)) for _ in range(2)]
    xn2 = reg.get([2, 1024], F32)
    xT32 = reg.get([2, 1024], F32)
    wr_sb = reg.get([8, 20], F32)
    shrep = reg.get([128], F32)
    cb_sb = reg.get([20], F32)
    L = reg.get([NT, 20], F32)
    g2b = reg.get([1024], F32)
    comb_pad = reg.get([NT, 128], BF16)
    cT_sb = reg.get([T], BF16)
    Cb = reg.get([2, 512], BF16)
    s_sb = reg.get([2, 512], BF16)
    gc_sb = reg.get([2, 512], BF16)
    hid = reg.get([2, EB * 2, 512], BF16)
    sq2 = reg.get([1024], BF16)
    gmax = reg.get([NT], F32)
    goh = reg.get([NT, 4], F32)
    gex = reg.get([NT, 4], F32)
    pg = reg.get([NT], F32)
    tmp16 = reg.get([NT, 16], F32)
    esel = reg.get([NT, 4], F32)
    esel2 = reg.get([NT, 4], F32)
    m1 = reg.get([NT], F32)
    m2 = reg.get([NT], F32)
    oh1 = reg.get([NT, 4], F32)
    oh2 = reg.get([NT, 4], F32)
    rr = reg.get([NT], F32)
    wa = reg.get([NT], F32)
    wb2 = reg.get([NT], F32)
    wsel = reg.get([NT, 4], F32)
    print("phase2 region bytes", reg.off, "of", reg.n)

    def load_expert_batch(bi):
        buf = wb[bi % 2]
        for j in range(EB):
            e_ = bi * EB + j
            fw.dma("pool", buf["w1"][:, j, :, :], w1_d[e_].rearrange("(k p) f -> p k f", p=128), writes=[("w1", bi % 2, j)])
            fw.dma("pool", buf["w3"][:, j, :, :], w3_d[e_].rearrange("(k p) f -> p k f", p=128), writes=[("w3", bi % 2, j)])
            fw.dma("pool", buf["w2"][:, j, :, :], w2_d[e_].rearrange("(k p) d -> p k d", p=128), writes=[("w2", bi % 2, j)])

    fw.dma("sp", wr_sb, wr_d.rearrange("(k p) n -> p k n", p=128), writes=["wr"])
    load_expert_batch(0)
    load_expert_batch(1)

    for k in range(8):
        fw.op("dve", lambda e, k=k: e.tensor_scalar(out=shrep, in0=ident_f, scalar1=mod[:, 40 + k:41 + k], scalar2=None, op0=ALU.mult),
              reads=["ident_f", ("mod", 5)], writes=["shrep"])
        fw.op("pe", lambda e, k=k: e.matmul(out=PS[k // 4][:, (k % 4) * 128:(k % 4 + 1) * 128], lhsT=ones_f, rhs=shrep, start=True, stop=True),
              reads=["ones_f", "shrep"], writes=[bk(k // 4)])
    for hh in range(2):
        fw.op("act", lambda e, hh=hh: e.activation(out=g2b[:, hh * 512:(hh + 1) * 512], in_=PS[hh], func=AF.Copy), reads=[bk(hh)], writes=["g2b"])
    for k in range(8):
        fw.op("dve", lambda e, k=k: e.tensor_copy(out=shrep, in_=mod[:, 24 + k:25 + k].to_broadcast([128, 128])),
              reads=[("mod", 3)], writes=["shrep"])
        fw.op("pe", lambda e, k=k: e.matmul(out=PS[2][:, 0:20], lhsT=shrep, rhs=wr_sb[:, k, :], start=(k == 0), stop=(k == 7)),
              reads=["shrep", "wr"], writes=[bk(2)])
    fw.op("dve", lambda e: e.tensor_tensor(out=cb_sb, in0=PS[2][:, 0:20], in1=brg, op=ALU.add), reads=[bk(2), "brg"], writes=["cb"])
    for k in range(8):
        fw.op("dve", lambda e, k=k: e.tensor_scalar(out=wr_sb[:, k, :], in0=wr_sb[:, k, :], scalar1=gs2[:, k:k + 1], scalar2=None, op0=ALU.mult),
              reads=["wr", "gs2", bk(2)], writes=["wr"])
    fw.op("dve", lambda e: e.memset(comb_pad, 0.0), writes=["comb_pad"])
    fw.op("dve", lambda e: e.memset(cT_sb, 0.0), writes=["cT"])

    def stage_N(t):
        p_ = t % 2
        xs = p_
        b0_, b1_, rb = 2 * p_, 2 * p_ + 1, 4 + p_
        tb = (b0_, b1_)
        fw.op("act", lambda e: e.activation(out=sq2, in_=X1[:, t, :], func=AF.Square, accum_out=ssx[:, t:t + 1]),
              reads=[("X1", t)], writes=["sq2", ("ssx", t)])
        rstd_act(rsx[:, t:t + 1], ssx[:, t:t + 1], D, 0.0, [("ssx", t)], [("rsx", t)])
        fw.op("dve", lambda e: e.tensor_scalar(out=xn2[:, xs, :], in0=X1[:, t, :], scalar1=rsx[:, t:t + 1], scalar2=None, op0=ALU.mult),
              reads=[("X1", t), ("rsx", t)], writes=[("xn2", xs)])
        yield
        for k in range(8):
            fw.op("pe", lambda e, k=k: e.transpose(out=PS[tb[k // 4]][:, (k % 4) * 128:(k % 4 + 1) * 128], in_=xn2[:, xs, k * 128:(k + 1) * 128], identity=ident_f),
                  reads=[("xn2", xs), "ident_f"], writes=[bk(tb[k // 4])], sig=(k % 4 == 3))
        yield
        for hh in range(2):
            fw.op("dve", lambda e, hh=hh: e.tensor_copy(out=xT32[:, xs, hh * 512:(hh + 1) * 512], in_=PS[tb[hh]]), reads=[bk(tb[hh])], writes=[("xT32", xs)])
        yield
        for k in range(8):
            evac_affine(h2T[:, k, t * 128:(t + 1) * 128], PS[tb[k // 4]][:, (k % 4) * 128:(k % 4 + 1) * 128], gs2[:, k:k + 1], mod[:, 24 + k:25 + k],
                        [bk(tb[k // 4]), "gs2", ("mod", 3)], [("h2T", t // 4)])
        yield
        for k in range(8):
            fw.op("pe", lambda e, k=k: e.matmul(out=PS[rb][:, 0:20], lhsT=xT32[:, xs, k * 128:(k + 1) * 128], rhs=wr_sb[:, k, :], start=(k == 0), stop=(k == 7)),
                  reads=[("xT32", xs), "wr"], writes=[bk(rb)], sig=(k == 7))
        yield
        fw.op("dve", lambda e: e.tensor_tensor(out=L[:, t, :], in0=PS[rb][:, 0:20], in1=cb_sb, op=ALU.add), reads=[bk(rb), "cb"], writes=["L"])
        yield

    run_streams([chain(*[stage_N(t) for t in range(0, NT, 2)]), chain(*[stage_N(t) for t in range(1, NT, 2)])])

    def dv(fn, reads, writes):
        fw.op("dve", fn, reads=reads, writes=writes)
    gl = L[:, :, 0:4]
    el = L[:, :, 4:20].rearrange("p t (g i) -> p t g i", g=4)
    dv(lambda e: e.tensor_reduce(out=gmax, in_=gl, axis=AX.X, op=ALU.max), ["L"], ["gmax"])
    dv(lambda e: e.tensor_tensor(out=goh, in0=gl, in1=gmax.unsqueeze(2).to_broadcast([128, NT, 4]), op=ALU.is_equal), ["L", "gmax"], ["goh"])
    dv(lambda e: e.tensor_tensor(out=gex, in0=gl, in1=gmax.unsqueeze(2).to_broadcast([128, NT, 4]), op=ALU.subtract), ["L", "gmax"], ["gex"])
    fw.op("act", lambda e: e.activation(out=gex, in_=gex, func=AF.Exp), reads=["gex"], writes=["gex"])
    dv(lambda e: e.tensor_reduce(out=pg, in_=gex, axis=AX.X, op=ALU.add), ["gex"], ["pg"])
    dv(lambda e: e.reciprocal(out=pg, in_=pg), ["pg"], ["pg"])
    dv(lambda e: e.tensor_tensor(out=tmp16.rearrange("p t (g i) -> p t g i", g=4), in0=el, in1=goh.unsqueeze(3).to_broadcast([128, NT, 4, 4]), op=ALU.mult),
       ["L", "goh"], ["tmp16"])
    dv(lambda e: e.tensor_reduce(out=esel, in_=tmp16.rearrange("p t (g i) -> p t i g", g=4), axis=AX.X, op=ALU.add), ["tmp16"], ["esel"])
    dv(lambda e: e.tensor_reduce(out=m1, in_=esel, axis=AX.X, op=ALU.max), ["esel"], ["m1"])
    dv(lambda e: e.tensor_tensor(out=oh1, in0=esel, in1=m1.unsqueeze(2).to_broadcast([128, NT, 4]), op=ALU.is_equal), ["esel", "m1"], ["oh1"])
    dv(lambda e: e.scalar_tensor_tensor(out=esel2, in0=oh1, scalar=-1e30, in1=esel, op0=ALU.mult, op1=ALU.add), ["oh1", "esel"], ["esel2"])
    dv(lambda e: e.tensor_reduce(out=m2, in_=esel2, axis=AX.X, op=ALU.max), ["esel2"], ["m2"])
    dv(lambda e: e.tensor_tensor(out=oh2, in0=esel2, in1=m2.unsqueeze(2).to_broadcast([128, NT, 4]), op=ALU.is_equal), ["esel2", "m2"], ["oh2"])
    dv(lambda e: e.tensor_tensor(out=rr, in0=m2, in1=m1, op=ALU.subtract), ["m1", "m2"], ["rr"])
    fw.op("act", lambda e: e.activation(out=rr, in_=rr, func=AF.Exp), reads=["rr"], writes=["rr"])
    dv(lambda e: e.tensor_scalar_add(out=wa, in0=rr, scalar1=1.0), ["rr"], ["wa"])
    dv(lambda e: e.reciprocal(out=wa, in_=wa), ["wa"], ["wa"])
    dv(lambda e: e.tensor_tensor(out=wa, in0=wa, in1=pg, op=ALU.mult), ["wa", "pg"], ["wa"])
    dv(lambda e: e.tensor_tensor(out=wb2, in0=wa, in1=rr, op=ALU.mult), ["wa", "rr"], ["wb2"])
    dv(lambda e: e.tensor_tensor(out=wsel, in0=oh1, in1=wa.unsqueeze(2).to_broadcast([128, NT, 4]), op=ALU.mult), ["oh1", "wa"], ["wsel"])
    dv(lambda e: e.tensor_tensor(out=oh2, in0=oh2, in1=wb2.unsqueeze(2).to_broadcast([128, NT, 4]), op=ALU.mult), ["oh2", "wb2"], ["oh2"])
    dv(lambda e: e.tensor_tensor(out=wsel, in0=wsel, in1=oh2, op=ALU.add), ["wsel", "oh2"], ["wsel"])
    dv(lambda e: e.tensor_tensor(out=comb_pad[:, :, 0:16].rearrange("p t (g i) -> p t g i", g=4), in0=goh.unsqueeze(3).to_broadcast([128, NT, 4, 4]),
                                 in1=wsel.unsqueeze(2).to_broadcast([128, NT, 4, 4]), op=ALU.mult), ["goh", "wsel"], ["comb_pad"])
    for half in range(2):
        for i in range(8):
            t = half * 8 + i
            fw.op("pe", lambda e, t=t, i=i: e.transpose(out=PSB[3][:, i * 128:(i + 1) * 128], in_=comb_pad[:, t, :], identity=ident_b),
                  reads=["comb_pad", "ident_b"], writes=[bk(3)], sig=(i == 7))
        fw.op("act", lambda e, half=half: e.activation(out=cT_sb[0:16, half * 1024:(half + 1) * 1024], in_=PSB[3][0:16, :], func=AF.Copy), reads=[bk(3)], writes=["cT"])

    NB = NEXP // EB
    AG = (0, 1, 2, 3)
    CBK = 4
    YB = (5, 6, 7)
    yi = [0]
    ui = [0]
    for bi in range(NB):
        buf = wb[bi % 2]
        for j in range(EB):
            for fc in range(2):
                fw.op("dve", lambda e, j=j, fc=fc: e.tensor_tensor(out=buf["w2"][:, j, fc, :], in0=buf["w2"][:, j, fc, :], in1=g2b, op=ALU.mult),
                      reads=[("w2", bi % 2, j), "g2b"], writes=[("w2", bi % 2, j)])
        for tg in range(4):
            hs = tg % 2
            for j in range(EB):
                e_ = bi * EB + j
                fw.op("pe", lambda e, e_=e_, tg=tg: e.matmul(out=PS[CBK], lhsT=sel[:, e_, :], rhs=cT_sb[:, tg * 512:(tg + 1) * 512], start=True, stop=True),
                      reads=["sel", "cT"], writes=[bk(CBK)])
                cs = ui[0] % 2
                fw.op("act", lambda e, cs=cs: e.activation(out=Cb[:, cs, :], in_=PS[CBK], func=AF.Copy), reads=[bk(CBK)], writes=[("Cb", cs)])
                for fc in range(2):
                    u = ui[0] % 2
                    ab, gb = AG[2 * u], AG[2 * u + 1]
                    ui[0] += 1
                    for (bank, wname) in ((ab, "w1"), (gb, "w3")):
                        for k in range(8):
                            fw.op("pe", lambda e, bank=bank, wname=wname, j=j, fc=fc, k=k, tg=tg: e.matmul(
                                out=PS[bank], lhsT=buf[wname][:, j, k, fc * 128:(fc + 1) * 128], rhs=h2T[:, k, tg * 512:(tg + 1) * 512],
                                start=(k == 0), stop=(k == 7)),
                                reads=[(wname, bi % 2, j), ("h2T", tg)], writes=[bk(bank)], sig=(k == 7))
                    fw.op("act", lambda e, ab=ab, u=u: e.activation(out=s_sb[:, u, :], in_=PS[ab], func=AF.Silu), reads=[bk(ab)], writes=[("s", u)])
                    fw.op("dve", lambda e, gb=gb, u=u, cs=cs: e.tensor_tensor(out=gc_sb[:, u, :], in0=PS[gb], in1=Cb[:, cs, :], op=ALU.mult),
                          reads=[bk(gb), ("Cb", cs)], writes=[("gc", u)])
                    fw.op("dve", lambda e, u=u, j=j, fc=fc, hs=hs: e.tensor_tensor(out=hid[:, hs, j * 2 + fc, :], in0=s_sb[:, u, :], in1=gc_sb[:, u, :], op=ALU.mult),
                          reads=[("s", u), ("gc", u)], writes=[("hid", hs)])
                ui[0] += 0
            for tt in range(4):
                t = tg * 4 + tt
                for dh in range(2):
                    yb = YB[yi[0] % 3]
                    yi[0] += 1
                    n = EB * 2
                    for q in range(n):
                        j, fc = q // 2, q % 2
                        fw.op("pe", lambda e, yb=yb, q=q, j=j, fc=fc, tt=tt, dh=dh, hs=hs: e.matmul(
                            out=PS[yb], lhsT=hid[:, hs, q, tt * 128:(tt + 1) * 128], rhs=buf["w2"][:, j, fc, dh * 512:(dh + 1) * 512],
                            start=(q == 0), stop=(q == n - 1)),
                            reads=[("hid", hs), ("w2", bi % 2, j)], writes=[bk(yb)], sig=(q == n - 1))
                    fw.op("dve", lambda e, yb=yb, t=t, dh=dh: e.tensor_tensor(out=X1[:, t, dh * 512:(dh + 1) * 512], in0=PS[yb], in1=X1[:, t, dh * 512:(dh + 1) * 512], op=ALU.add),
                          reads=[bk(yb), ("X1", t)], writes=[("X1", t)])
            if bi == NB - 1:
                fw.dma("sp", out_v[:, 4 * tg:4 * tg + 4, :], X1[:, 4 * tg:4 * tg + 4, :], reads=[("X1", t) for t in range(4 * tg, 4 * tg + 4)], writes=[("out", tg)])
        if bi + 2 < NB:
            load_expert_batch(bi + 2)
    fw.barrier()
    print("ops", fw.nops, "sem counts", fw.cnt)
    return nc, fw, None


def host_inputs(inputs, b):
    f = np.float32
    g = lambda k: np.asarray(inputs[k], dtype=f)
    P = [0, 4, 1, 5, 2, 6, 3, 7]
    w_in = g("w_in")[0]
    qcols = np.concatenate([np.arange(h * 64, (h + 1) * 64) for h in P])
    w_in_tm = np.concatenate([w_in[:, qcols], w_in[:, 512:768], w_in[:, 1280:1792], w_in[:, 1808:2320]], axis=1)
    w_in_fm = np.concatenate([w_in[:, 768:1280], w_in[:, 1792:1920]], axis=1)
    col = lambda v: np.ascontiguousarray(v.reshape(-1, 128).T)
    vecs = np.concatenate([
        col(g("b_ada")[0]), col(g("g_norm1")[0]), col(g("g_norm2")[0]), col(g("g_att_out")[0]), col(g("g_gla_out")[0]),
        col(g("b_gk")[0]), col(np.tile(g("q_norm")[0], 2)), col(np.tile(g("k_norm")[0], 2)), col(g("c")[b]),
    ], axis=1)
    assert vecs.shape == (128, NV)
    sinks_b = np.ascontiguousarray(np.broadcast_to(g("sinks")[0][None, :], (128, 8)))
    brg = np.ascontiguousarray(np.broadcast_to(np.concatenate([g("b_group")[0], g("b_router")[0]])[None, :], (128, 20)))
    slopes = np.exp2(-8.0 * np.arange(1, 9) / 8).astype(f)
    kj = np.arange(128)[:, None]
    qi = np.arange(128)[None, :]
    abias = np.zeros((128, 4, 512), f)
    for kv in range(2):
        for gg in range(4):
            h = kv * 4 + gg
            dcur = (qi - kj).astype(f)
            abias[:, kv * 2 + 0, gg * 128:(gg + 1) * 128] = np.where(dcur >= 0, -slopes[h] * dcur, -30000.0)
            dprev = (qi - kj + 128).astype(f)
            abias[:, kv * 2 + 1, gg * 128:(gg + 1) * 128] = np.where(dprev < 128, -slopes[h] * dprev, -30000.0)
    gmask = (kj <= qi).astype(f)
    sel = np.zeros((128, 16, 128), f)
    for e in range(16):
        sel[e, e, :] = 1.0
    w_rt = np.concatenate([g("w_group")[0], g("w_router")[0]], axis=1)
    return {
        "x": np.ascontiguousarray(g("x")[b]), "vecs": vecs, "sinks_b": sinks_b, "brg": brg,
        "ident": np.eye(128, dtype=f), "abias": abias, "gmask": gmask, "sel": sel,
        "w_ada": g("w_ada")[0], "w_in_tm": np.ascontiguousarray(w_in_tm), "w_in_fm": np.ascontiguousarray(w_in_fm),
        "w_gk2": g("w_gk2")[0], "w_out": g("w_out")[0], "w_rt": np.ascontiguousarray(w_rt),
        "w1": g("w1")[0], "w3": g("w3")[0], "w2": g("w2")[0],
    }


def kernel(**inputs):
    nc, fw, _ = build_nc()
    in_maps = [host_inputs(inputs, b) for b in range(8)]
    res = run_bass_kernel_spmd(nc, in_maps, core_ids=list(range(8)))
    return np.stack([r["out"] for r in res.results], axis=0).astype(np.float32)
```

```python
import math
import numpy as np
import concourse.bass as bass
import concourse.mybir as mybir
from concourse.bass_utils import run_bass_kernel_spmd

F32 = mybir.dt.float32
BF16 = mybir.dt.bfloat16
AF = mybir.ActivationFunctionType
ALU = mybir.AluOpType
AX = mybir.AxisListType

T = 2048
D = 1024
NT = 16
EPS = 1e-6
LN8 = math.log(8.0)
NV = 81
NEXP = 16
EB = 2
SAME_ENGINE_FULL_SYNC = True
SEQ_STREAMS = False
GREEDY = True
PE_SLOW = 1.0
PIPE_A = True
GROUPS = tuple((2 * i, 2) for i in range(8))
PRIO_B = 0.0
M_LAT = 0.15
M_ACT0 = 0.22
M_DVE0 = 0.12
LEAD = 1
DBG_SKIP = ()
DBG_STAGE = 99
MODCOL = 320
DBG_NT = 4
DBG_SB = (3, 4)
DBG_OB = (5, 4)


class FW:
    def __init__(self, nc):
        self.nc = nc
        self.eng = {"pe": nc.tensor, "act": nc.scalar, "dve": nc.vector, "pool": nc.gpsimd, "sp": nc.sync}
        self.sem = {e: nc.alloc_semaphore("s_" + e) for e in self.eng}
        self.cnt = {e: 0 for e in self.eng}
        self.seen = {e: {} for e in self.eng}
        self.dsem = {q: [nc.alloc_semaphore("d_%s%d" % (q, i)) for i in range(14)] for q in ("sp", "pool")}
        self.dval = {q: [0] * 14 for q in ("sp", "pool")}
        self.drr = {"sp": 0, "pool": 0}
        self.lastw = {}
        self.readers = {}
        self.pe_pending = []
        self.nops = 0
        self.log = None
        self.tags = {}
        self.eng_free = {e: 0.0 for e in self.eng}
        self.tok_end = {}
        self.cur_stream_end = 0.0

    def _deps(self, eng, reads, writes, is_dma):
        toks = []
        for k in reads:
            w = self.lastw.get(k)
            if w is not None:
                toks.append((w, "raw"))
            if isinstance(k, tuple) and k[0] == "ps":
                for r in self.readers.get(k, ()):
                    toks.append((r, "rar"))
        for k in writes:
            w = self.lastw.get(k)
            if w is not None:
                toks.append((w, "waw"))
            for r in self.readers.get(k, ()):
                toks.append((r, "war"))
        need = {}
        for tok, kind in toks:
            if tok[0] == "c":
                src = tok[1]
                if src == eng and not is_dma:
                    if eng == "pe":
                        continue
                    if kind != "raw" and not SAME_ENGINE_FULL_SYNC:
                        continue
                assert tok[2] is not None, "unresolved PE token"
                key = ("c", src)
                need[key] = max(need.get(key, 0), tok[2])
            else:
                key = ("d", tok[1], tok[2])
                need[key] = max(need.get(key, 0), tok[3])
        waits = []
        for key, val in need.items():
            if self.seen[eng].get(key, 0) >= val:
                continue
            self.seen[eng][key] = val
            if key[0] == "c":
                waits.append((self.sem[key[1]], val))
            else:
                waits.append((self.dsem[key[1]][key[2]], val))
        return waits

    def _record(self, tok, reads, writes):
        for k in reads:
            self.readers.setdefault(k, []).append(tok)
        for k in writes:
            self.lastw[k] = tok
            self.readers[k] = []

    def _tagcheck(self, reads, writes, rtag, wtag, keep_tag):
        for k in reads:
            if rtag is not None and k in rtag:
                assert self.tags.get(k) == rtag[k], ("stale read", k, self.tags.get(k), rtag[k])
        if not keep_tag:
            for k in writes:
                self.tags[k] = (wtag or {}).get(k)

    def op(self, eng, fn, reads=(), writes=(), sig=True, rtag=None, wtag=None, keep_tag=False):
        self._tagcheck(reads, writes, rtag, wtag, keep_tag)
        waits = self._deps(eng, reads, writes, False)
        e = self.eng[eng]
        for s, v in waits[1:]:
            e.wait_ge(s, v)
        rec = {}
        ins = fn(_EngProxy(e, rec))
        self._model(eng, rec, reads, writes)
        if waits:
            ins._wait_ge(waits[0][0], waits[0][1])
        tok = ["c", eng, None]
        if eng == "pe" and not sig:
            self.pe_pending.append(tok)
        else:
            self.cnt[eng] += 1
            ins.then_inc(self.sem[eng], 1)
            tok[2] = self.cnt[eng]
            if eng == "pe":
                for p in self.pe_pending:
                    p[2] = tok[2]
                self.pe_pending = []
        self._record(tok, reads, writes)
        self.nops += 1
        if self.log is not None:
            import sys as _s
            fr = _s._getframe(1)
            nm = None
            ln = fr.f_lineno
            while fr is not None:
                if fr.f_code.co_name.startswith(("stage_", "stream_", "mod_chunk")):
                    nm = fr.f_code.co_name
                    break
                fr = fr.f_back
            self.log.append((eng, tok[2], (nm, ln), list(reads), list(writes), [(str(s), v) for s, v in waits]))
        return ins

    def dma(self, q, out, in_, reads=(), writes=()):
        waits = self._deps(q, reads, writes, True)
        i = self.drr[q]
        self.drr[q] = (i + 1) % len(self.dsem[q])
        prev = self.dval[q][i]
        key = ("d", q, i)
        if prev and self.seen[q].get(key, 0) < prev:
            self.seen[q][key] = prev
            waits.append((self.dsem[q][i], prev))
        e = self.eng[q]
        for s, v in waits:
            e.wait_ge(s, v)
        e.dma_start(out=out, in_=in_).then_inc(self.dsem[q][i], 16)
        self.dval[q][i] = prev + 16
        tok = ["d", q, i, prev + 16]
        self._record(tok, reads, writes)
        try:
            nb = 1
            for d_ in in_.shape:
                nb *= d_
            nb *= 4
        except Exception:
            nb = 1 << 20
        st = max(self.eng_free[q], max([self.tok_end.get(("w", k), 0.0) for k in list(reads) + list(writes)] + [self.tok_end.get(("r", k), 0.0) for k in writes] + [0.0]))
        en = st + 2.0 + nb / 180e3
        self.eng_free[q] = st + nb / 180e3
        for k in writes:
            self.tok_end[("w", k)] = en
            self.tok_end[("r", k)] = 0.0
        self.nops += 1

    def _model(self, eng, rec, reads, writes):
        kw = rec.get("kw", {})
        name = rec.get("name", "")

        def fsz(ap):
            try:
                sh = ap.shape
                n = 1
                for d in sh[1:]:
                    n *= d
                return n
            except Exception:
                return 128
        if eng == "pe":
            if name == "transpose":
                n = 128
                dur = 0.07 if kw["in_"].dtype == BF16 else 0.3
            else:
                n = fsz(kw["rhs"])
                dur = (max(n, 64) / 2400.0 + 0.02) * PE_SLOW
                if kw["rhs"].dtype == F32:
                    dur *= 4
        elif eng == "act":
            n = fsz(kw.get("out"))
            dur = M_ACT0 + n / 1200.0 + (0.1 if kw.get("accum_out") is not None else 0.0)
        else:
            n = fsz(kw.get("out")) if kw.get("out") is not None else 128
            dur = M_DVE0 + n / 960.0
            if name == "tensor_tensor_scan":
                dur = 0.12 + 2 * n / 960.0
            if name == "reciprocal":
                dur = 0.12 + 6 * n / 960.0
        ready = 0.0
        for k in reads:
            ready = max(ready, self.tok_end.get(("w", k), 0.0))
        for k in writes:
            ready = max(ready, self.tok_end.get(("w", k), 0.0), self.tok_end.get(("r", k), 0.0))
        start = max(self.eng_free[eng], ready + M_LAT)
        end = start + dur
        self.eng_free[eng] = end
        for k in reads:
            self.tok_end[("r", k)] = max(self.tok_end.get(("r", k), 0.0), end)
        for k in writes:
            self.tok_end[("w", k)] = end
            self.tok_end[("r", k)] = 0.0
        self.cur_stream_end = max(self.cur_stream_end, end)

    def barrier(self):
        for e in self.eng:
            for src in self.eng:
                if src == e:
                    continue
                v = self.cnt[src]
                if v and self.seen[e].get(("c", src), 0) < v:
                    self.seen[e][("c", src)] = v
                    self.eng[e].wait_ge(self.sem[src], v)
            for q in ("sp", "pool"):
                for i, v in enumerate(self.dval[q]):
                    if v and self.seen[e].get(("d", q, i), 0) < v:
                        self.seen[e][("d", q, i)] = v
                        self.eng[e].wait_ge(self.dsem[q][i], v)


class _EngProxy:
    def __init__(self, real, rec):
        self._real = real
        self._rec = rec

    def __getattr__(self, name):
        f = getattr(self._real, name)
        rec = self._rec

        def w(*a, **k):
            rec["name"] = name
            rec["kw"] = k
            return f(*a, **k)
        return w


class Carver:
    def __init__(self, ap, nbytes):
        self.ap = ap
        self.n = nbytes
        self.off = 0

    def get(self, free_shape, dt):
        esz = 4 if dt == F32 else 2
        n = int(np.prod(free_shape)) * esz
        n_al = (n + 63) // 64 * 64
        assert self.off + n_al <= self.n, "carver overflow %d + %d > %d" % (self.off, n_al, self.n)
        v = self.ap[:, self.off // 2:(self.off + n) // 2]
        self.off += n_al
        if dt == F32:
            v = v.bitcast(F32)
        if len(free_shape) == 2:
            v = v.rearrange("p (a b) -> p a b", a=free_shape[0])
        elif len(free_shape) == 3:
            v = v.rearrange("p (a b c) -> p a b c", a=free_shape[0], b=free_shape[1])
        return v

    def reset(self):
        self.off = 0


def build_nc(stage=99, dbg=False):
    nc = bass.Bass("TRN2", target_bir_lowering=False)
    fw = FW(nc)

    def din(name, shape, dt=F32):
        return nc.dram_tensor(name, list(shape), dt, kind="ExternalInput").ap()

    x_d = din("x", [T, D])
    vecs_d = din("vecs", [128, NV])
    sinks_d = din("sinks_b", [128, 8])
    brg_d = din("brg", [128, 20])
    ident_d = din("ident", [128, 128])
    abias_d = din("abias", [128, 4, 512])
    gmask_d = din("gmask", [128, 128])
    sel_d = din("sel", [128, 16, 128])
    wada_d = din("w_ada", [D, 6 * D])
    wintm_d = din("w_in_tm", [D, 1792])
    winfm_d = din("w_in_fm", [D, 640])
    wgk2_d = din("w_gk2", [16, 256])
    wout_d = din("w_out", [D, D])
    wr_d = din("w_rt", [D, 20])
    w1_d = din("w1", [NEXP, D, 256])
    w3_d = din("w3", [NEXP, D, 256])
    w2_d = din("w2", [NEXP, 256, D])
    out_d = nc.dram_tensor("out", [T, D], F32, kind="ExternalOutput").ap()
    dbg_d = nc.dram_tensor("dbg", [128, 4096], F32, kind="ExternalOutput").ap() if dbg else None

    x_v = x_d.rearrange("(t p) d -> p t d", p=128)
    out_v = out_d.rearrange("(t p) d -> p t d", p=128)

    X1 = nc.alloc_sbuf_tensor("X1", [128, NT, D], F32).ap()
    PERS_BYTES = 11776
    pers = Carver(nc.alloc_sbuf_tensor("pers", [128, PERS_BYTES // 2], BF16).ap(), PERS_BYTES)
    REG_BYTES = 135168
    reg = Carver(nc.alloc_sbuf_tensor("reg", [128, REG_BYTES // 2], BF16).ap(), REG_BYTES)

    ident_f = pers.get([128], F32)
    ident_b = pers.get([128], BF16)
    ones_f = pers.get([128], F32)
    ones_b = pers.get([128], BF16)
    abias = pers.get([4, 512], BF16)
    gmask = pers.get([128], BF16)
    sel = pers.get([16, 128], BF16)
    vecs = pers.get([NV], F32)
    es = pers.get([8], F32)
    brg = pers.get([20], F32)
    mod = pers.get([48], F32)
    gs1 = pers.get([8], F32)
    gs2 = pers.get([8], F32)
    nbgk = pers.get([2], F32)
    ssx = pers.get([NT], F32)
    rsx = pers.get([NT], F32)
    sc_b = pers.get([8], BF16)
    tmp8 = pers.get([8], F32)
    dbg_sb = pers.get([16], F32)

    PS = [nc.alloc_psum_tensor("ps%d" % i, [128, 512], F32).ap() for i in range(8)]
    PSB = [p.bitcast(BF16) for p in PS]

    def bk(i):
        return ("ps", i)

    fw.dma("sp", ident_f, ident_d, writes=["ident_f"])
    fw.dma("sp", vecs, vecs_d, writes=["vecs"])
    fw.dma("sp", es, sinks_d, writes=["es"])
    fw.dma("sp", brg, brg_d, writes=["brg"])
    fw.dma("pool", ident_b, ident_d, writes=["ident_b"])
    fw.dma("pool", abias, abias_d, writes=["abias"])
    fw.dma("pool", gmask, gmask_d, writes=["gmask"])
    fw.dma("pool", sel, sel_d, writes=["sel"])
    fw.dma("sp", X1[:, 0:4, :], x_v[:, 0:4, :], writes=[("X1", t) for t in range(4)])
    fw.op("dve", lambda e: e.memset(ones_f, 1.0), writes=["ones_f"])
    fw.op("dve", lambda e: e.memset(ones_b, 1.0), writes=["ones_b"])

    HR = 8
    hT = reg.get([8, HR * 128], BF16)
    win_tm = reg.get([8, 1792], BF16)
    win_fm = reg.get([8, 640], BF16)
    wout_sb = reg.get([8, 1024], BF16)
    wada_buf = [reg.get([8, 128], BF16) for _ in range(2)]
    xn = reg.get([1, 1024], BF16)
    sqq = reg.get([512], F32)
    qn = reg.get([512], BF16)
    kn = reg.get([128], BF16)
    ssq = reg.get([8], F32)
    rq = reg.get([8], F32)
    ssk = reg.get([2], F32)
    rk = reg.get([2], F32)
    qTA = reg.get([2, 512], BF16)
    qTB = reg.get([2, 512], BF16)
    kT = reg.get([4 * 128], BF16)
    vaug = reg.get([4, 2, 65], BF16)
    vg = reg.get([2, 512], BF16)
    sog = reg.get([2, 512], BF16)
    PT = reg.get([4, 512], BF16)
    den = reg.get([8], F32)
    on = reg.get([512], F32)
    ssa = reg.get([1], F32)
    ra = reg.get([1], F32)
    ya = reg.get([512], BF16)
    yA = reg.get([8, 4, 128], BF16)
    yG = reg.get([4, 128], BF16)
    OFF_LBUF = reg.off
    lbuf = reg.get([2, 512], F32)
    Bc = reg.get([2, 512], F32)
    elast = reg.get([2, 4], F32)
    OFF_QDPAD = reg.off
    qd_pad = reg.get([4, 512], BF16)
    kdT = reg.get([2, 512], BF16)
    kdecT = reg.get([2, 512], BF16)
    kdec = reg.get([4, 256], BF16)
    AT = reg.get([4, 128], BF16)
    S = reg.get([2, 128], F32)
    Sbf = reg.get([2, 128], BF16)
    ssg = reg.get([4], F32)
    rg = reg.get([4], F32)
    t1 = reg.get([512], F32)
    sq_b = sqq.bitcast(BF16)
    esc = t1
    sqB = reg.get([512], BF16)
    yg = reg.get([512], BF16)
    lrT_pad = reg.get([512], BF16)
    wgk2_pad = reg.get([256], BF16)
    print("phase1 region bytes", reg.off, "of", reg.n)

    wada_v = wada_d.rearrange("(k p) n -> p k n", p=128)

    ccol = vecs[:, 73:81]
    fw.op("act", lambda e: e.activation(out=tmp8, in_=ccol, func=AF.Exp, scale=-1.0), reads=["vecs"], writes=["tmp8"])
    fw.op("dve", lambda e: e.tensor_scalar_add(out=tmp8, in0=tmp8, scalar1=1.0), reads=["tmp8"], writes=["tmp8"])
    fw.op("dve", lambda e: e.reciprocal(out=tmp8, in_=tmp8), reads=["tmp8"], writes=["tmp8"])
    fw.op("dve", lambda e: e.tensor_tensor(out=sc_b, in0=tmp8, in1=ccol, op=ALU.mult), reads=["tmp8", "vecs"], writes=["sc_b"])
    MODB = 5
    wctr = [0]

    def mod_chunk(c):
        bi = wctr[0] % 2
        wctr[0] += 1
        buf = wada_buf[bi]
        if 'chunkdma' in DBG_SKIP and c >= 16:
            return
        fw.dma("pool", buf, wada_v[:, :, c * 128:(c + 1) * 128], writes=[("wada", bi)])
        for k in range(8):
            fw.op("pe", lambda e, k=k: e.matmul(out=PS[MODB][:, MODCOL:MODCOL + 1], lhsT=buf[:, k, :], rhs=sc_b[:, k:k + 1], start=(k == 0), stop=(k == 7)),
                  reads=[("wada", bi), "sc_b"], writes=[bk(MODB)], sig=(k == 7), keep_tag=True)
        fw.op("dve", lambda e: e.tensor_tensor(out=mod[:, c:c + 1], in0=PS[MODB][:, MODCOL:MODCOL + 1], in1=vecs[:, c:c + 1], op=ALU.add),
              reads=[bk(MODB), "vecs"], writes=[("mod", c // 8)])

    MK = [("mod", i) for i in range(6)]
    big = [reg.ap[:, boff // 2:(boff + 8192) // 2].rearrange("p (k n) -> p k n", k=8) for boff in (OFF_LBUF, OFF_QDPAD)]
    bigkeys = [[("lbuf", 0), ("lbuf", 1), ("Bc", 0), ("Bc", 1)], ["qd_pad", ("kdT", 0), ("kdT", 1), ("kdecT", 0), ("kdecT", 1)]]
    for cc in range(4):
        fw.dma("pool", big[cc % 2], wada_v[:, :, cc * 512:(cc + 1) * 512], writes=bigkeys[cc % 2])
        if cc == 0:
            for g_ in range(1, 4):
                pass
        for j in range(4):
            c = cc * 4 + j
            for k in range(8):
                fw.op("pe", lambda e, k=k, j=j, cc=cc: e.matmul(out=PS[MODB][:, MODCOL:MODCOL + 1], lhsT=big[cc % 2][:, k, j * 128:(j + 1) * 128], rhs=sc_b[:, k:k + 1], start=(k == 0), stop=(k == 7)),
                      reads=bigkeys[cc % 2] + ["sc_b"], writes=[bk(MODB)], sig=(k == 7), keep_tag=True)
            fw.op("dve", lambda e, c=c: e.tensor_tensor(out=mod[:, c:c + 1], in0=PS[MODB][:, MODCOL:MODCOL + 1], in1=vecs[:, c:c + 1], op=ALU.add),
                  reads=[bk(MODB), "vecs"], writes=[("mod", c // 8)])
    fw.op("dve", lambda e: e.scalar_tensor_tensor(out=gs1, in0=mod[:, 8:16], scalar=1.0, in1=vecs[:, 48:56], op0=ALU.add, op1=ALU.mult),
          reads=[("mod", 1), "vecs"], writes=["gs1"])
    fw.op("dve", lambda e: e.tensor_scalar(out=nbgk, in0=vecs[:, 69:71], scalar1=-1.0, scalar2=None, op0=ALU.mult),
          reads=["vecs"], writes=["nbgk"])
    fw.op("act", lambda e: e.activation(out=es, in_=es, func=AF.Exp), reads=["es"], writes=["es"])
    wintm_v = wintm_d.rearrange("(k p) n -> p k n", p=128)
    fw.dma("pool", win_tm[:, :, 0:768], wintm_v[:, :, 0:768], writes=["win_tmA"])
    fw.dma("pool", win_tm[:, :, 768:1792], wintm_v[:, :, 768:1792], writes=["win_tmB"])
    fw.dma("pool", win_fm, winfm_d.rearrange("(k p) n -> p k n", p=128), writes=["win_fm"])
    for g in range(1, 4):
        fw.dma("sp", X1[:, 4 * g:4 * g + 4, :], x_v[:, 4 * g:4 * g + 4, :], reads=["win_tmA"], writes=[("X1", t) for t in range(4 * g, 4 * g + 4)])
    fw.op("dve", lambda e: e.memset(wgk2_pad, 0.0), writes=["wgk2"])
    fw.dma("pool", wgk2_pad[0:16, :], wgk2_d, reads=[], writes=["wgk2"])

    GB = (6, 7)

    def stream_M1():
        for c in range(16, 24):
            mod_chunk(c)
            yield
        fw.dma("pool", wout_sb, wout_d.rearrange("(k p) n -> p k n", p=128), writes=["wout"])
        yield
        for hh in range(2):
            for kk in range(4):
                k = hh * 4 + kk
                fw.op("dve", lambda e, k=k: e.tensor_scalar(out=yg[:, 0:256].bitcast(F32), in0=ident_f, scalar1=mod[:, 16 + k:17 + k], scalar2=None, op0=ALU.mult),
                      reads=["ident_f", ("mod", 2)], writes=["yg"])
                fw.op("pe", lambda e, k=k, hh=hh, kk=kk: e.matmul(out=PS[GB[hh]][:, kk * 128:(kk + 1) * 128], lhsT=ones_f, rhs=yg[:, 0:256].bitcast(F32), start=True, stop=True),
                      reads=["ones_f", "yg"], writes=[bk(GB[hh])])
            for k in range(8):
                fw.op("dve", lambda e, k=k, hh=hh: e.tensor_tensor(out=wout_sb[:, k, hh * 512:(hh + 1) * 512], in0=wout_sb[:, k, hh * 512:(hh + 1) * 512],
                                                               in1=PS[GB[hh]], op=ALU.mult),
                      reads=["wout", bk(GB[hh])], writes=["wout"])
            yield

    def stream_M2():
        for c in range(24, 48):
            mod_chunk(c)
            yield
        fw.op("dve", lambda e: e.scalar_tensor_tensor(out=gs2, in0=mod[:, 32:40], scalar=1.0, in1=vecs[:, 56:64], op0=ALU.add, op1=ALU.mult),
              reads=[("mod", 4), "vecs"], writes=["gs2"])
        yield

    fw.op("dve", lambda e: e.memset(qTA, 0.0), writes=["qTA0", "qTA1"])
    fw.op("dve", lambda e: e.memset(qTB, 0.0), writes=["qTB0", "qTB1"])
    fw.op("dve", lambda e: e.memset(vaug, 1.0), writes=[("vaug", t) for t in range(4)])
    fw.op("dve", lambda e: e.memset(qd_pad, 0.0), writes=["qd_pad"])
    fw.op("dve", lambda e: e.memset(lrT_pad, 0.0), writes=["lrT"])
    fw.op("dve", lambda e: e.memset(S, 0.0), writes=["S"])
    fw.op("dve", lambda e: e.memset(Sbf, 0.0), writes=["Sbf"])

    alt = [0]

    def evac_affine(out, in_, scale, bias, reads, writes, wtag=None, rtag=None):
        alt[0] ^= 1
        if alt[0]:
            if bias is None:
                fw.op("act", lambda e: e.activation(out=out, in_=in_, func=AF.Copy, scale=scale), reads=reads, writes=writes, wtag=wtag, rtag=rtag)
            else:
                fw.op("act", lambda e: e.activation(out=out, in_=in_, func=AF.Identity, scale=scale, bias=bias), reads=reads, writes=writes, wtag=wtag, rtag=rtag)
        else:
            if bias is None:
                fw.op("dve", lambda e: e.tensor_scalar(out=out, in0=in_, scalar1=scale, scalar2=None, op0=ALU.mult), reads=reads, writes=writes, wtag=wtag, rtag=rtag)
            else:
                fw.op("dve", lambda e: e.tensor_scalar(out=out, in0=in_, scalar1=scale, scalar2=bias, op0=ALU.mult, op1=ALU.add), reads=reads, writes=writes, wtag=wtag, rtag=rtag)

    def rstd_act(out, in_, n, extra_bias, rk_, wk_):
        fw.op("act", lambda e: e.activation(out=out, in_=in_, func=AF.Ln, scale=1.0 / n, bias=EPS), reads=rk_, writes=wk_)
        fw.op("act", lambda e: e.activation(out=out, in_=out, func=AF.Exp, scale=-0.5, bias=extra_bias), reads=wk_, writes=wk_)

    TB = 0

    def stage_T1(t):
        hs = t % HR
        xs = 0
        fw.op("act", lambda e: e.activation(out=sq_b, in_=X1[:, t, :], func=AF.Square, accum_out=ssx[:, t:t + 1]),
              reads=[("X1", t)], writes=["sqq", ("ssx", t)])
        rstd_act(rsx[:, t:t + 1], ssx[:, t:t + 1], D, 0.0, [("ssx", t)], [("rsx", t)])
        fw.op("dve", lambda e: e.tensor_scalar(out=xn[:, xs, :], in0=X1[:, t, :], scalar1=rsx[:, t:t + 1], scalar2=None, op0=ALU.mult),
              reads=[("X1", t), ("rsx", t)], writes=[("xn", xs)])
        yield
        T1B = (TB, 3)
        for k in range(8):
            fw.op("pe", lambda e, k=k: e.transpose(out=PSB[T1B[k // 4]][:, (k % 4) * 128:(k % 4 + 1) * 128], in_=xn[:, xs, k * 128:(k + 1) * 128], identity=ident_b),
                  reads=[("xn", xs), "ident_b"], writes=[bk(T1B[k // 4])], sig=(k % 4 == 3), wtag={bk(T1B[k // 4]): ("T1", t)})
        for k in range(4):
            fw.op("act", lambda e, k=k: e.activation(out=hT[:, k, hs * 128:(hs + 1) * 128], in_=PSB[TB][:, k * 128:(k + 1) * 128], func=AF.Identity,
                                                     scale=gs1[:, k:k + 1], bias=mod[:, k:k + 1]),
                  reads=[bk(TB), "gs1", ("mod", 0)], writes=[("hT", hs)], wtag={("hT", hs): t}, rtag={bk(TB): ("T1", t)})
        hv = hT[:, 4:8, hs * 128:(hs + 1) * 128]
        fw.op("dve", lambda e: e.tensor_tensor(out=hv, in0=PSB[3][:, 0:512].rearrange("p (k n) -> p k n", k=4),
                                               in1=gs1[:, 4:8].unsqueeze(2).to_broadcast([128, 4, 128]), op=ALU.mult),
              reads=[bk(3), "gs1"], writes=[("hT", hs)], wtag={("hT", hs): t}, rtag={bk(3): ("T1", t)})
        fw.op("dve", lambda e: e.tensor_tensor(out=hv, in0=hv, in1=mod[:, 4:8].unsqueeze(2).to_broadcast([128, 4, 128]), op=ALU.add),
              reads=[("hT", hs), ("mod", 0)], writes=[("hT", hs)], wtag={("hT", hs): t})
        yield

    QB, KVB, VGB, OGB = 1, 2, 6, 7

    def stage_T2(t):
        hs = t % HR
        for bank, c0, n in ((QB, 0, 512), (KVB, 512, 256)):
            for k in range(8):
                fw.op("pe", lambda e, bank=bank, c0=c0, n=n, k=k: e.matmul(
                    out=PS[bank][:, 0:n], lhsT=hT[:, k, hs * 128:(hs + 1) * 128], rhs=win_tm[:, k, c0:c0 + n],
                    start=(k == 0), stop=(k == 7)),
                    reads=[("hT", hs), "win_tmA"], writes=[bk(bank)], sig=(k == 7), rtag={("hT", hs): t}, wtag={bk(bank): ("proj", t)})
            yield

    def stage_T3(t):
        qs = t % 2
        vs = t % 4
        fw.op("act", lambda e: e.activation(out=sqq, in_=PS[QB], func=AF.Square), reads=[bk(QB)], writes=["sqq"], rtag={bk(QB): ("proj", t)})
        fw.op("dve", lambda e: e.tensor_reduce(out=ssq, in_=sqq.rearrange("p (h d) -> p h d", h=8), axis=AX.X, op=ALU.add),
              reads=["sqq"], writes=["ssq"])
        rstd_act(rq, ssq, 64, -LN8, ["ssq"], ["rq"])
        fw.op("dve", lambda e: e.tensor_tensor(out=qn.rearrange("p (h d) -> p h d", h=8), in0=PS[QB].rearrange("p (h d) -> p h d", h=8),
                                               in1=rq.unsqueeze(2).to_broadcast([128, 8, 64]), op=ALU.mult),
              reads=[bk(QB), "rq"], writes=["qn"], rtag={bk(QB): ("proj", t)})
        yield
        fw.op("act", lambda e: e.activation(out=sqq[:, 0:128], in_=PS[KVB][:, 0:128], func=AF.Square), reads=[bk(KVB)], writes=["sqq"])
        fw.op("dve", lambda e: e.tensor_reduce(out=ssk, in_=sqq[:, 0:128].rearrange("p (h d) -> p h d", h=2), axis=AX.X, op=ALU.add),
              reads=["sqq"], writes=["ssk"])
        rstd_act(rk, ssk, 64, 0.0, ["ssk"], ["rk"])
        fw.op("dve", lambda e: e.tensor_tensor(out=kn.rearrange("p (h d) -> p h d", h=2), in0=PS[KVB][:, 0:128].rearrange("p (h d) -> p h d", h=2),
                                               in1=rk.unsqueeze(2).to_broadcast([128, 2, 64]), op=ALU.mult),
              reads=[bk(KVB), "rk"], writes=["kn"], rtag={bk(KVB): ("proj", t)})
        yield
        if 'T3v' in DBG_SKIP:
            return
        fw.op("act", lambda e: e.activation(out=vaug[:, t % 4, :, 0:64], in_=PS[KVB][:, 128:256].rearrange("p (h d) -> p h d", h=2), func=AF.Copy),
              reads=[bk(KVB)], writes=[("vaug", t % 4)])
        if 'T3t' in DBG_SKIP:
            return
        for i in range(4):
            fw.op("pe", lambda e, i=i: e.transpose(out=PSB[TB][:, i * 128:(i + 1) * 128], in_=qn[:, i * 128:(i + 1) * 128], identity=ident_b),
                  reads=["qn", "ident_b"], writes=[bk(TB)], sig=False)
        fw.op("pe", lambda e: e.transpose(out=PSB[TB][:, 512:640], in_=kn, identity=ident_b),
              reads=["kn", "ident_b"], writes=[bk(TB)], sig=True, wtag={bk(TB): ("T3", t)})
        if 'T3e' in DBG_SKIP:
            return
        gq = vecs[:, 71:72]
        gk = vecs[:, 72:73]
        fw.op("act", lambda e: e.activation(out=qTA[0:64, qs, :], in_=PSB[TB][0:64, 0:512], func=AF.Copy, scale=gq[0:64, :]),
              reads=[bk(TB), "vecs"], writes=["qTA%d" % qs])
        fw.op("dve", lambda e: e.tensor_scalar(out=qTB[64:128, qs, :], in0=PSB[TB][64:128, 0:512], scalar1=gq[64:128, :], scalar2=None, op0=ALU.mult),
              reads=[bk(TB), "vecs"], writes=["qTB%d" % qs])
        fw.op("dve", lambda e: e.tensor_scalar(out=kT[:, (t % 4) * 128:(t % 4 + 1) * 128], in0=PSB[TB][:, 512:640], scalar1=gk, scalar2=None, op0=ALU.mult),
              reads=[bk(TB), "vecs"], writes=[("kT", t % 4)], rtag={bk(TB): ("T3", t)})
        yield

    SB = DBG_SB
    OB = DBG_OB

    def stage_T4(t):
        qs = t % 2
        ys = t % 8
        blocks = [(t, 0)] + ([(t - 1, 1)] if t > 0 else [])
        si = 0
        allpts = []
        for kv in range(2):
            qT = qTA if kv == 0 else qTB
            qkey = ("qTA%d" if kv == 0 else "qTB%d") % qs
            pts = []
            for (blk, which) in blocks:
                bank = SB[si % 2]
                pslot = si % 4
                si += 1
                fw.op("pe", lambda e, bank=bank, blk=blk, qT=qT: e.matmul(out=PS[bank], lhsT=kT[:, (blk % 4) * 128:(blk % 4 + 1) * 128], rhs=qT[:, qs, :], start=True, stop=False),
                      reads=[("kT", blk % 4), qkey], writes=[bk(bank)], sig=False)
                fw.op("pe", lambda e, bank=bank, which=which, kv=kv: e.matmul(out=PS[bank], lhsT=ident_b, rhs=abias[:, kv * 2 + which, :], start=False, stop=True),
                      reads=["ident_b", "abias"], writes=[bk(bank)], sig=True)
                fw.op("act", lambda e, bank=bank, pslot=pslot: e.activation(out=PT[:, pslot, :], in_=PS[bank], func=AF.Exp),
                      reads=[bk(bank)], writes=[("PT", pslot)])
                pts.append((pslot, blk))
                yield
            allpts.append(pts)
        for kv in range(2):
            pts = allpts[kv]
            for g in range(4):
                h = kv * 4 + g
                ob = OB[kv]
                for pi, (pslot, blk) in enumerate(pts):
                    fw.op("pe", lambda e, ob=ob, g=g, pslot=pslot, blk=blk, pi=pi, kv=kv, pts=pts: e.matmul(
                        out=PS[ob][:, g * 65:(g + 1) * 65], lhsT=PT[:, pslot, g * 128:(g + 1) * 128], rhs=vaug[:, blk % 4, kv, :],
                        start=(pi == 0), stop=(pi == len(pts) - 1)),
                        reads=[("PT", pslot), ("vaug", blk % 4)], writes=[bk(ob)], sig=(g == 3 and pi == len(pts) - 1))
            yield
        for kv in range(2):
            ov = PS[OB[kv]][:, 0:260].rearrange("p (h d) -> p h d", h=4)
            fw.op("dve", lambda e, ov=ov, kv=kv: e.tensor_tensor(out=den[:, kv * 4:(kv + 1) * 4], in0=ov[:, :, 64], in1=es[:, kv * 4:(kv + 1) * 4], op=ALU.add),
                  reads=[bk(OB[kv]), "es"], writes=["den"])
        fw.op("dve", lambda e: e.reciprocal(out=den, in_=den), reads=["den"], writes=["den"])
        yield
        for kv in range(2):
            ov = PS[OB[kv]][:, 0:260].rearrange("p (h d) -> p h d", h=4)
            fw.op("dve", lambda e, ov=ov, kv=kv: e.tensor_tensor(
                out=on[:, kv * 256:(kv + 1) * 256].rearrange("p (h d) -> p h d", h=4), in0=ov[:, :, 0:64],
                in1=den[:, kv * 4:(kv + 1) * 4].unsqueeze(2).to_broadcast([128, 4, 64]), op=ALU.mult),
                reads=[bk(OB[kv]), "den"], writes=["on"])
        yield
        fw.op("act", lambda e: e.activation(out=sqq, in_=on, func=AF.Square, accum_out=ssa), reads=["on"], writes=["sqq", "ssa"])
        rstd_act(ra, ssa, 512, 0.0, ["ssa"], ["ra"])
        fw.op("dve", lambda e: e.tensor_scalar(out=ya, in0=on, scalar1=ra, scalar2=None, op0=ALU.mult), reads=["on", "ra"], writes=["ya"])
        yield
        for i in range(4):
            fw.op("pe", lambda e, i=i: e.transpose(out=PSB[TB][:, i * 128:(i + 1) * 128], in_=ya[:, i * 128:(i + 1) * 128], identity=ident_b),
                  reads=["ya", "ident_b"], writes=[bk(TB)], sig=(i == 3), wtag={bk(TB): ("T4", t)})
        for i in range(4):
            evac_affine(yA[:, ys, i, :], PSB[TB][:, i * 128:(i + 1) * 128], vecs[:, 64 + i:65 + i], None, [bk(TB), "vecs"], [("yA", ys)], wtag={("yA", ys): t}, rtag={bk(TB): ("T4", t)})
        yield

    FB = (6, 7)

    def stage_G1(T0, GT):
        hcol = (T0 % HR) * 128
        NG_ = GT * 128
        hkeys = [("hT", (T0 + i) % HR) for i in range(GT)]

        def fm_proj(m, bank):
            for k in range(8):
                fw.op("pe", lambda e, k=k: e.matmul(out=PS[bank][:, 0:NG_], lhsT=win_fm[:, k, m * 128:(m + 1) * 128], rhs=hT[:, k, hcol:hcol + NG_],
                                                    start=(k == 0), stop=(k == 7)),
                      reads=hkeys + ["win_fm"], writes=[bk(bank)], sig=(k == 7), rtag={("hT", (T0 + i) % HR): T0 + i for i in range(GT)})
        fm_proj(4, FB[0])
        fw.op("act", lambda e: e.activation(out=lrT_pad[0:16, 0:NG_], in_=PS[FB[0]][0:16, 0:NG_], func=AF.Copy), reads=[bk(FB[0])], writes=["lrT"])
        yield
        for c in range(2):
            bank = FB[(c + 1) % 2]
            fw.op("pe", lambda e, c=c, bank=bank: e.matmul(out=PS[bank][:, 0:NG_], lhsT=wgk2_pad[:, c * 128:(c + 1) * 128], rhs=lrT_pad[:, 0:NG_], start=True, stop=True),
                  reads=["wgk2", "lrT"], writes=[bk(bank)])
            fw.op("act", lambda e, c=c, bank=bank: e.activation(out=lbuf[:, c, 0:NG_], in_=PS[bank][:, 0:NG_], func=AF.Exp, scale=-1.0, bias=nbgk[:, c:c + 1]),
                  reads=[bk(bank), "nbgk"], writes=[("lbuf", c)])
            fw.op("act", lambda e, c=c: e.activation(out=lbuf[:, c, 0:NG_], in_=lbuf[:, c, 0:NG_], func=AF.Ln, bias=1.0), reads=[("lbuf", c)], writes=[("lbuf", c)])
            yield
            for j in range(GT):
                fw.op("dve", lambda e, c=c, j=j: e.tensor_tensor_scan(out=Bc[:, c, j * 128:(j + 1) * 128], data0=ones_f, data1=lbuf[:, c, j * 128:(j + 1) * 128],
                                                                      initial=0.0, op0=ALU.mult, op1=ALU.add),
                      reads=[("lbuf", c), "ones_f"], writes=[("Bc", c)])
            yield
            fw.op("act", lambda e, c=c: e.activation(out=lbuf[:, c, 0:NG_], in_=Bc[:, c, 0:NG_], func=AF.Exp, scale=-1.0 / 16, bias=-LN8), reads=[("Bc", c)], writes=[("lbuf", c)])
            fw.op("act", lambda e, c=c: e.activation(out=elast[:, c, 0:GT], in_=Bc[:, c, 0:NG_].rearrange("p (j i) -> p j i", j=GT)[:, :, 127], func=AF.Exp, scale=-1.0 / 16),
                  reads=[("Bc", c)], writes=[("elast", c)])
            fw.op("act", lambda e, c=c: e.activation(out=Bc[:, c, 0:NG_], in_=Bc[:, c, 0:NG_], func=AF.Exp, scale=1.0 / 16), reads=[("Bc", c)], writes=[("Bc", c)])
            yield
        for c in range(2):
            bq = FB[0]
            fm_proj(c, bq)
            for hh in range(2):
                fw.op("dve", lambda e, c=c, hh=hh: e.tensor_tensor(out=qd_pad[hh * 64:(hh + 1) * 64, 2 * c + hh, 0:NG_], in0=PS[bq][hh * 64:(hh + 1) * 64, 0:NG_],
                                                                   in1=lbuf[hh * 64:(hh + 1) * 64, c, 0:NG_], op=ALU.mult),
                      reads=[bk(bq), ("lbuf", c)], writes=["qd_pad"])
            yield
            bkk = FB[1]
            fm_proj(2 + c, bkk)
            fw.op("dve", lambda e, c=c: e.tensor_tensor(out=kdT[:, c, 0:NG_], in0=PS[bkk][:, 0:NG_], in1=Bc[:, c, 0:NG_], op=ALU.mult),
                  reads=[bk(bkk), ("Bc", c)], writes=[("kdT", c)])
            yield
            for j in range(GT):
                fw.op("dve", lambda e, c=c, j=j: e.tensor_scalar(out=kdecT[:, c, j * 128:(j + 1) * 128], in0=kdT[:, c, j * 128:(j + 1) * 128],
                                                                 scalar1=elast[:, c, j:j + 1], scalar2=None, op0=ALU.mult),
                      reads=[("kdT", c), ("elast", c)], writes=[("kdecT", c)])
            yield
        for j in range(GT):
            for c in range(2):
                fw.op("pe", lambda e, c=c, j=j: e.transpose(out=PSB[6][:, (j * 2 + c) * 128:(j * 2 + c + 1) * 128], in_=kdecT[:, c, j * 128:(j + 1) * 128], identity=ident_b),
                      reads=[("kdecT", c), "ident_b"], writes=[bk(6)], sig=(j == GT - 1 and c == 1))
        fw.op("act", lambda e: e.activation(out=kdec[:, 0:GT, :].rearrange("p j c -> p (j c)"), in_=PSB[6][:, 0:GT * 256], func=AF.Copy), reads=[bk(6)], writes=["kdec"])
        yield

    def stage_B0(t):
        hs = t % HR
        vs = t % 2
        for bank, c0, n in ((VGB, 768, 512), (OGB, 1280, 512)):
            for k in range(8):
                fw.op("pe", lambda e, bank=bank, c0=c0, n=n, k=k: e.matmul(
                    out=PS[bank][:, 0:n], lhsT=hT[:, k, hs * 128:(hs + 1) * 128], rhs=win_tm[:, k, c0:c0 + n],
                    start=(k == 0), stop=(k == 7)),
                    reads=[("hT", hs), "win_tmB"], writes=[bk(bank)], sig=(k == 7), rtag={("hT", hs): t})
            yield
        fw.op("act", lambda e: e.activation(out=vg[:, vs, :], in_=PS[VGB], func=AF.Copy), reads=[bk(VGB)], writes=[("vg", vs)])
        yield
        fw.op("act", lambda e: e.activation(out=esc, in_=PS[OGB], func=AF.Exp, scale=-1.0), reads=[bk(OGB)], writes=["t1"])
        fw.op("act", lambda e: e.activation(out=esc, in_=esc, func=AF.Ln, bias=1.0), reads=["t1"], writes=["t1"])
        fw.op("act", lambda e: e.activation(out=esc, in_=esc, func=AF.Exp, scale=-1.0), reads=["t1"], writes=["t1"])
        fw.op("dve", lambda e: e.tensor_tensor(out=sog[:, vs, :], in0=PS[OGB], in1=esc, op=ALU.mult), reads=[bk(OGB), "t1"], writes=[("sog", vs)])
        yield

    AB, OGB2, UB = 6, 7, 6

    def stage_G2(t, j):
        vs = t % 2
        ys = t % 8
        for h in range(4):
            c = h // 2
            fw.op("pe", lambda e, h=h, c=c: e.matmul(out=PS[AB][:, h * 128:(h + 1) * 128], lhsT=kdT[:, c, j * 128:(j + 1) * 128],
                                                     rhs=qd_pad[:, h, j * 128:(j + 1) * 128], start=True, stop=True),
                  reads=[("kdT", c), "qd_pad"], writes=[bk(AB)], sig=(h == 3))
        yield
        fw.op("dve", lambda e: e.tensor_tensor(out=AT, in0=PS[AB].rearrange("p (h i) -> p h i", h=4), in1=gmask.unsqueeze(1).to_broadcast([128, 4, 128]), op=ALU.mult),
              reads=[bk(AB), "gmask"], writes=["AT"])
        yield
        for h in range(4):
            c = h // 2
            fw.op("pe", lambda e, h=h: e.matmul(out=PS[OGB2][:, h * 128:(h + 1) * 128], lhsT=AT[:, h, :], rhs=vg[:, vs, h * 128:(h + 1) * 128], start=True, stop=False),
                  reads=["AT", ("vg", vs)], writes=[bk(OGB2)], sig=False)
            fw.op("pe", lambda e, h=h, c=c: e.matmul(out=PS[OGB2][:, h * 128:(h + 1) * 128], lhsT=qd_pad[:, h, j * 128:(j + 1) * 128], rhs=Sbf[:, c, :], start=False, stop=True),
                  reads=["qd_pad", "Sbf"], writes=[bk(OGB2)], sig=(h == 3))
        yield
        for c in range(2):
            fw.op("pe", lambda e, c=c: e.matmul(out=PS[UB][:, c * 256:(c + 1) * 256], lhsT=kdec[:, j, c * 128:(c + 1) * 128], rhs=vg[:, vs, c * 256:(c + 1) * 256], start=True, stop=True),
                  reads=["kdec", ("vg", vs)], writes=[bk(UB)], sig=(c == 1))
        yield
        for c in range(2):
            for hh in range(2):
                fw.op("dve", lambda e, c=c, hh=hh: e.scalar_tensor_tensor(
                    out=S[hh * 64:(hh + 1) * 64, c, :], in0=S[hh * 64:(hh + 1) * 64, c, :], scalar=elast[hh * 64:(hh + 1) * 64, c, j:j + 1],
                    in1=PS[UB][hh * 64:(hh + 1) * 64, c * 256 + hh * 128:c * 256 + (hh + 1) * 128], op0=ALU.mult, op1=ALU.add),
                    reads=["S", ("elast", c), bk(UB)], writes=["S"])
        fw.op("act", lambda e: e.activation(out=Sbf, in_=S, func=AF.Copy), reads=["S"], writes=["Sbf"])
        yield
        fw.op("act", lambda e: e.activation(out=sqB, in_=PS[OGB2], func=AF.Square), reads=[bk(OGB2)], writes=["sqB"])
        fw.op("dve", lambda e: e.tensor_reduce(out=ssg, in_=sqB.rearrange("p (h d) -> p h d", h=4), axis=AX.X, op=ALU.add), reads=["sqB"], writes=["ssg"])
        rstd_act(rg, ssg, 128, 0.0, ["ssg"], ["rg"])
        fw.op("dve", lambda e: e.tensor_tensor(out=t1.rearrange("p (h d) -> p h d", h=4), in0=PS[OGB2].rearrange("p (h d) -> p h d", h=4),
                                               in1=rg.unsqueeze(2).to_broadcast([128, 4, 128]), op=ALU.mult),
              reads=[bk(OGB2), "rg"], writes=["t1"])
        yield
        fw.op("dve", lambda e: e.tensor_tensor(out=yg, in0=t1, in1=sog[:, vs, :], op=ALU.mult), reads=["t1", ("sog", vs)], writes=["yg"])
        yield
        for i in range(4):
            fw.op("pe", lambda e, i=i: e.transpose(out=PSB[6][:, i * 128:(i + 1) * 128], in_=yg[:, i * 128:(i + 1) * 128], identity=ident_b),
                  reads=["yg", "ident_b"], writes=[bk(6)], sig=(i == 3))
        yield
        fw.op("act", lambda e: e.activation(out=yG.rearrange("p a b -> p (a b)"), in_=PSB[6][:, 0:512], func=AF.Copy, scale=vecs[:, 68:69]),
              reads=[bk(6), "vecs"], writes=["yG"], wtag={"yG": t})
        yield

    WB = (6, 7)

    def stage_W(t):
        ys = t % 8
        for dh in range(2):
            for k in range(8):
                fw.op("pe", lambda e, dh=dh, k=k: e.matmul(out=PS[WB[dh]], lhsT=(yA[:, ys, k, :] if k < 4 else yG[:, k - 4, :]), rhs=wout_sb[:, k, dh * 512:(dh + 1) * 512], start=(k == 0), stop=(k == 7)),
                      reads=[("yA", ys), "yG", "wout"], writes=[bk(WB[dh])], sig=(k == 7), rtag={("yA", ys): t, "yG": t})
            fw.op("dve", lambda e, dh=dh: e.tensor_tensor(out=X1[:, t, dh * 512:(dh + 1) * 512], in0=PS[WB[dh]], in1=X1[:, t, dh * 512:(dh + 1) * 512], op=ALU.add),
                  reads=[bk(WB[dh]), ("X1", t)], writes=[("X1", t)])
            yield

    def chain(*gens):
        for g_ in gens:
            yield from g_

    def run_streams(streams):
        streams = [s for s in streams if s is not None]
        if SEQ_STREAMS:
            for s in streams:
                for _ in s:
                    pass
            return
        if not GREEDY:
            while streams:
                nxt = []
                for s in streams:
                    try:
                        next(s)
                        nxt.append(s)
                    except StopIteration:
                        pass
                streams = nxt
            return
        ready = [0.0] * len(streams)
        alive = list(range(len(streams)))
        while alive:
            i = min(alive, key=lambda j: ready[j])
            fw.cur_stream_end = 0.0
            try:
                next(streams[i])
                ready[i] = max(ready[i], fw.cur_stream_end)
            except StopIteration:
                alive.remove(i)

    def interleave(a, b):
        gens = [a, b]
        while gens:
            nxt = []
            for g_ in gens:
                try:
                    next(g_)
                    nxt.append(g_)
                    yield
                except StopIteration:
                    pass
            gens = nxt

    def stream_A(g):
        ts = list(range(GROUPS[g][0], GROUPS[g][0] + GROUPS[g][1]))
        parts = [stage_T1(ts[0]), stage_T2(ts[0])]
        for i, t in enumerate(ts):
            parts.append(stage_T3(t))
            if i + 1 < len(ts) and PIPE_A:
                parts.append(interleave(stage_T4(t), chain(stage_T1(ts[i + 1]), stage_T2(ts[i + 1]))))
            else:
                parts.append(stage_T4(t))
                if i + 1 < len(ts):
                    parts.append(stage_T1(ts[i + 1]))
                    parts.append(stage_T2(ts[i + 1]))
        return chain(*parts)

    def stream_B(g):
        T0, n_ = GROUPS[g]
        return chain(stage_G1(T0, n_), *[chain(stage_B0(t), stage_G2(t, t - T0), stage_W(t)) for t in range(T0, T0 + n_)])

    def early_exit():
        fw.barrier()
        for g_ in range(4):
            fw.dma("sp", out_v[:, 4 * g_:4 * g_ + 4, :], X1[:, 4 * g_:4 * g_ + 4, :], reads=[("X1", t) for t in range(4 * g_, 4 * g_ + 4)], writes=[("out", g_)])
        fw.barrier()
        return nc, fw, None
    if DBG_STAGE == 10:
        return early_exit()
    run_streams([stream_A(0) if 'A0' not in DBG_SKIP else None, stream_M1() if 'M1' not in DBG_SKIP else None])
    if DBG_STAGE == 11:
        return early_exit()
    m2 = stream_M2()
    NGRP = len(GROUPS)

    class GStream:
        def __init__(self, make, first, n):
            self.make = make
            self.cur = first
            self.n = n
            self.gen = None
            self.ready = 0.0

        def finished(self):
            return self.cur >= self.n and self.gen is None

        def at_boundary(self):
            return self.gen is None

        def step(self):
            if self.gen is None:
                self.gen = self.make(self.cur)
            fw.cur_stream_end = 0.0
            try:
                next(self.gen)
                self.ready = max(self.ready, fw.cur_stream_end)
            except StopIteration:
                self.gen = None
                self.cur += 1

    class PlainStream(GStream):
        def __init__(self, gen):
            self.gen = gen
            self.ready = 0.0
            self.done = False

        def finished(self):
            return self.done

        def at_boundary(self):
            return False

        def step(self):
            fw.cur_stream_end = 0.0
            try:
                next(self.gen)
                self.ready = max(self.ready, fw.cur_stream_end)
            except StopIteration:
                self.done = True

    STA = GStream(stream_A, 1, NGRP)
    STB = GStream(stream_B, 0, NGRP)
    STM = PlainStream(m2) if 'M2' not in DBG_SKIP else None
    while True:
        cands = []
        if not STA.finished() and not (STA.at_boundary() and (STA.cur >= NGRP or STA.cur - STB.cur > LEAD)):
            cands.append(STA)
        if not STB.finished() and not (STB.at_boundary() and not (STB.cur < STA.cur)):
            cands.append(STB)
        if STM is not None and not STM.finished():
            cands.append(STM)
        if not cands:
            assert STA.finished() and STB.finished(), (STA.cur, STB.cur)
            break
        s_ = min(cands, key=lambda s: s.ready - (PRIO_B if s is STB else 0.0)) if GREEDY else cands[0]
        s_.step()

    if stage == 1:
        fw.barrier()
        for g in range(4):
            fw.dma("sp", out_v[:, 4 * g:4 * g + 4, :], X1[:, 4 * g:4 * g + 4, :], reads=[("X1", t) for t in range(4 * g, 4 * g + 4)], writes=[("out", g)])
        fw.barrier()
        return nc, fw, None

    fw.barrier()
    reg.reset()
    h2T = reg.get([8, T], BF16)
    wb = [dict(w1=reg.get([EB, 8, 256], BF16), w3=reg.get([EB, 8, 256], BF16), w2=reg.get([EB, 2, 1024], BF16)) for _ in range(2)]
    xn2 = reg.get([2, 1024], F32)
    xT32 = reg.get([2, 1024], F32)
    wr_sb = reg.get([8, 20], F32)
    shrep = reg.get([128], F32)
    cb_sb = reg.get([20], F32)
    L = reg.get([NT, 20], F32)
    g2b = reg.get([1024], F32)
    comb_pad = reg.get([NT, 128], BF16)
    cT_sb = reg.get([T], BF16)
    Cb = reg.get([2, 512], BF16)
    s_sb = reg.get([2, 512], BF16)
    gc_sb = reg.get([2, 512], BF16)
    hid = reg.get([2, EB * 2, 512], BF16)
    sq2 = reg.get([1024], BF16)
    gmax = reg.get([NT], F32)
    goh = reg.get([NT, 4], F32)
    gex = reg.get([NT, 4], F32)
    pg = reg.get([NT], F32)
    tmp16 = reg.get([NT, 16], F32)
    esel = reg.get([NT, 4], F32)
    esel2 = reg.get([NT, 4], F32)
    m1 = reg.get([NT], F32)
    m2 = reg.get([NT], F32)
    oh1 = reg.get([NT, 4], F32)
    oh2 = reg.get([NT, 4], F32)
    rr = reg.get([NT], F32)
    wa = reg.get([NT], F32)
    wb2 = reg.get([NT], F32)
    wsel = reg.get([NT, 4], F32)
    print("phase2 region bytes", reg.off, "of", reg.n)

    def load_expert_batch(bi):
        buf = wb[bi % 2]
        for j in range(EB):
            e_ = bi * EB + j
            fw.dma("pool", buf["w1"][:, j, :, :], w1_d[e_].rearrange("(k p) f -> p k f", p=128), writes=[("w1", bi % 2, j)])
            fw.dma("pool", buf["w3"][:, j, :, :], w3_d[e_].rearrange("(k p) f -> p k f", p=128), writes=[("w3", bi % 2, j)])
            fw.dma("pool", buf["w2"][:, j, :, :], w2_d[e_].rearrange("(k p) d -> p k d", p=128), writes=[("w2", bi % 2, j)])

    fw.dma("sp", wr_sb, wr_d.rearrange("(k p) n -> p k n", p=128), writes=["wr"])
    load_expert_batch(0)
    load_expert_batch(1)

    for k in range(8):
        fw.op("dve", lambda e, k=k: e.tensor_scalar(out=shrep, in0=ident_f, scalar1=mod[:, 40 + k:41 + k], scalar2=None, op0=ALU.mult),
              reads=["ident_f", ("mod", 5)], writes=["shrep"])
        fw.op("pe", lambda e, k=k: e.matmul(out=PS[k // 4][:, (k % 4) * 128:(k % 4 + 1) * 128], lhsT=ones_f, rhs=shrep, start=True, stop=True),
              reads=["ones_f", "shrep"], writes=[bk(k // 4)])
    for hh in range(2):
        fw.op("act", lambda e, hh=hh: e.activation(out=g2b[:, hh * 512:(hh + 1) * 512], in_=PS[hh], func=AF.Copy), reads=[bk(hh)], writes=["g2b"])
    for k in range(8):
        fw.op("dve", lambda e, k=k: e.tensor_copy(out=shrep, in_=mod[:, 24 + k:25 + k].to_broadcast([128, 128])),
              reads=[("mod", 3)], writes=["shrep"])
        fw.op("pe", lambda e, k=k: e.matmul(out=PS[2][:, 0:20], lhsT=shrep, rhs=wr_sb[:, k, :], start=(k == 0), stop=(k == 7)),
              reads=["shrep", "wr"], writes=[bk(2)])
    fw.op("dve", lambda e: e.tensor_tensor(out=cb_sb, in0=PS[2][:, 0:20], in1=brg, op=ALU.add), reads=[bk(2), "brg"], writes=["cb"])
    for k in range(8):
        fw.op("dve", lambda e, k=k: e.tensor_scalar(out=wr_sb[:, k, :], in0=wr_sb[:, k, :], scalar1=gs2[:, k:k + 1], scalar2=None, op0=ALU.mult),
              reads=["wr", "gs2", bk(2)], writes=["wr"])
    fw.op("dve", lambda e: e.memset(comb_pad, 0.0), writes=["comb_pad"])
    fw.op("dve", lambda e: e.memset(cT_sb, 0.0), writes=["cT"])

    def stage_N(t):
        p_ = t % 2
        xs = p_
        b0_, b1_, rb = 2 * p_, 2 * p_ + 1, 4 + p_
        tb = (b0_, b1_)
        fw.op("act", lambda e: e.activation(out=sq2, in_=X1[:, t, :], func=AF.Square, accum_out=ssx[:, t:t + 1]),
              reads=[("X1", t)], writes=["sq2", ("ssx", t)])
        rstd_act(rsx[:, t:t + 1], ssx[:, t:t + 1], D, 0.0, [("ssx", t)], [("rsx", t)])
        fw.op("dve", lambda e: e.tensor_scalar(out=xn2[:, xs, :], in0=X1[:, t, :], scalar1=rsx[:, t:t + 1], scalar2=None, op0=ALU.mult),
              reads=[("X1", t), ("rsx", t)], writes=[("xn2", xs)])
        yield
        for k in range(8):
            fw.op("pe", lambda e, k=k: e.transpose(out=PS[tb[k // 4]][:, (k % 4) * 128:(k % 4 + 1) * 128], in_=xn2[:, xs, k * 128:(k + 1) * 128], identity=ident_f),
                  reads=[("xn2", xs), "ident_f"], writes=[bk(tb[k // 4])], sig=(k % 4 == 3))
        yield
        for hh in range(2):
            fw.op("dve", lambda e, hh=hh: e.tensor_copy(out=xT32[:, xs, hh * 512:(hh + 1) * 512], in_=PS[tb[hh]]), reads=[bk(tb[hh])], writes=[("xT32", xs)])
        yield
        for k in range(8):
            evac_affine(h2T[:, k, t * 128:(t + 1) * 128], PS[tb[k // 4]][:, (k % 4) * 128:(k % 4 + 1) * 128], gs2[:, k:k + 1], mod[:, 24 + k:25 + k],
                        [bk(tb[k // 4]), "gs2", ("mod", 3)], [("h2T", t // 4)])
        yield
        for k in range(8):
            fw.op("pe", lambda e, k=k: e.matmul(out=PS[rb][:, 0:20], lhsT=xT32[:, xs, k * 128:(k + 1) * 128], rhs=wr_sb[:, k, :], start=(k == 0), stop=(k == 7)),
                  reads=[("xT32", xs), "wr"], writes=[bk(rb)], sig=(k == 7))
        yield
        fw.op("dve", lambda e: e.tensor_tensor(out=L[:, t, :], in0=PS[rb][:, 0:20], in1=cb_sb, op=ALU.add), reads=[bk(rb), "cb"], writes=["L"])
        yield

    run_streams([chain(*[stage_N(t) for t in range(0, NT, 2)]), chain(*[stage_N(t) for t in range(1, NT, 2)])])

    def dv(fn, reads, writes):
        fw.op("dve", fn, reads=reads, writes=writes)
    gl = L[:, :, 0:4]
    el = L[:, :, 4:20].rearrange("p t (g i) -> p t g i", g=4)
    dv(lambda e: e.tensor_reduce(out=gmax, in_=gl, axis=AX.X, op=ALU.max), ["L"], ["gmax"])
    dv(lambda e: e.tensor_tensor(out=goh, in0=gl, in1=gmax.unsqueeze(2).to_broadcast([128, NT, 4]), op=ALU.is_equal), ["L", "gmax"], ["goh"])
    dv(lambda e: e.tensor_tensor(out=gex, in0=gl, in1=gmax.unsqueeze(2).to_broadcast([128, NT, 4]), op=ALU.subtract), ["L", "gmax"], ["gex"])
    fw.op("act", lambda e: e.activation(out=gex, in_=gex, func=AF.Exp), reads=["gex"], writes=["gex"])
    dv(lambda e: e.tensor_reduce(out=pg, in_=gex, axis=AX.X, op=ALU.add), ["gex"], ["pg"])
    dv(lambda e: e.reciprocal(out=pg, in_=pg), ["pg"], ["pg"])
    dv(lambda e: e.tensor_tensor(out=tmp16.rearrange("p t (g i) -> p t g i", g=4), in0=el, in1=goh.unsqueeze(3).to_broadcast([128, NT, 4, 4]), op=ALU.mult),
       ["L", "goh"], ["tmp16"])
    dv(lambda e: e.tensor_reduce(out=esel, in_=tmp16.rearrange("p t (g i) -> p t i g", g=4), axis=AX.X, op=ALU.add), ["tmp16"], ["esel"])
    dv(lambda e: e.tensor_reduce(out=m1, in_=esel, axis=AX.X, op=ALU.max), ["esel"], ["m1"])
    dv(lambda e: e.tensor_tensor(out=oh1, in0=esel, in1=m1.unsqueeze(2).to_broadcast([128, NT, 4]), op=ALU.is_equal), ["esel", "m1"], ["oh1"])
    dv(lambda e: e.scalar_tensor_tensor(out=esel2, in0=oh1, scalar=-1e30, in1=esel, op0=ALU.mult, op1=ALU.add), ["oh1", "esel"], ["esel2"])
    dv(lambda e: e.tensor_reduce(out=m2, in_=esel2, axis=AX.X, op=ALU.max), ["esel2"], ["m2"])
    dv(lambda e: e.tensor_tensor(out=oh2, in0=esel2, in1=m2.unsqueeze(2).to_broadcast([128, NT, 4]), op=ALU.is_equal), ["esel2", "m2"], ["oh2"])
    dv(lambda e: e.tensor_tensor(out=rr, in0=m2, in1=m1, op=ALU.subtract), ["m1", "m2"], ["rr"])
    fw.op("act", lambda e: e.activation(out=rr, in_=rr, func=AF.Exp), reads=["rr"], writes=["rr"])
    dv(lambda e: e.tensor_scalar_add(out=wa, in0=rr, scalar1=1.0), ["rr"], ["wa"])
    dv(lambda e: e.reciprocal(out=wa, in_=wa), ["wa"], ["wa"])
    dv(lambda e: e.tensor_tensor(out=wa, in0=wa, in1=pg, op=ALU.mult), ["wa", "pg"], ["wa"])
    dv(lambda e: e.tensor_tensor(out=wb2, in0=wa, in1=rr, op=ALU.mult), ["wa", "rr"], ["wb2"])
    dv(lambda e: e.tensor_tensor(out=wsel, in0=oh1, in1=wa.unsqueeze(2).to_broadcast([128, NT, 4]), op=ALU.mult), ["oh1", "wa"], ["wsel"])
    dv(lambda e: e.tensor_tensor(out=oh2, in0=oh2, in1=wb2.unsqueeze(2).to_broadcast([128, NT, 4]), op=ALU.mult), ["oh2", "wb2"], ["oh2"])
    dv(lambda e: e.tensor_tensor(out=wsel, in0=wsel, in1=oh2, op=ALU.add), ["wsel", "oh2"], ["wsel"])
    dv(lambda e: e.tensor_tensor(out=comb_pad[:, :, 0:16].rearrange("p t (g i) -> p t g i", g=4), in0=goh.unsqueeze(3).to_broadcast([128, NT, 4, 4]),
                                 in1=wsel.unsqueeze(2).to_broadcast([128, NT, 4, 4]), op=ALU.mult), ["goh", "wsel"], ["comb_pad"])
    for half in range(2):
        for i in range(8):
            t = half * 8 + i
            fw.op("pe", lambda e, t=t, i=i: e.transpose(out=PSB[3][:, i * 128:(i + 1) * 128], in_=comb_pad[:, t, :], identity=ident_b),
                  reads=["comb_pad", "ident_b"], writes=[bk(3)], sig=(i == 7))
        fw.op("act", lambda e, half=half: e.activation(out=cT_sb[0:16, half * 1024:(half + 1) * 1024], in_=PSB[3][0:16, :], func=AF.Copy), reads=[bk(3)], writes=["cT"])

    NB = NEXP // EB
    AG = (0, 1, 2, 3)
    CBK = 4
    YB = (5, 6, 7)
    yi = [0]
    ui = [0]
    for bi in range(NB):
        buf = wb[bi % 2]
        for j in range(EB):
            for fc in range(2):
                fw.op("dve", lambda e, j=j, fc=fc: e.tensor_tensor(out=buf["w2"][:, j, fc, :], in0=buf["w2"][:, j, fc, :], in1=g2b, op=ALU.mult),
                      reads=[("w2", bi % 2, j), "g2b"], writes=[("w2", bi % 2, j)])
        for tg in range(4):
            hs = tg % 2
            for j in range(EB):
                e_ = bi * EB + j
                fw.op("pe", lambda e, e_=e_, tg=tg: e.matmul(out=PS[CBK], lhsT=sel[:, e_, :], rhs=cT_sb[:, tg * 512:(tg + 1) * 512], start=True, stop=True),
                      reads=["sel", "cT"], writes=[bk(CBK)])
                cs = ui[0] % 2
                fw.op("act", lambda e, cs=cs: e.activation(out=Cb[:, cs, :], in_=PS[CBK], func=AF.Copy), reads=[bk(CBK)], writes=[("Cb", cs)])
                for fc in range(2):
                    u = ui[0] % 2
                    ab, gb = AG[2 * u], AG[2 * u + 1]
                    ui[0] += 1
                    for (bank, wname) in ((ab, "w1"), (gb, "w3")):
                        for k in range(8):
                            fw.op("pe", lambda e, bank=bank, wname=wname, j=j, fc=fc, k=k, tg=tg: e.matmul(
                                out=PS[bank], lhsT=buf[wname][:, j, k, fc * 128:(fc + 1) * 128], rhs=h2T[:, k, tg * 512:(tg + 1) * 512],
                                start=(k == 0), stop=(k == 7)),
                                reads=[(wname, bi % 2, j), ("h2T", tg)], writes=[bk(bank)], sig=(k == 7))
                    fw.op("act", lambda e, ab=ab, u=u: e.activation(out=s_sb[:, u, :], in_=PS[ab], func=AF.Silu), reads=[bk(ab)], writes=[("s", u)])
                    fw.op("dve", lambda e, gb=gb, u=u, cs=cs: e.tensor_tensor(out=gc_sb[:, u, :], in0=PS[gb], in1=Cb[:, cs, :], op=ALU.mult),
                          reads=[bk(gb), ("Cb", cs)], writes=[("gc", u)])
                    fw.op("dve", lambda e, u=u, j=j, fc=fc, hs=hs: e.tensor_tensor(out=hid[:, hs, j * 2 + fc, :], in0=s_sb[:, u, :], in1=gc_sb[:, u, :], op=ALU.mult),
                          reads=[("s", u), ("gc", u)], writes=[("hid", hs)])
                ui[0] += 0
            for tt in range(4):
                t = tg * 4 + tt
                for dh in range(2):
                    yb = YB[yi[0] % 3]
                    yi[0] += 1
                    n = EB * 2
                    for q in range(n):
                        j, fc = q // 2, q % 2
                        fw.op("pe", lambda e, yb=yb, q=q, j=j, fc=fc, tt=tt, dh=dh, hs=hs: e.matmul(
                            out=PS[yb], lhsT=hid[:, hs, q, tt * 128:(tt + 1) * 128], rhs=buf["w2"][:, j, fc, dh * 512:(dh + 1) * 512],
                            start=(q == 0), stop=(q == n - 1)),
                            reads=[("hid", hs), ("w2", bi % 2, j)], writes=[bk(yb)], sig=(q == n - 1))
                    fw.op("dve", lambda e, yb=yb, t=t, dh=dh: e.tensor_tensor(out=X1[:, t, dh * 512:(dh + 1) * 512], in0=PS[yb], in1=X1[:, t, dh * 512:(dh + 1) * 512], op=ALU.add),
                          reads=[bk(yb), ("X1", t)], writes=[("X1", t)])
            if bi == NB - 1:
                fw.dma("sp", out_v[:, 4 * tg:4 * tg + 4, :], X1[:, 4 * tg:4 * tg + 4, :], reads=[("X1", t) for t in range(4 * tg, 4 * tg + 4)], writes=[("out", tg)])
        if bi + 2 < NB:
            load_expert_batch(bi + 2)
    fw.barrier()
    print("ops", fw.nops, "sem counts", fw.cnt)
    return nc, fw, None


def host_inputs(inputs, b):
    f = np.float32
    g = lambda k: np.asarray(inputs[k], dtype=f)
    P = [0, 4, 1, 5, 2, 6, 3, 7]
    w_in = g("w_in")[0]
    qcols = np.concatenate([np.arange(h * 64, (h + 1) * 64) for h in P])
    w_in_tm = np.concatenate([w_in[:, qcols], w_in[:, 512:768], w_in[:, 1280:1792], w_in[:, 1808:2320]], axis=1)
    w_in_fm = np.concatenate([w_in[:, 768:1280], w_in[:, 1792:1920]], axis=1)
    col = lambda v: np.ascontiguousarray(v.reshape(-1, 128).T)
    vecs = np.concatenate([
        col(g("b_ada")[0]), col(g("g_norm1")[0]), col(g("g_norm2")[0]), col(g("g_att_out")[0]), col(g("g_gla_out")[0]),
        col(g("b_gk")[0]), col(np.tile(g("q_norm")[0], 2)), col(np.tile(g("k_norm")[0], 2)), col(g("c")[b]),
    ], axis=1)
    assert vecs.shape == (128, NV)
    sinks_b = np.ascontiguousarray(np.broadcast_to(g("sinks")[0][None, :], (128, 8)))
    brg = np.ascontiguousarray(np.broadcast_to(np.concatenate([g("b_group")[0], g("b_router")[0]])[None, :], (128, 20)))
    slopes = np.exp2(-8.0 * np.arange(1, 9) / 8).astype(f)
    kj = np.arange(128)[:, None]
    qi = np.arange(128)[None, :]
    abias = np.zeros((128, 4, 512), f)
    for kv in range(2):
        for gg in range(4):
            h = kv * 4 + gg
            dcur = (qi - kj).astype(f)
            abias[:, kv * 2 + 0, gg * 128:(gg + 1) * 128] = np.where(dcur >= 0, -slopes[h] * dcur, -30000.0)
            dprev = (qi - kj + 128).astype(f)
            abias[:, kv * 2 + 1, gg * 128:(gg + 1) * 128] = np.where(dprev < 128, -slopes[h] * dprev, -30000.0)
    gmask = (kj <= qi).astype(f)
    sel = np.zeros((128, 16, 128), f)
    for e in range(16):
        sel[e, e, :] = 1.0
    w_rt = np.concatenate([g("w_group")[0], g("w_router")[0]], axis=1)
    return {
        "x": np.ascontiguousarray(g("x")[b]), "vecs": vecs, "sinks_b": sinks_b, "brg": brg,
        "ident": np.eye(128, dtype=f), "abias": abias, "gmask": gmask, "sel": sel,
        "w_ada": g("w_ada")[0], "w_in_tm": np.ascontiguousarray(w_in_tm), "w_in_fm": np.ascontiguousarray(w_in_fm),
        "w_gk2": g("w_gk2")[0], "w_out": g("w_out")[0], "w_rt": np.ascontiguousarray(w_rt),
        "w1": g("w1")[0], "w3": g("w3")[0], "w2": g("w2")[0],
    }


def kernel(**inputs):
    nc, fw, _ = build_nc()
    in_maps = [host_inputs(inputs, b) for b in range(8)]
    res = run_bass_kernel_spmd(nc, in_maps, core_ids=list(range(8)))
    return np.stack([r["out"] for r in res.results], axis=0).astype(np.float32)
```

```python
import math
import numpy as np
import concourse.bass as bass
import concourse.mybir as mybir
from concourse.bass_utils import run_bass_kernel_spmd

F32 = mybir.dt.float32
BF16 = mybir.dt.bfloat16
AF = mybir.ActivationFunctionType
ALU = mybir.AluOpType
AX = mybir.AxisListType

T = 2048
D = 1024
NT = 16
EPS = 1e-6
LN8 = math.log(8.0)
NV = 81
NEXP = 16
EB = 2
SAME_ENGINE_FULL_SYNC = True
SEQ_STREAMS = False
GREEDY = True
PE_SLOW = 1.0
PIPE_A = True
GROUPS = tuple((2 * i, 2) for i in range(8))
PRIO_B = 0.0
MOE_PIPE = True
M_LAT = 0.15
M_ACT0 = 0.22
M_DVE0 = 0.12
LEAD = 1
DBG_SKIP = ()
DBG_STAGE = 99
MODCOL = 320
DBG_NT = 4
DBG_SB = (3, 4)
DBG_OB = (5, 4)


class FW:
    def __init__(self, nc):
        self.nc = nc
        self.eng = {"pe": nc.tensor, "act": nc.scalar, "dve": nc.vector, "pool": nc.gpsimd, "sp": nc.sync}
        self.sem = {e: nc.alloc_semaphore("s_" + e) for e in self.eng}
        self.cnt = {e: 0 for e in self.eng}
        self.seen = {e: {} for e in self.eng}
        self.dsem = {q: [nc.alloc_semaphore("d_%s%d" % (q, i)) for i in range(14)] for q in ("sp", "pool")}
        self.dval = {q: [0] * 14 for q in ("sp", "pool")}
        self.drr = {"sp": 0, "pool": 0}
        self.lastw = {}
        self.readers = {}
        self.pe_pending = []
        self.nops = 0
        self.log = None
        self.tags = {}
        self.eng_free = {e: 0.0 for e in self.eng}
        self.tok_end = {}
        self.cur_stream_end = 0.0

    def _deps(self, eng, reads, writes, is_dma):
        toks = []
        for k in reads:
            w = self.lastw.get(k)
            if w is not None:
                toks.append((w, "raw"))
            if isinstance(k, tuple) and k[0] == "ps":
                for r in self.readers.get(k, ()):
                    toks.append((r, "rar"))
        for k in writes:
            w = self.lastw.get(k)
            if w is not None:
                toks.append((w, "waw"))
            for r in self.readers.get(k, ()):
                toks.append((r, "war"))
        need = {}
        for tok, kind in toks:
            if tok[0] == "c":
                src = tok[1]
                if src == eng and not is_dma:
                    if eng == "pe":
                        continue
                    if kind != "raw" and not SAME_ENGINE_FULL_SYNC:
                        continue
                assert tok[2] is not None, "unresolved PE token"
                key = ("c", src)
                need[key] = max(need.get(key, 0), tok[2])
            else:
                key = ("d", tok[1], tok[2])
                need[key] = max(need.get(key, 0), tok[3])
        waits = []
        for key, val in need.items():
            if self.seen[eng].get(key, 0) >= val:
                continue
            self.seen[eng][key] = val
            if key[0] == "c":
                waits.append((self.sem[key[1]], val))
            else:
                waits.append((self.dsem[key[1]][key[2]], val))
        return waits

    def _record(self, tok, reads, writes):
        for k in reads:
            self.readers.setdefault(k, []).append(tok)
        for k in writes:
            self.lastw[k] = tok
            self.readers[k] = []

    def _tagcheck(self, reads, writes, rtag, wtag, keep_tag):
        for k in reads:
            if rtag is not None and k in rtag:
                assert self.tags.get(k) == rtag[k], ("stale read", k, self.tags.get(k), rtag[k])
        if not keep_tag:
            for k in writes:
                self.tags[k] = (wtag or {}).get(k)

    def op(self, eng, fn, reads=(), writes=(), sig=True, rtag=None, wtag=None, keep_tag=False):
        self._tagcheck(reads, writes, rtag, wtag, keep_tag)
        waits = self._deps(eng, reads, writes, False)
        e = self.eng[eng]
        for s, v in waits[1:]:
            e.wait_ge(s, v)
        rec = {}
        ins = fn(_EngProxy(e, rec))
        self._model(eng, rec, reads, writes)
        if waits:
            ins._wait_ge(waits[0][0], waits[0][1])
        tok = ["c", eng, None]
        if eng == "pe" and not sig:
            self.pe_pending.append(tok)
        else:
            self.cnt[eng] += 1
            ins.then_inc(self.sem[eng], 1)
            tok[2] = self.cnt[eng]
            if eng == "pe":
                for p in self.pe_pending:
                    p[2] = tok[2]
                self.pe_pending = []
        self._record(tok, reads, writes)
        self.nops += 1
        if self.log is not None:
            import sys as _s
            fr = _s._getframe(1)
            nm = None
            ln = fr.f_lineno
            while fr is not None:
                if fr.f_code.co_name.startswith(("stage_", "stream_", "mod_chunk")):
                    nm = fr.f_code.co_name
                    break
                fr = fr.f_back
            self.log.append((eng, tok[2], (nm, ln), list(reads), list(writes), [(str(s), v) for s, v in waits]))
        return ins

    def dma(self, q, out, in_, reads=(), writes=()):
        waits = self._deps(q, reads, writes, True)
        i = self.drr[q]
        self.drr[q] = (i + 1) % len(self.dsem[q])
        prev = self.dval[q][i]
        key = ("d", q, i)
        if prev and self.seen[q].get(key, 0) < prev:
            self.seen[q][key] = prev
            waits.append((self.dsem[q][i], prev))
        e = self.eng[q]
        for s, v in waits:
            e.wait_ge(s, v)
        e.dma_start(out=out, in_=in_).then_inc(self.dsem[q][i], 16)
        self.dval[q][i] = prev + 16
        tok = ["d", q, i, prev + 16]
        self._record(tok, reads, writes)
        try:
            nb = 1
            for d_ in in_.shape:
                nb *= d_
            nb *= 4
        except Exception:
            nb = 1 << 20
        st = max(self.eng_free[q], max([self.tok_end.get(("w", k), 0.0) for k in list(reads) + list(writes)] + [self.tok_end.get(("r", k), 0.0) for k in writes] + [0.0]))
        en = st + 2.0 + nb / 180e3
        self.eng_free[q] = st + nb / 180e3
        for k in writes:
            self.tok_end[("w", k)] = en
            self.tok_end[("r", k)] = 0.0
        self.nops += 1

    def _model(self, eng, rec, reads, writes):
        kw = rec.get("kw", {})
        name = rec.get("name", "")

        def fsz(ap):
            try:
                sh = ap.shape
                n = 1
                for d in sh[1:]:
                    n *= d
                return n
            except Exception:
                return 128
        if eng == "pe":
            if name == "transpose":
                n = 128
                dur = 0.07 if kw["in_"].dtype == BF16 else 0.3
            else:
                n = fsz(kw["rhs"])
                dur = (max(n, 64) / 2400.0 + 0.02) * PE_SLOW
                if kw["rhs"].dtype == F32:
                    dur *= 4
        elif eng == "act":
            n = fsz(kw.get("out"))
            dur = M_ACT0 + n / 1200.0 + (0.1 if kw.get("accum_out") is not None else 0.0)
        else:
            n = fsz(kw.get("out")) if kw.get("out") is not None else 128
            dur = M_DVE0 + n / 960.0
            if name == "tensor_tensor_scan":
                dur = 0.12 + 2 * n / 960.0
            if name == "reciprocal":
                dur = 0.12 + 6 * n / 960.0
        ready = 0.0
        for k in reads:
            ready = max(ready, self.tok_end.get(("w", k), 0.0))
        for k in writes:
            ready = max(ready, self.tok_end.get(("w", k), 0.0), self.tok_end.get(("r", k), 0.0))
        start = max(self.eng_free[eng], ready + M_LAT)
        end = start + dur
        self.eng_free[eng] = end
        for k in reads:
            self.tok_end[("r", k)] = max(self.tok_end.get(("r", k), 0.0), end)
        for k in writes:
            self.tok_end[("w", k)] = end
            self.tok_end[("r", k)] = 0.0
        self.cur_stream_end = max(self.cur_stream_end, end)

    def barrier(self):
        for e in self.eng:
            for src in self.eng:
                if src == e:
                    continue
                v = self.cnt[src]
                if v and self.seen[e].get(("c", src), 0) < v:
                    self.seen[e][("c", src)] = v
                    self.eng[e].wait_ge(self.sem[src], v)
            for q in ("sp", "pool"):
                for i, v in enumerate(self.dval[q]):
                    if v and self.seen[e].get(("d", q, i), 0) < v:
                        self.seen[e][("d", q, i)] = v
                        self.eng[e].wait_ge(self.dsem[q][i], v)


class _EngProxy:
    def __init__(self, real, rec):
        self._real = real
        self._rec = rec

    def __getattr__(self, name):
        f = getattr(self._real, name)
        rec = self._rec

        def w(*a, **k):
            rec["name"] = name
            rec["kw"] = k
            return f(*a, **k)
        return w


class Carver:
    def __init__(self, ap, nbytes):
        self.ap = ap
        self.n = nbytes
        self.off = 0

    def get(self, free_shape, dt):
        esz = 4 if dt == F32 else 2
        n = int(np.prod(free_shape)) * esz
        n_al = (n + 63) // 64 * 64
        assert self.off + n_al <= self.n, "carver overflow %d + %d > %d" % (self.off, n_al, self.n)
        v = self.ap[:, self.off // 2:(self.off + n) // 2]
        self.off += n_al
        if dt == F32:
            v = v.bitcast(F32)
        if len(free_shape) == 2:
            v = v.rearrange("p (a b) -> p a b", a=free_shape[0])
        elif len(free_shape) == 3:
            v = v.rearrange("p (a b c) -> p a b c", a=free_shape[0], b=free_shape[1])
        return v

    def reset(self):
        self.off = 0


def build_nc(stage=99, dbg=False):
    nc = bass.Bass("TRN2", target_bir_lowering=False)
    fw = FW(nc)

    def din(name, shape, dt=F32):
        return nc.dram_tensor(name, list(shape), dt, kind="ExternalInput").ap()

    x_d = din("x", [T, D])
    vecs_d = din("vecs", [128, NV])
    sinks_d = din("sinks_b", [128, 8])
    brg_d = din("brg", [128, 20])
    ident_d = din("ident", [128, 128])
    abias_d = din("abias", [128, 4, 512])
    gmask_d = din("gmask", [128, 128])
    sel_d = din("sel", [128, 16, 128])
    wada_d = din("w_ada", [D, 6 * D])
    wintm_d = din("w_in_tm", [D, 1792])
    winfm_d = din("w_in_fm", [D, 640])
    wgk2_d = din("w_gk2", [16, 256])
    wout_d = din("w_out", [D, D])
    wr_d = din("w_rt", [D, 20])
    w1_d = din("w1", [NEXP, D, 256])
    w3_d = din("w3", [NEXP, D, 256])
    w2_d = din("w2", [NEXP, 256, D])
    out_d = nc.dram_tensor("out", [T, D], F32, kind="ExternalOutput").ap()
    dbg_d = nc.dram_tensor("dbg", [128, 4096], F32, kind="ExternalOutput").ap() if dbg else None

    x_v = x_d.rearrange("(t p) d -> p t d", p=128)
    out_v = out_d.rearrange("(t p) d -> p t d", p=128)

    X1 = nc.alloc_sbuf_tensor("X1", [128, NT, D], F32).ap()
    PERS_BYTES = 11776
    pers = Carver(nc.alloc_sbuf_tensor("pers", [128, PERS_BYTES // 2], BF16).ap(), PERS_BYTES)
    REG_BYTES = 135168
    reg = Carver(nc.alloc_sbuf_tensor("reg", [128, REG_BYTES // 2], BF16).ap(), REG_BYTES)

    ident_f = pers.get([128], F32)
    ident_b = pers.get([128], BF16)
    ones_f = pers.get([128], F32)
    ones_b = pers.get([128], BF16)
    abias = pers.get([4, 512], BF16)
    gmask = pers.get([128], BF16)
    sel = pers.get([16, 128], BF16)
    vecs = pers.get([NV], F32)
    es = pers.get([8], F32)
    brg = pers.get([20], F32)
    mod = pers.get([48], F32)
    gs1 = pers.get([8], F32)
    gs2 = pers.get([8], F32)
    nbgk = pers.get([2], F32)
    ssx = pers.get([NT], F32)
    rsx = pers.get([NT], F32)
    sc_b = pers.get([8], BF16)
    tmp8 = pers.get([8], F32)
    dbg_sb = pers.get([16], F32)

    PS = [nc.alloc_psum_tensor("ps%d" % i, [128, 512], F32).ap() for i in range(8)]
    PSB = [p.bitcast(BF16) for p in PS]

    def bk(i):
        return ("ps", i)

    fw.dma("sp", ident_f, ident_d, writes=["ident_f"])
    fw.dma("sp", vecs, vecs_d, writes=["vecs"])
    fw.dma("sp", es, sinks_d, writes=["es"])
    fw.dma("sp", brg, brg_d, writes=["brg"])
    fw.dma("pool", ident_b, ident_d, writes=["ident_b"])
    fw.dma("pool", abias, abias_d, writes=["abias"])
    fw.dma("pool", gmask, gmask_d, writes=["gmask"])
    fw.dma("pool", sel, sel_d, writes=["sel"])
    fw.dma("sp", X1[:, 0:4, :], x_v[:, 0:4, :], writes=[("X1", t) for t in range(4)])
    fw.op("dve", lambda e: e.memset(ones_f, 1.0), writes=["ones_f"])
    fw.op("dve", lambda e: e.memset(ones_b, 1.0), writes=["ones_b"])

    HR = 8
    hT = reg.get([8, HR * 128], BF16)
    win_tm = reg.get([8, 1792], BF16)
    win_fm = reg.get([8, 640], BF16)
    wout_sb = reg.get([8, 1024], BF16)
    wada_buf = [reg.get([8, 128], BF16) for _ in range(2)]
    xn = reg.get([1, 1024], BF16)
    sqq = reg.get([512], F32)
    qn = reg.get([512], BF16)
    kn = reg.get([128], BF16)
    ssq = reg.get([8], F32)
    rq = reg.get([8], F32)
    ssk = reg.get([2], F32)
    rk = reg.get([2], F32)
    qTA = reg.get([2, 512], BF16)
    qTB = reg.get([2, 512], BF16)
    kT = reg.get([4 * 128], BF16)
    vaug = reg.get([4, 2, 65], BF16)
    vg = reg.get([2, 512], BF16)
    sog = reg.get([2, 512], BF16)
    PT = reg.get([4, 512], BF16)
    den = reg.get([8], F32)
    on = reg.get([512], F32)
    ssa = reg.get([1], F32)
    ra = reg.get([1], F32)
    ya = reg.get([512], BF16)
    yA = reg.get([8, 4, 128], BF16)
    yG = reg.get([4, 128], BF16)
    OFF_LBUF = reg.off
    lbuf = reg.get([2, 512], F32)
    Bc = reg.get([2, 512], F32)
    elast = reg.get([2, 4], F32)
    OFF_QDPAD = reg.off
    qd_pad = reg.get([4, 512], BF16)
    kdT = reg.get([2, 512], BF16)
    kdecT = reg.get([2, 512], BF16)
    kdec = reg.get([4, 256], BF16)
    AT = reg.get([4, 128], BF16)
    S = reg.get([2, 128], F32)
    Sbf = reg.get([2, 128], BF16)
    ssg = reg.get([4], F32)
    rg = reg.get([4], F32)
    t1 = reg.get([512], F32)
    sq_b = sqq.bitcast(BF16)
    esc = t1
    sqB = reg.get([512], BF16)
    yg = reg.get([512], BF16)
    lrT_pad = reg.get([512], BF16)
    wgk2_pad = reg.get([256], BF16)
    print("phase1 region bytes", reg.off, "of", reg.n)

    wada_v = wada_d.rearrange("(k p) n -> p k n", p=128)

    ccol = vecs[:, 73:81]
    fw.op("act", lambda e: e.activation(out=tmp8, in_=ccol, func=AF.Exp, scale=-1.0), reads=["vecs"], writes=["tmp8"])
    fw.op("dve", lambda e: e.tensor_scalar_add(out=tmp8, in0=tmp8, scalar1=1.0), reads=["tmp8"], writes=["tmp8"])
    fw.op("dve", lambda e: e.reciprocal(out=tmp8, in_=tmp8), reads=["tmp8"], writes=["tmp8"])
    fw.op("dve", lambda e: e.tensor_tensor(out=sc_b, in0=tmp8, in1=ccol, op=ALU.mult), reads=["tmp8", "vecs"], writes=["sc_b"])
    MODB = 5
    wctr = [0]

    def mod_chunk(c):
        bi = wctr[0] % 2
        wctr[0] += 1
        buf = wada_buf[bi]
        if 'chunkdma' in DBG_SKIP and c >= 16:
            return
        fw.dma("pool", buf, wada_v[:, :, c * 128:(c + 1) * 128], writes=[("wada", bi)])
        for k in range(8):
            fw.op("pe", lambda e, k=k: e.matmul(out=PS[MODB][:, MODCOL:MODCOL + 1], lhsT=buf[:, k, :], rhs=sc_b[:, k:k + 1], start=(k == 0), stop=(k == 7)),
                  reads=[("wada", bi), "sc_b"], writes=[bk(MODB)], sig=(k == 7), keep_tag=True)
        fw.op("dve", lambda e: e.tensor_tensor(out=mod[:, c:c + 1], in0=PS[MODB][:, MODCOL:MODCOL + 1], in1=vecs[:, c:c + 1], op=ALU.add),
              reads=[bk(MODB), "vecs"], writes=[("mod", c // 8)])

    MK = [("mod", i) for i in range(6)]
    big = [reg.ap[:, boff // 2:(boff + 8192) // 2].rearrange("p (k n) -> p k n", k=8) for boff in (OFF_LBUF, OFF_QDPAD)]
    bigkeys = [[("lbuf", 0), ("lbuf", 1), ("Bc", 0), ("Bc", 1)], ["qd_pad", ("kdT", 0), ("kdT", 1), ("kdecT", 0), ("kdecT", 1)]]
    for cc in range(4):
        fw.dma("pool", big[cc % 2], wada_v[:, :, cc * 512:(cc + 1) * 512], writes=bigkeys[cc % 2])
        if cc == 0:
            for g_ in range(1, 4):
                pass
        for j in range(4):
            c = cc * 4 + j
            for k in range(8):
                fw.op("pe", lambda e, k=k, j=j, cc=cc: e.matmul(out=PS[MODB][:, MODCOL:MODCOL + 1], lhsT=big[cc % 2][:, k, j * 128:(j + 1) * 128], rhs=sc_b[:, k:k + 1], start=(k == 0), stop=(k == 7)),
                      reads=bigkeys[cc % 2] + ["sc_b"], writes=[bk(MODB)], sig=(k == 7), keep_tag=True)
            fw.op("dve", lambda e, c=c: e.tensor_tensor(out=mod[:, c:c + 1], in0=PS[MODB][:, MODCOL:MODCOL + 1], in1=vecs[:, c:c + 1], op=ALU.add),
                  reads=[bk(MODB), "vecs"], writes=[("mod", c // 8)])
    fw.op("dve", lambda e: e.scalar_tensor_tensor(out=gs1, in0=mod[:, 8:16], scalar=1.0, in1=vecs[:, 48:56], op0=ALU.add, op1=ALU.mult),
          reads=[("mod", 1), "vecs"], writes=["gs1"])
    fw.op("dve", lambda e: e.tensor_scalar(out=nbgk, in0=vecs[:, 69:71], scalar1=-1.0, scalar2=None, op0=ALU.mult),
          reads=["vecs"], writes=["nbgk"])
    fw.op("act", lambda e: e.activation(out=es, in_=es, func=AF.Exp), reads=["es"], writes=["es"])
    wintm_v = wintm_d.rearrange("(k p) n -> p k n", p=128)
    fw.dma("pool", win_tm[:, :, 0:768], wintm_v[:, :, 0:768], writes=["win_tmA"])
    fw.dma("pool", win_tm[:, :, 768:1792], wintm_v[:, :, 768:1792], writes=["win_tmB"])
    fw.dma("pool", win_fm, winfm_d.rearrange("(k p) n -> p k n", p=128), writes=["win_fm"])
    for g in range(1, 4):
        fw.dma("sp", X1[:, 4 * g:4 * g + 4, :], x_v[:, 4 * g:4 * g + 4, :], reads=["win_tmA"], writes=[("X1", t) for t in range(4 * g, 4 * g + 4)])
    fw.op("dve", lambda e: e.memset(wgk2_pad, 0.0), writes=["wgk2"])
    fw.dma("pool", wgk2_pad[0:16, :], wgk2_d, reads=[], writes=["wgk2"])

    GB = (6, 7)

    def stream_M1():
        for c in range(16, 24):
            mod_chunk(c)
            yield
        fw.dma("pool", wout_sb, wout_d.rearrange("(k p) n -> p k n", p=128), writes=["wout"])
        yield
        for hh in range(2):
            for kk in range(4):
                k = hh * 4 + kk
                fw.op("dve", lambda e, k=k: e.tensor_scalar(out=yg[:, 0:256].bitcast(F32), in0=ident_f, scalar1=mod[:, 16 + k:17 + k], scalar2=None, op0=ALU.mult),
                      reads=["ident_f", ("mod", 2)], writes=["yg"])
                fw.op("pe", lambda e, k=k, hh=hh, kk=kk: e.matmul(out=PS[GB[hh]][:, kk * 128:(kk + 1) * 128], lhsT=ones_f, rhs=yg[:, 0:256].bitcast(F32), start=True, stop=True),
                      reads=["ones_f", "yg"], writes=[bk(GB[hh])])
            for k in range(8):
                fw.op("dve", lambda e, k=k, hh=hh: e.tensor_tensor(out=wout_sb[:, k, hh * 512:(hh + 1) * 512], in0=wout_sb[:, k, hh * 512:(hh + 1) * 512],
                                                               in1=PS[GB[hh]], op=ALU.mult),
                      reads=["wout", bk(GB[hh])], writes=["wout"])
            yield

    def stream_M2():
        for c in range(24, 48):
            mod_chunk(c)
            yield
        fw.op("dve", lambda e: e.scalar_tensor_tensor(out=gs2, in0=mod[:, 32:40], scalar=1.0, in1=vecs[:, 56:64], op0=ALU.add, op1=ALU.mult),
              reads=[("mod", 4), "vecs"], writes=["gs2"])
        yield

    fw.op("dve", lambda e: e.memset(qTA, 0.0), writes=["qTA0", "qTA1"])
    fw.op("dve", lambda e: e.memset(qTB, 0.0), writes=["qTB0", "qTB1"])
    fw.op("dve", lambda e: e.memset(vaug, 1.0), writes=[("vaug", t) for t in range(4)])
    fw.op("dve", lambda e: e.memset(qd_pad, 0.0), writes=["qd_pad"])
    fw.op("dve", lambda e: e.memset(lrT_pad, 0.0), writes=["lrT"])
    fw.op("dve", lambda e: e.memset(S, 0.0), writes=["S"])
    fw.op("dve", lambda e: e.memset(Sbf, 0.0), writes=["Sbf"])

    alt = [0]

    def evac_affine(out, in_, scale, bias, reads, writes, wtag=None, rtag=None):
        alt[0] ^= 1
        if alt[0]:
            if bias is None:
                fw.op("act", lambda e: e.activation(out=out, in_=in_, func=AF.Copy, scale=scale), reads=reads, writes=writes, wtag=wtag, rtag=rtag)
            else:
                fw.op("act", lambda e: e.activation(out=out, in_=in_, func=AF.Identity, scale=scale, bias=bias), reads=reads, writes=writes, wtag=wtag, rtag=rtag)
        else:
            if bias is None:
                fw.op("dve", lambda e: e.tensor_scalar(out=out, in0=in_, scalar1=scale, scalar2=None, op0=ALU.mult), reads=reads, writes=writes, wtag=wtag, rtag=rtag)
            else:
                fw.op("dve", lambda e: e.tensor_scalar(out=out, in0=in_, scalar1=scale, scalar2=bias, op0=ALU.mult, op1=ALU.add), reads=reads, writes=writes, wtag=wtag, rtag=rtag)

    def rstd_act(out, in_, n, extra_bias, rk_, wk_):
        fw.op("act", lambda e: e.activation(out=out, in_=in_, func=AF.Ln, scale=1.0 / n, bias=EPS), reads=rk_, writes=wk_)
        fw.op("act", lambda e: e.activation(out=out, in_=out, func=AF.Exp, scale=-0.5, bias=extra_bias), reads=wk_, writes=wk_)

    TB = 0

    def stage_T1(t):
        hs = t % HR
        xs = 0
        fw.op("act", lambda e: e.activation(out=sq_b, in_=X1[:, t, :], func=AF.Square, accum_out=ssx[:, t:t + 1]),
              reads=[("X1", t)], writes=["sqq", ("ssx", t)])
        rstd_act(rsx[:, t:t + 1], ssx[:, t:t + 1], D, 0.0, [("ssx", t)], [("rsx", t)])
        fw.op("dve", lambda e: e.tensor_scalar(out=xn[:, xs, :], in0=X1[:, t, :], scalar1=rsx[:, t:t + 1], scalar2=None, op0=ALU.mult),
              reads=[("X1", t), ("rsx", t)], writes=[("xn", xs)])
        yield
        T1B = (TB, 3)
        for k in range(8):
            fw.op("pe", lambda e, k=k: e.transpose(out=PSB[T1B[k // 4]][:, (k % 4) * 128:(k % 4 + 1) * 128], in_=xn[:, xs, k * 128:(k + 1) * 128], identity=ident_b),
                  reads=[("xn", xs), "ident_b"], writes=[bk(T1B[k // 4])], sig=(k % 4 == 3), wtag={bk(T1B[k // 4]): ("T1", t)})
        for k in range(4):
            fw.op("act", lambda e, k=k: e.activation(out=hT[:, k, hs * 128:(hs + 1) * 128], in_=PSB[TB][:, k * 128:(k + 1) * 128], func=AF.Identity,
                                                     scale=gs1[:, k:k + 1], bias=mod[:, k:k + 1]),
                  reads=[bk(TB), "gs1", ("mod", 0)], writes=[("hT", hs)], wtag={("hT", hs): t}, rtag={bk(TB): ("T1", t)})
        hv = hT[:, 4:8, hs * 128:(hs + 1) * 128]
        fw.op("dve", lambda e: e.tensor_tensor(out=hv, in0=PSB[3][:, 0:512].rearrange("p (k n) -> p k n", k=4),
                                               in1=gs1[:, 4:8].unsqueeze(2).to_broadcast([128, 4, 128]), op=ALU.mult),
              reads=[bk(3), "gs1"], writes=[("hT", hs)], wtag={("hT", hs): t}, rtag={bk(3): ("T1", t)})
        fw.op("dve", lambda e: e.tensor_tensor(out=hv, in0=hv, in1=mod[:, 4:8].unsqueeze(2).to_broadcast([128, 4, 128]), op=ALU.add),
              reads=[("hT", hs), ("mod", 0)], writes=[("hT", hs)], wtag={("hT", hs): t})
        yield

    QB, KVB, VGB, OGB = 1, 2, 6, 7

    def stage_T2(t):
        hs = t % HR
        for bank, c0, n in ((QB, 0, 512), (KVB, 512, 256)):
            for k in range(8):
                fw.op("pe", lambda e, bank=bank, c0=c0, n=n, k=k: e.matmul(
                    out=PS[bank][:, 0:n], lhsT=hT[:, k, hs * 128:(hs + 1) * 128], rhs=win_tm[:, k, c0:c0 + n],
                    start=(k == 0), stop=(k == 7)),
                    reads=[("hT", hs), "win_tmA"], writes=[bk(bank)], sig=(k == 7), rtag={("hT", hs): t}, wtag={bk(bank): ("proj", t)})
            yield

    def stage_T3(t):
        qs = t % 2
        vs = t % 4
        fw.op("act", lambda e: e.activation(out=sqq, in_=PS[QB], func=AF.Square), reads=[bk(QB)], writes=["sqq"], rtag={bk(QB): ("proj", t)})
        fw.op("dve", lambda e: e.tensor_reduce(out=ssq, in_=sqq.rearrange("p (h d) -> p h d", h=8), axis=AX.X, op=ALU.add),
              reads=["sqq"], writes=["ssq"])
        rstd_act(rq, ssq, 64, -LN8, ["ssq"], ["rq"])
        fw.op("dve", lambda e: e.tensor_tensor(out=qn.rearrange("p (h d) -> p h d", h=8), in0=PS[QB].rearrange("p (h d) -> p h d", h=8),
                                               in1=rq.unsqueeze(2).to_broadcast([128, 8, 64]), op=ALU.mult),
              reads=[bk(QB), "rq"], writes=["qn"], rtag={bk(QB): ("proj", t)})
        yield
        fw.op("act", lambda e: e.activation(out=sqq[:, 0:128], in_=PS[KVB][:, 0:128], func=AF.Square), reads=[bk(KVB)], writes=["sqq"])
        fw.op("dve", lambda e: e.tensor_reduce(out=ssk, in_=sqq[:, 0:128].rearrange("p (h d) -> p h d", h=2), axis=AX.X, op=ALU.add),
              reads=["sqq"], writes=["ssk"])
        rstd_act(rk, ssk, 64, 0.0, ["ssk"], ["rk"])
        fw.op("dve", lambda e: e.tensor_tensor(out=kn.rearrange("p (h d) -> p h d", h=2), in0=PS[KVB][:, 0:128].rearrange("p (h d) -> p h d", h=2),
                                               in1=rk.unsqueeze(2).to_broadcast([128, 2, 64]), op=ALU.mult),
              reads=[bk(KVB), "rk"], writes=["kn"], rtag={bk(KVB): ("proj", t)})
        yield
        if 'T3v' in DBG_SKIP:
            return
        fw.op("act", lambda e: e.activation(out=vaug[:, t % 4, :, 0:64], in_=PS[KVB][:, 128:256].rearrange("p (h d) -> p h d", h=2), func=AF.Copy),
              reads=[bk(KVB)], writes=[("vaug", t % 4)])
        if 'T3t' in DBG_SKIP:
            return
        for i in range(4):
            fw.op("pe", lambda e, i=i: e.transpose(out=PSB[TB][:, i * 128:(i + 1) * 128], in_=qn[:, i * 128:(i + 1) * 128], identity=ident_b),
                  reads=["qn", "ident_b"], writes=[bk(TB)], sig=False)
        fw.op("pe", lambda e: e.transpose(out=PSB[TB][:, 512:640], in_=kn, identity=ident_b),
              reads=["kn", "ident_b"], writes=[bk(TB)], sig=True, wtag={bk(TB): ("T3", t)})
        if 'T3e' in DBG_SKIP:
            return
        gq = vecs[:, 71:72]
        gk = vecs[:, 72:73]
        fw.op("act", lambda e: e.activation(out=qTA[0:64, qs, :], in_=PSB[TB][0:64, 0:512], func=AF.Copy, scale=gq[0:64, :]),
              reads=[bk(TB), "vecs"], writes=["qTA%d" % qs])
        fw.op("dve", lambda e: e.tensor_scalar(out=qTB[64:128, qs, :], in0=PSB[TB][64:128, 0:512], scalar1=gq[64:128, :], scalar2=None, op0=ALU.mult),
              reads=[bk(TB), "vecs"], writes=["qTB%d" % qs])
        fw.op("dve", lambda e: e.tensor_scalar(out=kT[:, (t % 4) * 128:(t % 4 + 1) * 128], in0=PSB[TB][:, 512:640], scalar1=gk, scalar2=None, op0=ALU.mult),
              reads=[bk(TB), "vecs"], writes=[("kT", t % 4)], rtag={bk(TB): ("T3", t)})
        yield

    SB = DBG_SB
    OB = DBG_OB

    def stage_T4(t):
        qs = t % 2
        ys = t % 8
        blocks = [(t, 0)] + ([(t - 1, 1)] if t > 0 else [])
        si = 0
        allpts = []
        for kv in range(2):
            qT = qTA if kv == 0 else qTB
            qkey = ("qTA%d" if kv == 0 else "qTB%d") % qs
            pts = []
            for (blk, which) in blocks:
                bank = SB[si % 2]
                pslot = si % 4
                si += 1
                fw.op("pe", lambda e, bank=bank, blk=blk, qT=qT: e.matmul(out=PS[bank], lhsT=kT[:, (blk % 4) * 128:(blk % 4 + 1) * 128], rhs=qT[:, qs, :], start=True, stop=False),
                      reads=[("kT", blk % 4), qkey], writes=[bk(bank)], sig=False)
                fw.op("pe", lambda e, bank=bank, which=which, kv=kv: e.matmul(out=PS[bank], lhsT=ident_b, rhs=abias[:, kv * 2 + which, :], start=False, stop=True),
                      reads=["ident_b", "abias"], writes=[bk(bank)], sig=True)
                fw.op("act", lambda e, bank=bank, pslot=pslot: e.activation(out=PT[:, pslot, :], in_=PS[bank], func=AF.Exp),
                      reads=[bk(bank)], writes=[("PT", pslot)])
                pts.append((pslot, blk))
                yield
            allpts.append(pts)
        for kv in range(2):
            pts = allpts[kv]
            for g in range(4):
                h = kv * 4 + g
                ob = OB[kv]
                for pi, (pslot, blk) in enumerate(pts):
                    fw.op("pe", lambda e, ob=ob, g=g, pslot=pslot, blk=blk, pi=pi, kv=kv, pts=pts: e.matmul(
                        out=PS[ob][:, g * 65:(g + 1) * 65], lhsT=PT[:, pslot, g * 128:(g + 1) * 128], rhs=vaug[:, blk % 4, kv, :],
                        start=(pi == 0), stop=(pi == len(pts) - 1)),
                        reads=[("PT", pslot), ("vaug", blk % 4)], writes=[bk(ob)], sig=(g == 3 and pi == len(pts) - 1))
            yield
        for kv in range(2):
            ov = PS[OB[kv]][:, 0:260].rearrange("p (h d) -> p h d", h=4)
            fw.op("dve", lambda e, ov=ov, kv=kv: e.tensor_tensor(out=den[:, kv * 4:(kv + 1) * 4], in0=ov[:, :, 64], in1=es[:, kv * 4:(kv + 1) * 4], op=ALU.add),
                  reads=[bk(OB[kv]), "es"], writes=["den"])
        fw.op("dve", lambda e: e.reciprocal(out=den, in_=den), reads=["den"], writes=["den"])
        yield
        for kv in range(2):
            ov = PS[OB[kv]][:, 0:260].rearrange("p (h d) -> p h d", h=4)
            fw.op("dve", lambda e, ov=ov, kv=kv: e.tensor_tensor(
                out=on[:, kv * 256:(kv + 1) * 256].rearrange("p (h d) -> p h d", h=4), in0=ov[:, :, 0:64],
                in1=den[:, kv * 4:(kv + 1) * 4].unsqueeze(2).to_broadcast([128, 4, 64]), op=ALU.mult),
                reads=[bk(OB[kv]), "den"], writes=["on"])
        yield
        fw.op("act", lambda e: e.activation(out=sqq, in_=on, func=AF.Square, accum_out=ssa), reads=["on"], writes=["sqq", "ssa"])
        rstd_act(ra, ssa, 512, 0.0, ["ssa"], ["ra"])
        fw.op("dve", lambda e: e.tensor_scalar(out=ya, in0=on, scalar1=ra, scalar2=None, op0=ALU.mult), reads=["on", "ra"], writes=["ya"])
        yield
        for i in range(4):
            fw.op("pe", lambda e, i=i: e.transpose(out=PSB[TB][:, i * 128:(i + 1) * 128], in_=ya[:, i * 128:(i + 1) * 128], identity=ident_b),
                  reads=["ya", "ident_b"], writes=[bk(TB)], sig=(i == 3), wtag={bk(TB): ("T4", t)})
        for i in range(4):
            evac_affine(yA[:, ys, i, :], PSB[TB][:, i * 128:(i + 1) * 128], vecs[:, 64 + i:65 + i], None, [bk(TB), "vecs"], [("yA", ys)], wtag={("yA", ys): t}, rtag={bk(TB): ("T4", t)})
        yield

    FB = (6, 7)

    def stage_G1(T0, GT):
        hcol = (T0 % HR) * 128
        NG_ = GT * 128
        hkeys = [("hT", (T0 + i) % HR) for i in range(GT)]

        def fm_proj(m, bank):
            for k in range(8):
                fw.op("pe", lambda e, k=k: e.matmul(out=PS[bank][:, 0:NG_], lhsT=win_fm[:, k, m * 128:(m + 1) * 128], rhs=hT[:, k, hcol:hcol + NG_],
                                                    start=(k == 0), stop=(k == 7)),
                      reads=hkeys + ["win_fm"], writes=[bk(bank)], sig=(k == 7), rtag={("hT", (T0 + i) % HR): T0 + i for i in range(GT)})
        fm_proj(4, FB[0])
        fw.op("act", lambda e: e.activation(out=lrT_pad[0:16, 0:NG_], in_=PS[FB[0]][0:16, 0:NG_], func=AF.Copy), reads=[bk(FB[0])], writes=["lrT"])
        yield
        for c in range(2):
            bank = FB[(c + 1) % 2]
            fw.op("pe", lambda e, c=c, bank=bank: e.matmul(out=PS[bank][:, 0:NG_], lhsT=wgk2_pad[:, c * 128:(c + 1) * 128], rhs=lrT_pad[:, 0:NG_], start=True, stop=True),
                  reads=["wgk2", "lrT"], writes=[bk(bank)])
            fw.op("act", lambda e, c=c, bank=bank: e.activation(out=lbuf[:, c, 0:NG_], in_=PS[bank][:, 0:NG_], func=AF.Exp, scale=-1.0, bias=nbgk[:, c:c + 1]),
                  reads=[bk(bank), "nbgk"], writes=[("lbuf", c)])
            fw.op("act", lambda e, c=c: e.activation(out=lbuf[:, c, 0:NG_], in_=lbuf[:, c, 0:NG_], func=AF.Ln, bias=1.0), reads=[("lbuf", c)], writes=[("lbuf", c)])
            yield
            for j in range(GT):
                fw.op("dve", lambda e, c=c, j=j: e.tensor_tensor_scan(out=Bc[:, c, j * 128:(j + 1) * 128], data0=ones_f, data1=lbuf[:, c, j * 128:(j + 1) * 128],
                                                                      initial=0.0, op0=ALU.mult, op1=ALU.add),
                      reads=[("lbuf", c), "ones_f"], writes=[("Bc", c)])
            yield
            fw.op("act", lambda e, c=c: e.activation(out=lbuf[:, c, 0:NG_], in_=Bc[:, c, 0:NG_], func=AF.Exp, scale=-1.0 / 16, bias=-LN8), reads=[("Bc", c)], writes=[("lbuf", c)])
            fw.op("act", lambda e, c=c: e.activation(out=elast[:, c, 0:GT], in_=Bc[:, c, 0:NG_].rearrange("p (j i) -> p j i", j=GT)[:, :, 127], func=AF.Exp, scale=-1.0 / 16),
                  reads=[("Bc", c)], writes=[("elast", c)])
            fw.op("act", lambda e, c=c: e.activation(out=Bc[:, c, 0:NG_], in_=Bc[:, c, 0:NG_], func=AF.Exp, scale=1.0 / 16), reads=[("Bc", c)], writes=[("Bc", c)])
            yield
        for c in range(2):
            bq = FB[0]
            fm_proj(c, bq)
            for hh in range(2):
                fw.op("dve", lambda e, c=c, hh=hh: e.tensor_tensor(out=qd_pad[hh * 64:(hh + 1) * 64, 2 * c + hh, 0:NG_], in0=PS[bq][hh * 64:(hh + 1) * 64, 0:NG_],
                                                                   in1=lbuf[hh * 64:(hh + 1) * 64, c, 0:NG_], op=ALU.mult),
                      reads=[bk(bq), ("lbuf", c)], writes=["qd_pad"])
            yield
            bkk = FB[1]
            fm_proj(2 + c, bkk)
            fw.op("dve", lambda e, c=c: e.tensor_tensor(out=kdT[:, c, 0:NG_], in0=PS[bkk][:, 0:NG_], in1=Bc[:, c, 0:NG_], op=ALU.mult),
                  reads=[bk(bkk), ("Bc", c)], writes=[("kdT", c)])
            yield
            for j in range(GT):
                fw.op("dve", lambda e, c=c, j=j: e.tensor_scalar(out=kdecT[:, c, j * 128:(j + 1) * 128], in0=kdT[:, c, j * 128:(j + 1) * 128],
                                                                 scalar1=elast[:, c, j:j + 1], scalar2=None, op0=ALU.mult),
                      reads=[("kdT", c), ("elast", c)], writes=[("kdecT", c)])
            yield
        for j in range(GT):
            for c in range(2):
                fw.op("pe", lambda e, c=c, j=j: e.transpose(out=PSB[6][:, (j * 2 + c) * 128:(j * 2 + c + 1) * 128], in_=kdecT[:, c, j * 128:(j + 1) * 128], identity=ident_b),
                      reads=[("kdecT", c), "ident_b"], writes=[bk(6)], sig=(j == GT - 1 and c == 1))
        fw.op("act", lambda e: e.activation(out=kdec[:, 0:GT, :].rearrange("p j c -> p (j c)"), in_=PSB[6][:, 0:GT * 256], func=AF.Copy), reads=[bk(6)], writes=["kdec"])
        yield

    def stage_B0(t):
        hs = t % HR
        vs = t % 2
        for bank, c0, n in ((VGB, 768, 512), (OGB, 1280, 512)):
            for k in range(8):
                fw.op("pe", lambda e, bank=bank, c0=c0, n=n, k=k: e.matmul(
                    out=PS[bank][:, 0:n], lhsT=hT[:, k, hs * 128:(hs + 1) * 128], rhs=win_tm[:, k, c0:c0 + n],
                    start=(k == 0), stop=(k == 7)),
                    reads=[("hT", hs), "win_tmB"], writes=[bk(bank)], sig=(k == 7), rtag={("hT", hs): t})
            yield
        fw.op("act", lambda e: e.activation(out=vg[:, vs, :], in_=PS[VGB], func=AF.Copy), reads=[bk(VGB)], writes=[("vg", vs)])
        yield
        fw.op("act", lambda e: e.activation(out=esc, in_=PS[OGB], func=AF.Exp, scale=-1.0), reads=[bk(OGB)], writes=["t1"])
        fw.op("act", lambda e: e.activation(out=esc, in_=esc, func=AF.Ln, bias=1.0), reads=["t1"], writes=["t1"])
        fw.op("act", lambda e: e.activation(out=esc, in_=esc, func=AF.Exp, scale=-1.0), reads=["t1"], writes=["t1"])
        fw.op("dve", lambda e: e.tensor_tensor(out=sog[:, vs, :], in0=PS[OGB], in1=esc, op=ALU.mult), reads=[bk(OGB), "t1"], writes=[("sog", vs)])
        yield

    AB, OGB2, UB = 6, 7, 6

    def stage_G2(t, j):
        vs = t % 2
        ys = t % 8
        for h in range(4):
            c = h // 2
            fw.op("pe", lambda e, h=h, c=c: e.matmul(out=PS[AB][:, h * 128:(h + 1) * 128], lhsT=kdT[:, c, j * 128:(j + 1) * 128],
                                                     rhs=qd_pad[:, h, j * 128:(j + 1) * 128], start=True, stop=True),
                  reads=[("kdT", c), "qd_pad"], writes=[bk(AB)], sig=(h == 3))
        yield
        fw.op("dve", lambda e: e.tensor_tensor(out=AT, in0=PS[AB].rearrange("p (h i) -> p h i", h=4), in1=gmask.unsqueeze(1).to_broadcast([128, 4, 128]), op=ALU.mult),
              reads=[bk(AB), "gmask"], writes=["AT"])
        yield
        for h in range(4):
            c = h // 2
            fw.op("pe", lambda e, h=h: e.matmul(out=PS[OGB2][:, h * 128:(h + 1) * 128], lhsT=AT[:, h, :], rhs=vg[:, vs, h * 128:(h + 1) * 128], start=True, stop=False),
                  reads=["AT", ("vg", vs)], writes=[bk(OGB2)], sig=False)
            fw.op("pe", lambda e, h=h, c=c: e.matmul(out=PS[OGB2][:, h * 128:(h + 1) * 128], lhsT=qd_pad[:, h, j * 128:(j + 1) * 128], rhs=Sbf[:, c, :], start=False, stop=True),
                  reads=["qd_pad", "Sbf"], writes=[bk(OGB2)], sig=(h == 3))
        yield
        for c in range(2):
            fw.op("pe", lambda e, c=c: e.matmul(out=PS[UB][:, c * 256:(c + 1) * 256], lhsT=kdec[:, j, c * 128:(c + 1) * 128], rhs=vg[:, vs, c * 256:(c + 1) * 256], start=True, stop=True),
                  reads=["kdec", ("vg", vs)], writes=[bk(UB)], sig=(c == 1))
        yield
        for c in range(2):
            for hh in range(2):
                fw.op("dve", lambda e, c=c, hh=hh: e.scalar_tensor_tensor(
                    out=S[hh * 64:(hh + 1) * 64, c, :], in0=S[hh * 64:(hh + 1) * 64, c, :], scalar=elast[hh * 64:(hh + 1) * 64, c, j:j + 1],
                    in1=PS[UB][hh * 64:(hh + 1) * 64, c * 256 + hh * 128:c * 256 + (hh + 1) * 128], op0=ALU.mult, op1=ALU.add),
                    reads=["S", ("elast", c), bk(UB)], writes=["S"])
        fw.op("act", lambda e: e.activation(out=Sbf, in_=S, func=AF.Copy), reads=["S"], writes=["Sbf"])
        yield
        fw.op("act", lambda e: e.activation(out=sqB, in_=PS[OGB2], func=AF.Square), reads=[bk(OGB2)], writes=["sqB"])
        fw.op("dve", lambda e: e.tensor_reduce(out=ssg, in_=sqB.rearrange("p (h d) -> p h d", h=4), axis=AX.X, op=ALU.add), reads=["sqB"], writes=["ssg"])
        rstd_act(rg, ssg, 128, 0.0, ["ssg"], ["rg"])
        fw.op("dve", lambda e: e.tensor_tensor(out=t1.rearrange("p (h d) -> p h d", h=4), in0=PS[OGB2].rearrange("p (h d) -> p h d", h=4),
                                               in1=rg.unsqueeze(2).to_broadcast([128, 4, 128]), op=ALU.mult),
              reads=[bk(OGB2), "rg"], writes=["t1"])
        yield
        fw.op("dve", lambda e: e.tensor_tensor(out=yg, in0=t1, in1=sog[:, vs, :], op=ALU.mult), reads=["t1", ("sog", vs)], writes=["yg"])
        yield
        for i in range(4):
            fw.op("pe", lambda e, i=i: e.transpose(out=PSB[6][:, i * 128:(i + 1) * 128], in_=yg[:, i * 128:(i + 1) * 128], identity=ident_b),
                  reads=["yg", "ident_b"], writes=[bk(6)], sig=(i == 3))
        yield
        fw.op("act", lambda e: e.activation(out=yG.rearrange("p a b -> p (a b)"), in_=PSB[6][:, 0:512], func=AF.Copy, scale=vecs[:, 68:69]),
              reads=[bk(6), "vecs"], writes=["yG"], wtag={"yG": t})
        yield

    WB = (6, 7)

    def stage_W(t):
        ys = t % 8
        for dh in range(2):
            for k in range(8):
                fw.op("pe", lambda e, dh=dh, k=k: e.matmul(out=PS[WB[dh]], lhsT=(yA[:, ys, k, :] if k < 4 else yG[:, k - 4, :]), rhs=wout_sb[:, k, dh * 512:(dh + 1) * 512], start=(k == 0), stop=(k == 7)),
                      reads=[("yA", ys), "yG", "wout"], writes=[bk(WB[dh])], sig=(k == 7), rtag={("yA", ys): t, "yG": t})
            fw.op("dve", lambda e, dh=dh: e.tensor_tensor(out=X1[:, t, dh * 512:(dh + 1) * 512], in0=PS[WB[dh]], in1=X1[:, t, dh * 512:(dh + 1) * 512], op=ALU.add),
                  reads=[bk(WB[dh]), ("X1", t)], writes=[("X1", t)])
            yield

    def chain(*gens):
        for g_ in gens:
            yield from g_

    def run_streams(streams):
        streams = [s for s in streams if s is not None]
        if SEQ_STREAMS:
            for s in streams:
                for _ in s:
                    pass
            return
        if not GREEDY:
            while streams:
                nxt = []
                for s in streams:
                    try:
                        next(s)
                        nxt.append(s)
                    except StopIteration:
                        pass
                streams = nxt
            return
        ready = [0.0] * len(streams)
        alive = list(range(len(streams)))
        while alive:
            i = min(alive, key=lambda j: ready[j])
            fw.cur_stream_end = 0.0
            try:
                next(streams[i])
                ready[i] = max(ready[i], fw.cur_stream_end)
            except StopIteration:
                alive.remove(i)

    def interleave(a, b):
        gens = [a, b]
        while gens:
            nxt = []
            for g_ in gens:
                try:
                    next(g_)
                    nxt.append(g_)
                    yield
                except StopIteration:
                    pass
            gens = nxt

    def stream_A(g):
        ts = list(range(GROUPS[g][0], GROUPS[g][0] + GROUPS[g][1]))
        parts = [stage_T1(ts[0]), stage_T2(ts[0])]
        for i, t in enumerate(ts):
            parts.append(stage_T3(t))
            if i + 1 < len(ts) and PIPE_A:
                parts.append(interleave(stage_T4(t), chain(stage_T1(ts[i + 1]), stage_T2(ts[i + 1]))))
            else:
                parts.append(stage_T4(t))
                if i + 1 < len(ts):
                    parts.append(stage_T1(ts[i + 1]))
                    parts.append(stage_T2(ts[i + 1]))
        return chain(*parts)

    def stream_B(g):
        T0, n_ = GROUPS[g]
        return chain(stage_G1(T0, n_), *[chain(stage_B0(t), stage_G2(t, t - T0), stage_W(t)) for t in range(T0, T0 + n_)])

    def early_exit():
        fw.barrier()
        for g_ in range(4):
            fw.dma("sp", out_v[:, 4 * g_:4 * g_ + 4, :], X1[:, 4 * g_:4 * g_ + 4, :], reads=[("X1", t) for t in range(4 * g_, 4 * g_ + 4)], writes=[("out", g_)])
        fw.barrier()
        return nc, fw, None
    if DBG_STAGE == 10:
        return early_exit()
    run_streams([stream_A(0) if 'A0' not in DBG_SKIP else None, stream_M1() if 'M1' not in DBG_SKIP else None])
    if DBG_STAGE == 11:
        return early_exit()
    m2 = stream_M2()
    NGRP = len(GROUPS)

    class GStream:
        def __init__(self, make, first, n):
            self.make = make
            self.cur = first
            self.n = n
            self.gen = None
            self.ready = 0.0

        def finished(self):
            return self.cur >= self.n and self.gen is None

        def at_boundary(self):
            return self.gen is None

        def step(self):
            if self.gen is None:
                self.gen = self.make(self.cur)
            fw.cur_stream_end = 0.0
            try:
                next(self.gen)
                self.ready = max(self.ready, fw.cur_stream_end)
            except StopIteration:
                self.gen = None
                self.cur += 1

    class PlainStream(GStream):
        def __init__(self, gen):
            self.gen = gen
            self.ready = 0.0
            self.done = False

        def finished(self):
            return self.done

        def at_boundary(self):
            return False

        def step(self):
            fw.cur_stream_end = 0.0
            try:
                next(self.gen)
                self.ready = max(self.ready, fw.cur_stream_end)
            except StopIteration:
                self.done = True

    STA = GStream(stream_A, 1, NGRP)
    STB = GStream(stream_B, 0, NGRP)
    STM = PlainStream(m2) if 'M2' not in DBG_SKIP else None
    while True:
        cands = []
        if not STA.finished() and not (STA.at_boundary() and (STA.cur >= NGRP or STA.cur - STB.cur > LEAD)):
            cands.append(STA)
        if not STB.finished() and not (STB.at_boundary() and not (STB.cur < STA.cur)):
            cands.append(STB)
        if STM is not None and not STM.finished():
            cands.append(STM)
        if not cands:
            assert STA.finished() and STB.finished(), (STA.cur, STB.cur)
            break
        s_ = min(cands, key=lambda s: s.ready - (PRIO_B if s is STB else 0.0)) if GREEDY else cands[0]
        s_.step()

    if stage == 1:
        fw.barrier()
        for g in range(4):
            fw.dma("sp", out_v[:, 4 * g:4 * g + 4, :], X1[:, 4 * g:4 * g + 4, :], reads=[("X1", t) for t in range(4 * g, 4 * g + 4)], writes=[("out", g)])
        fw.barrier()
        return nc, fw, None

    fw.barrier()
    reg.reset()
    h2T = reg.get([8, T], BF16)
    wb = [dict(w1=reg.get([EB, 8, 256], BF16), w3=reg.get([EB, 8, 256], BF16), w2=reg.get([EB, 2, 1024], BF16)) for _ in range(2)]
    xn2 = reg.get([2, 1024], F32)
    xT32 = reg.get([2, 1024], F32)
    wr_sb = reg.get([8, 20], F32)
    shrep = reg.get([128], F32)
    cb_sb = reg.get([20], F32)
    L = reg.get([NT, 20], F32)
    g2b = reg.get([1024], F32)
    comb_pad = reg.get([NT, 128], BF16)
    cT_sb = reg.get([T], BF16)
    Cb = reg.get([2, 512], BF16)
    s_sb = reg.get([2, 512], BF16)
    gc_sb = reg.get([2, 512], BF16)
    hid = reg.get([2, EB * 2, 512], BF16)
    sq2 = reg.get([1024], BF16)
    gmax = reg.get([NT], F32)
    goh = reg.get([NT, 4], F32)
    gex = reg.get([NT, 4], F32)
    pg = reg.get([NT], F32)
    tmp16 = reg.get([NT, 16], F32)
    esel = reg.get([NT, 4], F32)
    esel2 = reg.get([NT, 4], F32)
    m1 = reg.get([NT], F32)
    m2 = reg.get([NT], F32)
    oh1 = reg.get([NT, 4], F32)
    oh2 = reg.get([NT, 4], F32)
    rr = reg.get([NT], F32)
    wa = reg.get([NT], F32)
    wb2 = reg.get([NT], F32)
    wsel = reg.get([NT, 4], F32)
    print("phase2 region bytes", reg.off, "of", reg.n)

    def load_expert_batch(bi):
        buf = wb[bi % 2]
        for j in range(EB):
            e_ = bi * EB + j
            fw.dma("pool", buf["w1"][:, j, :, :], w1_d[e_].rearrange("(k p) f -> p k f", p=128), writes=[("w1", bi % 2, j)])
            fw.dma("pool", buf["w3"][:, j, :, :], w3_d[e_].rearrange("(k p) f -> p k f", p=128), writes=[("w3", bi % 2, j)])
            fw.dma("pool", buf["w2"][:, j, :, :], w2_d[e_].rearrange("(k p) d -> p k d", p=128), writes=[("w2", bi % 2, j)])

    fw.dma("sp", wr_sb, wr_d.rearrange("(k p) n -> p k n", p=128), writes=["wr"])
    load_expert_batch(0)
    load_expert_batch(1)

    for k in range(8):
        fw.op("dve", lambda e, k=k: e.tensor_scalar(out=shrep, in0=ident_f, scalar1=mod[:, 40 + k:41 + k], scalar2=None, op0=ALU.mult),
              reads=["ident_f", ("mod", 5)], writes=["shrep"])
        fw.op("pe", lambda e, k=k: e.matmul(out=PS[k // 4][:, (k % 4) * 128:(k % 4 + 1) * 128], lhsT=ones_f, rhs=shrep, start=True, stop=True),
              reads=["ones_f", "shrep"], writes=[bk(k // 4)])
    for hh in range(2):
        fw.op("act", lambda e, hh=hh: e.activation(out=g2b[:, hh * 512:(hh + 1) * 512], in_=PS[hh], func=AF.Copy), reads=[bk(hh)], writes=["g2b"])
    for k in range(8):
        fw.op("dve", lambda e, k=k: e.tensor_copy(out=shrep, in_=mod[:, 24 + k:25 + k].to_broadcast([128, 128])),
              reads=[("mod", 3)], writes=["shrep"])
        fw.op("pe", lambda e, k=k: e.matmul(out=PS[2][:, 0:20], lhsT=shrep, rhs=wr_sb[:, k, :], start=(k == 0), stop=(k == 7)),
              reads=["shrep", "wr"], writes=[bk(2)])
    fw.op("dve", lambda e: e.tensor_tensor(out=cb_sb, in0=PS[2][:, 0:20], in1=brg, op=ALU.add), reads=[bk(2), "brg"], writes=["cb"])
    for k in range(8):
        fw.op("dve", lambda e, k=k: e.tensor_scalar(out=wr_sb[:, k, :], in0=wr_sb[:, k, :], scalar1=gs2[:, k:k + 1], scalar2=None, op0=ALU.mult),
              reads=["wr", "gs2", bk(2)], writes=["wr"])
    fw.op("dve", lambda e: e.memset(comb_pad, 0.0), writes=["comb_pad"])
    fw.op("dve", lambda e: e.memset(cT_sb, 0.0), writes=["cT"])

    def stage_N(t):
        p_ = t % 2
        xs = p_
        b0_, b1_, rb = 2 * p_, 2 * p_ + 1, 4 + p_
        tb = (b0_, b1_)
        fw.op("act", lambda e: e.activation(out=sq2, in_=X1[:, t, :], func=AF.Square, accum_out=ssx[:, t:t + 1]),
              reads=[("X1", t)], writes=["sq2", ("ssx", t)])
        rstd_act(rsx[:, t:t + 1], ssx[:, t:t + 1], D, 0.0, [("ssx", t)], [("rsx", t)])
        fw.op("dve", lambda e: e.tensor_scalar(out=xn2[:, xs, :], in0=X1[:, t, :], scalar1=rsx[:, t:t + 1], scalar2=None, op0=ALU.mult),
              reads=[("X1", t), ("rsx", t)], writes=[("xn2", xs)])
        yield
        for k in range(8):
            fw.op("pe", lambda e, k=k: e.transpose(out=PS[tb[k // 4]][:, (k % 4) * 128:(k % 4 + 1) * 128], in_=xn2[:, xs, k * 128:(k + 1) * 128], identity=ident_f),
                  reads=[("xn2", xs), "ident_f"], writes=[bk(tb[k // 4])], sig=(k % 4 == 3))
        yield
        for hh in range(2):
            fw.op("dve", lambda e, hh=hh: e.tensor_copy(out=xT32[:, xs, hh * 512:(hh + 1) * 512], in_=PS[tb[hh]]), reads=[bk(tb[hh])], writes=[("xT32", xs)])
        yield
        for k in range(8):
            evac_affine(h2T[:, k, t * 128:(t + 1) * 128], PS[tb[k // 4]][:, (k % 4) * 128:(k % 4 + 1) * 128], gs2[:, k:k + 1], mod[:, 24 + k:25 + k],
                        [bk(tb[k // 4]), "gs2", ("mod", 3)], [("h2T", t // 4)])
        yield
        for k in range(8):
            fw.op("pe", lambda e, k=k: e.matmul(out=PS[rb][:, 0:20], lhsT=xT32[:, xs, k * 128:(k + 1) * 128], rhs=wr_sb[:, k, :], start=(k == 0), stop=(k == 7)),
                  reads=[("xT32", xs), "wr"], writes=[bk(rb)], sig=(k == 7))
        yield
        fw.op("dve", lambda e: e.tensor_tensor(out=L[:, t, :], in0=PS[rb][:, 0:20], in1=cb_sb, op=ALU.add), reads=[bk(rb), "cb"], writes=["L"])
        yield

    run_streams([chain(*[stage_N(t) for t in range(0, NT, 2)]), chain(*[stage_N(t) for t in range(1, NT, 2)])])

    def dv(fn, reads, writes):
        fw.op("dve", fn, reads=reads, writes=writes)
    gl = L[:, :, 0:4]
    el = L[:, :, 4:20].rearrange("p t (g i) -> p t g i", g=4)
    dv(lambda e: e.tensor_reduce(out=gmax, in_=gl, axis=AX.X, op=ALU.max), ["L"], ["gmax"])
    dv(lambda e: e.tensor_tensor(out=goh, in0=gl, in1=gmax.unsqueeze(2).to_broadcast([128, NT, 4]), op=ALU.is_equal), ["L", "gmax"], ["goh"])
    dv(lambda e: e.tensor_tensor(out=gex, in0=gl, in1=gmax.unsqueeze(2).to_broadcast([128, NT, 4]), op=ALU.subtract), ["L", "gmax"], ["gex"])
    fw.op("act", lambda e: e.activation(out=gex, in_=gex, func=AF.Exp), reads=["gex"], writes=["gex"])
    dv(lambda e: e.tensor_reduce(out=pg, in_=gex, axis=AX.X, op=ALU.add), ["gex"], ["pg"])
    dv(lambda e: e.reciprocal(out=pg, in_=pg), ["pg"], ["pg"])
    dv(lambda e: e.tensor_tensor(out=tmp16.rearrange("p t (g i) -> p t g i", g=4), in0=el, in1=goh.unsqueeze(3).to_broadcast([128, NT, 4, 4]), op=ALU.mult),
       ["L", "goh"], ["tmp16"])
    dv(lambda e: e.tensor_reduce(out=esel, in_=tmp16.rearrange("p t (g i) -> p t i g", g=4), axis=AX.X, op=ALU.add), ["tmp16"], ["esel"])
    dv(lambda e: e.tensor_reduce(out=m1, in_=esel, axis=AX.X, op=ALU.max), ["esel"], ["m1"])
    dv(lambda e: e.tensor_tensor(out=oh1, in0=esel, in1=m1.unsqueeze(2).to_broadcast([128, NT, 4]), op=ALU.is_equal), ["esel", "m1"], ["oh1"])
    dv(lambda e: e.scalar_tensor_tensor(out=esel2, in0=oh1, scalar=-1e30, in1=esel, op0=ALU.mult, op1=ALU.add), ["oh1", "esel"], ["esel2"])
    dv(lambda e: e.tensor_reduce(out=m2, in_=esel2, axis=AX.X, op=ALU.max), ["esel2"], ["m2"])
    dv(lambda e: e.tensor_tensor(out=oh2, in0=esel2, in1=m2.unsqueeze(2).to_broadcast([128, NT, 4]), op=ALU.is_equal), ["esel2", "m2"], ["oh2"])
    dv(lambda e: e.tensor_tensor(out=rr, in0=m2, in1=m1, op=ALU.subtract), ["m1", "m2"], ["rr"])
    fw.op("act", lambda e: e.activation(out=rr, in_=rr, func=AF.Exp), reads=["rr"], writes=["rr"])
    dv(lambda e: e.tensor_scalar_add(out=wa, in0=rr, scalar1=1.0), ["rr"], ["wa"])
    dv(lambda e: e.reciprocal(out=wa, in_=wa), ["wa"], ["wa"])
    dv(lambda e: e.tensor_tensor(out=wa, in0=wa, in1=pg, op=ALU.mult), ["wa", "pg"], ["wa"])
    dv(lambda e: e.tensor_tensor(out=wb2, in0=wa, in1=rr, op=ALU.mult), ["wa", "rr"], ["wb2"])
    dv(lambda e: e.tensor_tensor(out=wsel, in0=oh1, in1=wa.unsqueeze(2).to_broadcast([128, NT, 4]), op=ALU.mult), ["oh1", "wa"], ["wsel"])
    dv(lambda e: e.tensor_tensor(out=oh2, in0=oh2, in1=wb2.unsqueeze(2).to_broadcast([128, NT, 4]), op=ALU.mult), ["oh2", "wb2"], ["oh2"])
    dv(lambda e: e.tensor_tensor(out=wsel, in0=wsel, in1=oh2, op=ALU.add), ["wsel", "oh2"], ["wsel"])
    dv(lambda e: e.tensor_tensor(out=comb_pad[:, :, 0:16].rearrange("p t (g i) -> p t g i", g=4), in0=goh.unsqueeze(3).to_broadcast([128, NT, 4, 4]),
                                 in1=wsel.unsqueeze(2).to_broadcast([128, NT, 4, 4]), op=ALU.mult), ["goh", "wsel"], ["comb_pad"])
    for half in range(2):
        for i in range(8):
            t = half * 8 + i
            fw.op("pe", lambda e, t=t, i=i: e.transpose(out=PSB[3][:, i * 128:(i + 1) * 128], in_=comb_pad[:, t, :], identity=ident_b),
                  reads=["comb_pad", "ident_b"], writes=[bk(3)], sig=(i == 7))
        fw.op("act", lambda e, half=half: e.activation(out=cT_sb[0:16, half * 1024:(half + 1) * 1024], in_=PSB[3][0:16, :], func=AF.Copy), reads=[bk(3)], writes=["cT"])

    NB = NEXP // EB
    AG = (0, 1, 2, 3)
    CBK = 4
    YB = (5, 6, 7)
    yi = [0]
    ui = [0]
    def moe_fold(bi):
        buf = wb[bi % 2]
        for j in range(EB):
            for fc in range(2):
                fw.op("dve", lambda e, j=j, fc=fc: e.tensor_tensor(out=buf["w2"][:, j, fc, :], in0=buf["w2"][:, j, fc, :], in1=g2b, op=ALU.mult),
                      reads=[("w2", bi % 2, j), "g2b"], writes=[("w2", bi % 2, j)])

    def moe_p1(bi, tg):
        buf = wb[bi % 2]
        hs = tg % 2
        for j in range(EB):
            e_ = bi * EB + j
            fw.op("pe", lambda e, e_=e_: e.matmul(out=PS[CBK], lhsT=sel[:, e_, :], rhs=cT_sb[:, tg * 512:(tg + 1) * 512], start=True, stop=True),
                  reads=["sel", "cT"], writes=[bk(CBK)])
            cs = ui[0] % 2
            fw.op("act", lambda e, cs=cs: e.activation(out=Cb[:, cs, :], in_=PS[CBK], func=AF.Copy), reads=[bk(CBK)], writes=[("Cb", cs)])
            for fc in range(2):
                u = ui[0] % 2
                ab, gb = AG[2 * u], AG[2 * u + 1]
                ui[0] += 1
                for (bank, wname) in ((ab, "w1"), (gb, "w3")):
                    for k in range(8):
                        fw.op("pe", lambda e, bank=bank, wname=wname, j=j, fc=fc, k=k: e.matmul(
                            out=PS[bank], lhsT=buf[wname][:, j, k, fc * 128:(fc + 1) * 128], rhs=h2T[:, k, tg * 512:(tg + 1) * 512],
                            start=(k == 0), stop=(k == 7)),
                            reads=[(wname, bi % 2, j), ("h2T", tg)], writes=[bk(bank)], sig=(k == 7))
                fw.op("act", lambda e, ab=ab, u=u: e.activation(out=s_sb[:, u, :], in_=PS[ab], func=AF.Silu), reads=[bk(ab)], writes=[("s", u)])
                fw.op("dve", lambda e, gb=gb, u=u, cs=cs: e.tensor_tensor(out=gc_sb[:, u, :], in0=PS[gb], in1=Cb[:, cs, :], op=ALU.mult),
                      reads=[bk(gb), ("Cb", cs)], writes=[("gc", u)])
                fw.op("dve", lambda e, u=u, j=j, fc=fc: e.tensor_tensor(out=hid[:, hs, j * 2 + fc, :], in0=s_sb[:, u, :], in1=gc_sb[:, u, :], op=ALU.mult),
                      reads=[("s", u), ("gc", u)], writes=[("hid", hs)], wtag={("hid", hs): (bi, tg)})

    def moe_p2(bi, tg):
        buf = wb[bi % 2]
        hs = tg % 2
        for tt in range(4):
            t = tg * 4 + tt
            for dh in range(2):
                yb = YB[yi[0] % 3]
                yi[0] += 1
                n = EB * 2
                for q in range(n):
                    j, fc = q // 2, q % 2
                    fw.op("pe", lambda e, yb=yb, q=q, j=j, fc=fc, tt=tt, dh=dh: e.matmul(
                        out=PS[yb], lhsT=hid[:, hs, q, tt * 128:(tt + 1) * 128], rhs=buf["w2"][:, j, fc, dh * 512:(dh + 1) * 512],
                        start=(q == 0), stop=(q == n - 1)),
                        reads=[("hid", hs), ("w2", bi % 2, j)], writes=[bk(yb)], sig=(q == n - 1), rtag={("hid", hs): (bi, tg)})
                fw.op("dve", lambda e, yb=yb, t=t, dh=dh: e.tensor_tensor(out=X1[:, t, dh * 512:(dh + 1) * 512], in0=PS[yb], in1=X1[:, t, dh * 512:(dh + 1) * 512], op=ALU.add),
                      reads=[bk(yb), ("X1", t)], writes=[("X1", t)])
        if bi == NB - 1:
            fw.dma("sp", out_v[:, 4 * tg:4 * tg + 4, :], X1[:, 4 * tg:4 * tg + 4, :], reads=[("X1", t) for t in range(4 * tg, 4 * tg + 4)], writes=[("out", tg)])
        if tg == 3 and bi + 2 < NB:
            load_expert_batch(bi + 2)

    prev_ = None
    for bi in range(NB):
        for tg in range(4):
            if tg == 0:
                moe_fold(bi)
            moe_p1(bi, tg)
            if prev_ is not None and MOE_PIPE:
                moe_p2(*prev_)
            if MOE_PIPE:
                prev_ = (bi, tg)
            else:
                moe_p2(bi, tg)
    if prev_ is not None:
        moe_p2(*prev_)
    fw.barrier()
    print("ops", fw.nops, "sem counts", fw.cnt)
    return nc, fw, None


def host_inputs(inputs, b):
    f = np.float32
    g = lambda k: np.asarray(inputs[k], dtype=f)
    P = [0, 4, 1, 5, 2, 6, 3, 7]
    w_in = g("w_in")[0]
    qcols = np.concatenate([np.arange(h * 64, (h + 1) * 64) for h in P])
    w_in_tm = np.concatenate([w_in[:, qcols], w_in[:, 512:768], w_in[:, 1280:1792], w_in[:, 1808:2320]], axis=1)
    w_in_fm = np.concatenate([w_in[:, 768:1280], w_in[:, 1792:1920]], axis=1)
    col = lambda v: np.ascontiguousarray(v.reshape(-1, 128).T)
    vecs = np.concatenate([
        col(g("b_ada")[0]), col(g("g_norm1")[0]), col(g("g_norm2")[0]), col(g("g_att_out")[0]), col(g("g_gla_out")[0]),
        col(g("b_gk")[0]), col(np.tile(g("q_norm")[0], 2)), col(np.tile(g("k_norm")[0], 2)), col(g("c")[b]),
    ], axis=1)
    assert vecs.shape == (128, NV)
    sinks_b = np.ascontiguousarray(np.broadcast_to(g("sinks")[0][None, :], (128, 8)))
    brg = np.ascontiguousarray(np.broadcast_to(np.concatenate([g("b_group")[0], g("b_router")[0]])[None, :], (128, 20)))
    slopes = np.exp2(-8.0 * np.arange(1, 9) / 8).astype(f)
    kj = np.arange(128)[:, None]
    qi = np.arange(128)[None, :]
    abias = np.zeros((128, 4, 512), f)
    for kv in range(2):
        for gg in range(4):
            h = kv * 4 + gg
            dcur = (qi - kj).astype(f)
            abias[:, kv * 2 + 0, gg * 128:(gg + 1) * 128] = np.where(dcur >= 0, -slopes[h] * dcur, -30000.0)
            dprev = (qi - kj + 128).astype(f)
            abias[:, kv * 2 + 1, gg * 128:(gg + 1) * 128] = np.where(dprev < 128, -slopes[h] * dprev, -30000.0)
    gmask = (kj <= qi).astype(f)
    sel = np.zeros((128, 16, 128), f)
    for e in range(16):
        sel[e, e, :] = 1.0
    w_rt = np.concatenate([g("w_group")[0], g("w_router")[0]], axis=1)
    return {
        "x": np.ascontiguousarray(g("x")[b]), "vecs": vecs, "sinks_b": sinks_b, "brg": brg,
        "ident": np.eye(128, dtype=f), "abias": abias, "gmask": gmask, "sel": sel,
        "w_ada": g("w_ada")[0], "w_in_tm": np.ascontiguousarray(w_in_tm), "w_in_fm": np.ascontiguousarray(w_in_fm),
        "w_gk2": g("w_gk2")[0], "w_out": g("w_out")[0], "w_rt": np.ascontiguousarray(w_rt),
        "w1": g("w1")[0], "w3": g("w3")[0], "w2": g("w2")[0],
    }


def kernel(**inputs):
    nc, fw, _ = build_nc()
    in_maps = [host_inputs(inputs, b) for b in range(8)]
    res = run_bass_kernel_spmd(nc, in_maps, core_ids=list(range(8)))
    return np.stack([r["out"] for r in res.results], axis=0).astype(np.float32)
```

```python
import math
import numpy as np
import concourse.bass as bass
import concourse.mybir as mybir
from concourse.bass_utils import run_bass_kernel_spmd

F32 = mybir.dt.float32
BF16 = mybir.dt.bfloat16
AF = mybir.ActivationFunctionType
ALU = mybir.AluOpType
AX = mybir.AxisListType

T = 2048
D = 1024
NT = 16
EPS = 1e-6
LN8 = math.log(8.0)
NV = 81
NEXP = 16
EB = 2
SAME_ENGINE_FULL_SYNC = False
SEQ_STREAMS = False
GREEDY = True
PE_SLOW = 1.0
PIPE_A = True
GROUPS = tuple((2 * i, 2) for i in range(8))
PRIO_B = 0.0
MOE_PIPE = True
M_LAT = 0.15
M_ACT0 = 0.22
M_DVE0 = 0.12
LEAD = 1
DBG_SKIP = ()
DBG_STAGE = 99
MODCOL = 320
DBG_NT = 4
DBG_SB = (3, 4)
DBG_OB = (5, 4)


class FW:
    def __init__(self, nc):
        self.nc = nc
        self.eng = {"pe": nc.tensor, "act": nc.scalar, "dve": nc.vector, "pool": nc.gpsimd, "sp": nc.sync}
        self.sem = {e: nc.alloc_semaphore("s_" + e) for e in self.eng}
        self.cnt = {e: 0 for e in self.eng}
        self.seen = {e: {} for e in self.eng}
        self.dsem = {q: [nc.alloc_semaphore("d_%s%d" % (q, i)) for i in range(14)] for q in ("sp", "pool")}
        self.dval = {q: [0] * 14 for q in ("sp", "pool")}
        self.drr = {"sp": 0, "pool": 0}
        self.lastw = {}
        self.readers = {}
        self.pe_pending = []
        self.nops = 0
        self.log = None
        self.tags = {}
        self.eng_free = {e: 0.0 for e in self.eng}
        self.tok_end = {}
        self.cur_stream_end = 0.0

    def _deps(self, eng, reads, writes, is_dma):
        toks = []
        for k in reads:
            w = self.lastw.get(k)
            if w is not None:
                toks.append((w, "raw"))
            if isinstance(k, tuple) and k[0] == "ps":
                for r in self.readers.get(k, ()):
                    toks.append((r, "rar"))
        for k in writes:
            w = self.lastw.get(k)
            if w is not None:
                toks.append((w, "waw"))
            for r in self.readers.get(k, ()):
                toks.append((r, "war"))
        need = {}
        for tok, kind in toks:
            if tok[0] == "c":
                src = tok[1]
                if src == eng and not is_dma:
                    if eng == "pe":
                        continue
                    if kind != "raw" and not SAME_ENGINE_FULL_SYNC:
                        continue
                assert tok[2] is not None, "unresolved PE token"
                key = ("c", src)
                need[key] = max(need.get(key, 0), tok[2])
            else:
                key = ("d", tok[1], tok[2])
                need[key] = max(need.get(key, 0), tok[3])
        waits = []
        for key, val in need.items():
            if self.seen[eng].get(key, 0) >= val:
                continue
            self.seen[eng][key] = val
            if key[0] == "c":
                waits.append((self.sem[key[1]], val))
            else:
                waits.append((self.dsem[key[1]][key[2]], val))
        return waits

    def _record(self, tok, reads, writes):
        for k in reads:
            self.readers.setdefault(k, []).append(tok)
        for k in writes:
            self.lastw[k] = tok
            self.readers[k] = []

    def _tagcheck(self, reads, writes, rtag, wtag, keep_tag):
        for k in reads:
            if rtag is not None and k in rtag:
                assert self.tags.get(k) == rtag[k], ("stale read", k, self.tags.get(k), rtag[k])
        if not keep_tag:
            for k in writes:
                self.tags[k] = (wtag or {}).get(k)

    def op(self, eng, fn, reads=(), writes=(), sig=True, rtag=None, wtag=None, keep_tag=False):
        self._tagcheck(reads, writes, rtag, wtag, keep_tag)
        waits = self._deps(eng, reads, writes, False)
        e = self.eng[eng]
        for s, v in waits[1:]:
            e.wait_ge(s, v)
        rec = {}
        ins = fn(_EngProxy(e, rec))
        self._model(eng, rec, reads, writes)
        if waits:
            ins._wait_ge(waits[0][0], waits[0][1])
        tok = ["c", eng, None]
        if eng == "pe" and not sig:
            self.pe_pending.append(tok)
        else:
            self.cnt[eng] += 1
            ins.then_inc(self.sem[eng], 1)
            tok[2] = self.cnt[eng]
            if eng == "pe":
                for p in self.pe_pending:
                    p[2] = tok[2]
                self.pe_pending = []
        self._record(tok, reads, writes)
        self.nops += 1
        if self.log is not None:
            import sys as _s
            fr = _s._getframe(1)
            nm = None
            ln = fr.f_lineno
            while fr is not None:
                if fr.f_code.co_name.startswith(("stage_", "stream_", "mod_chunk")):
                    nm = fr.f_code.co_name
                    break
                fr = fr.f_back
            self.log.append((eng, tok[2], (nm, ln), list(reads), list(writes), [(str(s), v) for s, v in waits]))
        return ins

    def dma(self, q, out, in_, reads=(), writes=()):
        waits = self._deps(q, reads, writes, True)
        i = self.drr[q]
        self.drr[q] = (i + 1) % len(self.dsem[q])
        prev = self.dval[q][i]
        key = ("d", q, i)
        if prev and self.seen[q].get(key, 0) < prev:
            self.seen[q][key] = prev
            waits.append((self.dsem[q][i], prev))
        e = self.eng[q]
        for s, v in waits:
            e.wait_ge(s, v)
        e.dma_start(out=out, in_=in_).then_inc(self.dsem[q][i], 16)
        self.dval[q][i] = prev + 16
        tok = ["d", q, i, prev + 16]
        self._record(tok, reads, writes)
        try:
            nb = 1
            for d_ in in_.shape:
                nb *= d_
            nb *= 4
        except Exception:
            nb = 1 << 20
        st = max(self.eng_free[q], max([self.tok_end.get(("w", k), 0.0) for k in list(reads) + list(writes)] + [self.tok_end.get(("r", k), 0.0) for k in writes] + [0.0]))
        en = st + 2.0 + nb / 180e3
        self.eng_free[q] = st + nb / 180e3
        for k in writes:
            self.tok_end[("w", k)] = en
            self.tok_end[("r", k)] = 0.0
        self.nops += 1

    def _model(self, eng, rec, reads, writes):
        kw = rec.get("kw", {})
        name = rec.get("name", "")

        def fsz(ap):
            try:
                sh = ap.shape
                n = 1
                for d in sh[1:]:
                    n *= d
                return n
            except Exception:
                return 128
        if eng == "pe":
            if name == "transpose":
                n = 128
                dur = 0.07 if kw["in_"].dtype == BF16 else 0.3
            else:
                n = fsz(kw["rhs"])
                dur = (max(n, 64) / 2400.0 + 0.02) * PE_SLOW
                if kw["rhs"].dtype == F32:
                    dur *= 4
        elif eng == "act":
            n = fsz(kw.get("out"))
            dur = M_ACT0 + n / 1200.0 + (0.1 if kw.get("accum_out") is not None else 0.0)
        else:
            n = fsz(kw.get("out")) if kw.get("out") is not None else 128
            dur = M_DVE0 + n / 960.0
            if name == "tensor_tensor_scan":
                dur = 0.12 + 2 * n / 960.0
            if name == "reciprocal":
                dur = 0.12 + 6 * n / 960.0
        ready = 0.0
        for k in reads:
            ready = max(ready, self.tok_end.get(("w", k), 0.0))
        for k in writes:
            ready = max(ready, self.tok_end.get(("w", k), 0.0), self.tok_end.get(("r", k), 0.0))
        start = max(self.eng_free[eng], ready + M_LAT)
        end = start + dur
        self.eng_free[eng] = end
        for k in reads:
            self.tok_end[("r", k)] = max(self.tok_end.get(("r", k), 0.0), end)
        for k in writes:
            self.tok_end[("w", k)] = end
            self.tok_end[("r", k)] = 0.0
        self.cur_stream_end = max(self.cur_stream_end, end)

    def barrier(self):
        for e in self.eng:
            for src in self.eng:
                if src == e:
                    continue
                v = self.cnt[src]
                if v and self.seen[e].get(("c", src), 0) < v:
                    self.seen[e][("c", src)] = v
                    self.eng[e].wait_ge(self.sem[src], v)
            for q in ("sp", "pool"):
                for i, v in enumerate(self.dval[q]):
                    if v and self.seen[e].get(("d", q, i), 0) < v:
                        self.seen[e][("d", q, i)] = v
                        self.eng[e].wait_ge(self.dsem[q][i], v)


class _EngProxy:
    def __init__(self, real, rec):
        self._real = real
        self._rec = rec

    def __getattr__(self, name):
        f = getattr(self._real, name)
        rec = self._rec

        def w(*a, **k):
            rec["name"] = name
            rec["kw"] = k
            return f(*a, **k)
        return w


class Carver:
    def __init__(self, ap, nbytes):
        self.ap = ap
        self.n = nbytes
        self.off = 0

    def get(self, free_shape, dt):
        esz = 4 if dt == F32 else 2
        n = int(np.prod(free_shape)) * esz
        n_al = (n + 63) // 64 * 64
        assert self.off + n_al <= self.n, "carver overflow %d + %d > %d" % (self.off, n_al, self.n)
        v = self.ap[:, self.off // 2:(self.off + n) // 2]
        self.off += n_al
        if dt == F32:
            v = v.bitcast(F32)
        if len(free_shape) == 2:
            v = v.rearrange("p (a b) -> p a b", a=free_shape[0])
        elif len(free_shape) == 3:
            v = v.rearrange("p (a b c) -> p a b c", a=free_shape[0], b=free_shape[1])
        return v

    def reset(self):
        self.off = 0


def build_nc(stage=99, dbg=False):
    nc = bass.Bass("TRN2", target_bir_lowering=False)
    fw = FW(nc)

    def din(name, shape, dt=F32):
        return nc.dram_tensor(name, list(shape), dt, kind="ExternalInput").ap()

    x_d = din("x", [T, D])
    vecs_d = din("vecs", [128, NV])
    sinks_d = din("sinks_b", [128, 8])
    brg_d = din("brg", [128, 20])
    ident_d = din("ident", [128, 128])
    abias_d = din("abias", [128, 4, 512])
    gmask_d = din("gmask", [128, 128])
    sel_d = din("sel", [128, 16, 128])
    wada_d = din("w_ada", [D, 6 * D])
    wintm_d = din("w_in_tm", [D, 1792])
    winfm_d = din("w_in_fm", [D, 640])
    wgk2_d = din("w_gk2", [16, 256])
    wout_d = din("w_out", [D, D])
    wr_d = din("w_rt", [D, 20])
    w1_d = din("w1", [NEXP, D, 256])
    w3_d = din("w3", [NEXP, D, 256])
    w2_d = din("w2", [NEXP, 256, D])
    out_d = nc.dram_tensor("out", [T, D], F32, kind="ExternalOutput").ap()
    dbg_d = nc.dram_tensor("dbg", [128, 4096], F32, kind="ExternalOutput").ap() if dbg else None

    x_v = x_d.rearrange("(t p) d -> p t d", p=128)
    out_v = out_d.rearrange("(t p) d -> p t d", p=128)

    X1 = nc.alloc_sbuf_tensor("X1", [128, NT, D], F32).ap()
    PERS_BYTES = 11776
    pers = Carver(nc.alloc_sbuf_tensor("pers", [128, PERS_BYTES // 2], BF16).ap(), PERS_BYTES)
    REG_BYTES = 135168
    reg = Carver(nc.alloc_sbuf_tensor("reg", [128, REG_BYTES // 2], BF16).ap(), REG_BYTES)

    ident_f = pers.get([128], F32)
    ident_b = pers.get([128], BF16)
    ones_f = pers.get([128], F32)
    ones_b = pers.get([128], BF16)
    abias = pers.get([4, 512], BF16)
    gmask = pers.get([128], BF16)
    sel = pers.get([16, 128], BF16)
    vecs = pers.get([NV], F32)
    es = pers.get([8], F32)
    brg = pers.get([20], F32)
    mod = pers.get([48], F32)
    gs1 = pers.get([8], F32)
    gs2 = pers.get([8], F32)
    nbgk = pers.get([2], F32)
    ssx = pers.get([NT], F32)
    rsx = pers.get([NT], F32)
    sc_b = pers.get([8], BF16)
    tmp8 = pers.get([8], F32)
    dbg_sb = pers.get([16], F32)

    PS = [nc.alloc_psum_tensor("ps%d" % i, [128, 512], F32).ap() for i in range(8)]
    PSB = [p.bitcast(BF16) for p in PS]

    def bk(i):
        return ("ps", i)

    fw.dma("sp", ident_f, ident_d, writes=["ident_f"])
    fw.dma("sp", vecs, vecs_d, writes=["vecs"])
    fw.dma("sp", es, sinks_d, writes=["es"])
    fw.dma("sp", brg, brg_d, writes=["brg"])
    fw.dma("pool", ident_b, ident_d, writes=["ident_b"])
    fw.dma("pool", abias, abias_d, writes=["abias"])
    fw.dma("pool", gmask, gmask_d, writes=["gmask"])
    fw.dma("pool", sel, sel_d, writes=["sel"])
    fw.dma("sp", X1[:, 0:4, :], x_v[:, 0:4, :], writes=[("X1", t) for t in range(4)])
    fw.op("dve", lambda e: e.memset(ones_f, 1.0), writes=["ones_f"])
    fw.op("dve", lambda e: e.memset(ones_b, 1.0), writes=["ones_b"])

    HR = 8
    hT = reg.get([8, HR * 128], BF16)
    win_tm = reg.get([8, 1792], BF16)
    win_fm = reg.get([8, 640], BF16)
    wout_sb = reg.get([8, 1024], BF16)
    wada_buf = [reg.get([8, 128], BF16) for _ in range(2)]
    xn = reg.get([1, 1024], BF16)
    sqq = reg.get([512], F32)
    qn = reg.get([512], BF16)
    kn = reg.get([128], BF16)
    ssq = reg.get([8], F32)
    rq = reg.get([8], F32)
    ssk = reg.get([2], F32)
    rk = reg.get([2], F32)
    qTA = reg.get([2, 512], BF16)
    qTB = reg.get([2, 512], BF16)
    kT = reg.get([4 * 128], BF16)
    vaug = reg.get([4, 2, 65], BF16)
    vg = reg.get([2, 512], BF16)
    sog = reg.get([2, 512], BF16)
    PT = reg.get([4, 512], BF16)
    den = reg.get([8], F32)
    on = reg.get([512], F32)
    ssa = reg.get([1], F32)
    ra = reg.get([1], F32)
    ya = reg.get([512], BF16)
    yA = reg.get([8, 4, 128], BF16)
    yG = reg.get([4, 128], BF16)
    OFF_LBUF = reg.off
    lbuf = reg.get([2, 512], F32)
    Bc = reg.get([2, 512], F32)
    elast = reg.get([2, 4], F32)
    OFF_QDPAD = reg.off
    qd_pad = reg.get([4, 512], BF16)
    kdT = reg.get([2, 512], BF16)
    kdecT = reg.get([2, 512], BF16)
    kdec = reg.get([4, 256], BF16)
    AT = reg.get([4, 128], BF16)
    S = reg.get([2, 128], F32)
    Sbf = reg.get([2, 128], BF16)
    ssg = reg.get([4], F32)
    rg = reg.get([4], F32)
    t1 = reg.get([512], F32)
    sq_b = sqq.bitcast(BF16)
    esc = t1
    sqB = reg.get([512], BF16)
    yg = reg.get([512], BF16)
    lrT_pad = reg.get([512], BF16)
    wgk2_pad = reg.get([256], BF16)
    print("phase1 region bytes", reg.off, "of", reg.n)

    wada_v = wada_d.rearrange("(k p) n -> p k n", p=128)

    ccol = vecs[:, 73:81]
    fw.op("act", lambda e: e.activation(out=tmp8, in_=ccol, func=AF.Exp, scale=-1.0), reads=["vecs"], writes=["tmp8"])
    fw.op("dve", lambda e: e.tensor_scalar_add(out=tmp8, in0=tmp8, scalar1=1.0), reads=["tmp8"], writes=["tmp8"])
    fw.op("dve", lambda e: e.reciprocal(out=tmp8, in_=tmp8), reads=["tmp8"], writes=["tmp8"])
    fw.op("dve", lambda e: e.tensor_tensor(out=sc_b, in0=tmp8, in1=ccol, op=ALU.mult), reads=["tmp8", "vecs"], writes=["sc_b"])
    MODB = 5
    wctr = [0]

    def mod_chunk(c):
        bi = wctr[0] % 2
        wctr[0] += 1
        buf = wada_buf[bi]
        if 'chunkdma' in DBG_SKIP and c >= 16:
            return
        fw.dma("pool", buf, wada_v[:, :, c * 128:(c + 1) * 128], writes=[("wada", bi)])
        for k in range(8):
            fw.op("pe", lambda e, k=k: e.matmul(out=PS[MODB][:, MODCOL:MODCOL + 1], lhsT=buf[:, k, :], rhs=sc_b[:, k:k + 1], start=(k == 0), stop=(k == 7)),
                  reads=[("wada", bi), "sc_b"], writes=[bk(MODB)], sig=(k == 7), keep_tag=True)
        fw.op("dve", lambda e: e.tensor_tensor(out=mod[:, c:c + 1], in0=PS[MODB][:, MODCOL:MODCOL + 1], in1=vecs[:, c:c + 1], op=ALU.add),
              reads=[bk(MODB), "vecs"], writes=[("mod", c // 8)])

    MK = [("mod", i) for i in range(6)]
    big = [reg.ap[:, boff // 2:(boff + 8192) // 2].rearrange("p (k n) -> p k n", k=8) for boff in (OFF_LBUF, OFF_QDPAD)]
    bigkeys = [[("lbuf", 0), ("lbuf", 1), ("Bc", 0), ("Bc", 1)], ["qd_pad", ("kdT", 0), ("kdT", 1), ("kdecT", 0), ("kdecT", 1)]]
    for cc in range(4):
        fw.dma("pool", big[cc % 2], wada_v[:, :, cc * 512:(cc + 1) * 512], writes=bigkeys[cc % 2])
        if cc == 0:
            for g_ in range(1, 4):
                pass
        for j in range(4):
            c = cc * 4 + j
            for k in range(8):
                fw.op("pe", lambda e, k=k, j=j, cc=cc: e.matmul(out=PS[MODB][:, MODCOL:MODCOL + 1], lhsT=big[cc % 2][:, k, j * 128:(j + 1) * 128], rhs=sc_b[:, k:k + 1], start=(k == 0), stop=(k == 7)),
                      reads=bigkeys[cc % 2] + ["sc_b"], writes=[bk(MODB)], sig=(k == 7), keep_tag=True)
            fw.op("dve", lambda e, c=c: e.tensor_tensor(out=mod[:, c:c + 1], in0=PS[MODB][:, MODCOL:MODCOL + 1], in1=vecs[:, c:c + 1], op=ALU.add),
                  reads=[bk(MODB), "vecs"], writes=[("mod", c // 8)])
    fw.op("dve", lambda e: e.scalar_tensor_tensor(out=gs1, in0=mod[:, 8:16], scalar=1.0, in1=vecs[:, 48:56], op0=ALU.add, op1=ALU.mult),
          reads=[("mod", 1), "vecs"], writes=["gs1"])
    fw.op("dve", lambda e: e.tensor_scalar(out=nbgk, in0=vecs[:, 69:71], scalar1=-1.0, scalar2=None, op0=ALU.mult),
          reads=["vecs"], writes=["nbgk"])
    fw.op("act", lambda e: e.activation(out=es, in_=es, func=AF.Exp), reads=["es"], writes=["es"])
    wintm_v = wintm_d.rearrange("(k p) n -> p k n", p=128)
    fw.dma("pool", win_tm[:, :, 0:768], wintm_v[:, :, 0:768], writes=["win_tmA"])
    fw.dma("pool", win_tm[:, :, 768:1792], wintm_v[:, :, 768:1792], writes=["win_tmB"])
    fw.dma("pool", win_fm, winfm_d.rearrange("(k p) n -> p k n", p=128), writes=["win_fm"])
    for g in range(1, 4):
        fw.dma("sp", X1[:, 4 * g:4 * g + 4, :], x_v[:, 4 * g:4 * g + 4, :], reads=["win_tmA"], writes=[("X1", t) for t in range(4 * g, 4 * g + 4)])
    fw.op("dve", lambda e: e.memset(wgk2_pad, 0.0), writes=["wgk2"])
    fw.dma("pool", wgk2_pad[0:16, :], wgk2_d, reads=[], writes=["wgk2"])

    GB = (6, 7)

    def stream_M1():
        for c in range(16, 24):
            mod_chunk(c)
            yield
        fw.dma("pool", wout_sb, wout_d.rearrange("(k p) n -> p k n", p=128), writes=["wout"])
        yield
        for hh in range(2):
            for kk in range(4):
                k = hh * 4 + kk
                fw.op("dve", lambda e, k=k: e.tensor_scalar(out=yg[:, 0:256].bitcast(F32), in0=ident_f, scalar1=mod[:, 16 + k:17 + k], scalar2=None, op0=ALU.mult),
                      reads=["ident_f", ("mod", 2)], writes=["yg"])
                fw.op("pe", lambda e, k=k, hh=hh, kk=kk: e.matmul(out=PS[GB[hh]][:, kk * 128:(kk + 1) * 128], lhsT=ones_f, rhs=yg[:, 0:256].bitcast(F32), start=True, stop=True),
                      reads=["ones_f", "yg"], writes=[bk(GB[hh])])
            for k in range(8):
                fw.op("dve", lambda e, k=k, hh=hh: e.tensor_tensor(out=wout_sb[:, k, hh * 512:(hh + 1) * 512], in0=wout_sb[:, k, hh * 512:(hh + 1) * 512],
                                                               in1=PS[GB[hh]], op=ALU.mult),
                      reads=["wout", bk(GB[hh])], writes=["wout"])
            yield

    def stream_M2():
        for c in range(24, 48):
            mod_chunk(c)
            yield
        fw.op("dve", lambda e: e.scalar_tensor_tensor(out=gs2, in0=mod[:, 32:40], scalar=1.0, in1=vecs[:, 56:64], op0=ALU.add, op1=ALU.mult),
              reads=[("mod", 4), "vecs"], writes=["gs2"])
        yield

    fw.op("dve", lambda e: e.memset(qTA, 0.0), writes=["qTA0", "qTA1"])
    fw.op("dve", lambda e: e.memset(qTB, 0.0), writes=["qTB0", "qTB1"])
    fw.op("dve", lambda e: e.memset(vaug, 1.0), writes=[("vaug", t) for t in range(4)])
    fw.op("dve", lambda e: e.memset(qd_pad, 0.0), writes=["qd_pad"])
    fw.op("dve", lambda e: e.memset(lrT_pad, 0.0), writes=["lrT"])
    fw.op("dve", lambda e: e.memset(S, 0.0), writes=["S"])
    fw.op("dve", lambda e: e.memset(Sbf, 0.0), writes=["Sbf"])

    alt = [0]

    def evac_affine(out, in_, scale, bias, reads, writes, wtag=None, rtag=None):
        alt[0] ^= 1
        if alt[0]:
            if bias is None:
                fw.op("act", lambda e: e.activation(out=out, in_=in_, func=AF.Copy, scale=scale), reads=reads, writes=writes, wtag=wtag, rtag=rtag)
            else:
                fw.op("act", lambda e: e.activation(out=out, in_=in_, func=AF.Identity, scale=scale, bias=bias), reads=reads, writes=writes, wtag=wtag, rtag=rtag)
        else:
            if bias is None:
                fw.op("dve", lambda e: e.tensor_scalar(out=out, in0=in_, scalar1=scale, scalar2=None, op0=ALU.mult), reads=reads, writes=writes, wtag=wtag, rtag=rtag)
            else:
                fw.op("dve", lambda e: e.tensor_scalar(out=out, in0=in_, scalar1=scale, scalar2=bias, op0=ALU.mult, op1=ALU.add), reads=reads, writes=writes, wtag=wtag, rtag=rtag)

    def rstd_act(out, in_, n, extra_bias, rk_, wk_):
        fw.op("act", lambda e: e.activation(out=out, in_=in_, func=AF.Ln, scale=1.0 / n, bias=EPS), reads=rk_, writes=wk_)
        fw.op("act", lambda e: e.activation(out=out, in_=out, func=AF.Exp, scale=-0.5, bias=extra_bias), reads=wk_, writes=wk_)

    TB = 0

    def stage_T1(t):
        hs = t % HR
        xs = 0
        fw.op("act", lambda e: e.activation(out=sq_b, in_=X1[:, t, :], func=AF.Square, accum_out=ssx[:, t:t + 1]),
              reads=[("X1", t)], writes=["sqq", ("ssx", t)])
        rstd_act(rsx[:, t:t + 1], ssx[:, t:t + 1], D, 0.0, [("ssx", t)], [("rsx", t)])
        fw.op("dve", lambda e: e.tensor_scalar(out=xn[:, xs, :], in0=X1[:, t, :], scalar1=rsx[:, t:t + 1], scalar2=None, op0=ALU.mult),
              reads=[("X1", t), ("rsx", t)], writes=[("xn", xs)])
        yield
        T1B = (TB, 3)
        for k in range(8):
            fw.op("pe", lambda e, k=k: e.transpose(out=PSB[T1B[k // 4]][:, (k % 4) * 128:(k % 4 + 1) * 128], in_=xn[:, xs, k * 128:(k + 1) * 128], identity=ident_b),
                  reads=[("xn", xs), "ident_b"], writes=[bk(T1B[k // 4])], sig=(k % 4 == 3), wtag={bk(T1B[k // 4]): ("T1", t)})
        for k in range(4):
            fw.op("act", lambda e, k=k: e.activation(out=hT[:, k, hs * 128:(hs + 1) * 128], in_=PSB[TB][:, k * 128:(k + 1) * 128], func=AF.Identity,
                                                     scale=gs1[:, k:k + 1], bias=mod[:, k:k + 1]),
                  reads=[bk(TB), "gs1", ("mod", 0)], writes=[("hT", hs)], wtag={("hT", hs): t}, rtag={bk(TB): ("T1", t)})
        hv = hT[:, 4:8, hs * 128:(hs + 1) * 128]
        fw.op("dve", lambda e: e.tensor_tensor(out=hv, in0=PSB[3][:, 0:512].rearrange("p (k n) -> p k n", k=4),
                                               in1=gs1[:, 4:8].unsqueeze(2).to_broadcast([128, 4, 128]), op=ALU.mult),
              reads=[bk(3), "gs1"], writes=[("hT", hs)], wtag={("hT", hs): t}, rtag={bk(3): ("T1", t)})
        fw.op("dve", lambda e: e.tensor_tensor(out=hv, in0=hv, in1=mod[:, 4:8].unsqueeze(2).to_broadcast([128, 4, 128]), op=ALU.add),
              reads=[("hT", hs), ("mod", 0)], writes=[("hT", hs)], wtag={("hT", hs): t})
        yield

    QB, KVB, VGB, OGB = 1, 2, 6, 7

    def stage_T2(t):
        hs = t % HR
        for bank, c0, n in ((QB, 0, 512), (KVB, 512, 256)):
            for k in range(8):
                fw.op("pe", lambda e, bank=bank, c0=c0, n=n, k=k: e.matmul(
                    out=PS[bank][:, 0:n], lhsT=hT[:, k, hs * 128:(hs + 1) * 128], rhs=win_tm[:, k, c0:c0 + n],
                    start=(k == 0), stop=(k == 7)),
                    reads=[("hT", hs), "win_tmA"], writes=[bk(bank)], sig=(k == 7), rtag={("hT", hs): t}, wtag={bk(bank): ("proj", t)})
            yield

    def stage_T3(t):
        qs = t % 2
        vs = t % 4
        fw.op("act", lambda e: e.activation(out=sqq, in_=PS[QB], func=AF.Square), reads=[bk(QB)], writes=["sqq"], rtag={bk(QB): ("proj", t)})
        fw.op("dve", lambda e: e.tensor_reduce(out=ssq, in_=sqq.rearrange("p (h d) -> p h d", h=8), axis=AX.X, op=ALU.add),
              reads=["sqq"], writes=["ssq"])
        rstd_act(rq, ssq, 64, -LN8, ["ssq"], ["rq"])
        fw.op("dve", lambda e: e.tensor_tensor(out=qn.rearrange("p (h d) -> p h d", h=8), in0=PS[QB].rearrange("p (h d) -> p h d", h=8),
                                               in1=rq.unsqueeze(2).to_broadcast([128, 8, 64]), op=ALU.mult),
              reads=[bk(QB), "rq"], writes=["qn"], rtag={bk(QB): ("proj", t)})
        yield
        fw.op("act", lambda e: e.activation(out=sqq[:, 0:128], in_=PS[KVB][:, 0:128], func=AF.Square), reads=[bk(KVB)], writes=["sqq"])
        fw.op("dve", lambda e: e.tensor_reduce(out=ssk, in_=sqq[:, 0:128].rearrange("p (h d) -> p h d", h=2), axis=AX.X, op=ALU.add),
              reads=["sqq"], writes=["ssk"])
        rstd_act(rk, ssk, 64, 0.0, ["ssk"], ["rk"])
        fw.op("dve", lambda e: e.tensor_tensor(out=kn.rearrange("p (h d) -> p h d", h=2), in0=PS[KVB][:, 0:128].rearrange("p (h d) -> p h d", h=2),
                                               in1=rk.unsqueeze(2).to_broadcast([128, 2, 64]), op=ALU.mult),
              reads=[bk(KVB), "rk"], writes=["kn"], rtag={bk(KVB): ("proj", t)})
        yield
        if 'T3v' in DBG_SKIP:
            return
        fw.op("act", lambda e: e.activation(out=vaug[:, t % 4, :, 0:64], in_=PS[KVB][:, 128:256].rearrange("p (h d) -> p h d", h=2), func=AF.Copy),
              reads=[bk(KVB)], writes=[("vaug", t % 4)])
        if 'T3t' in DBG_SKIP:
            return
        for i in range(4):
            fw.op("pe", lambda e, i=i: e.transpose(out=PSB[TB][:, i * 128:(i + 1) * 128], in_=qn[:, i * 128:(i + 1) * 128], identity=ident_b),
                  reads=["qn", "ident_b"], writes=[bk(TB)], sig=False)
        fw.op("pe", lambda e: e.transpose(out=PSB[TB][:, 512:640], in_=kn, identity=ident_b),
              reads=["kn", "ident_b"], writes=[bk(TB)], sig=True, wtag={bk(TB): ("T3", t)})
        if 'T3e' in DBG_SKIP:
            return
        gq = vecs[:, 71:72]
        gk = vecs[:, 72:73]
        fw.op("act", lambda e: e.activation(out=qTA[0:64, qs, :], in_=PSB[TB][0:64, 0:512], func=AF.Copy, scale=gq[0:64, :]),
              reads=[bk(TB), "vecs"], writes=["qTA%d" % qs])
        fw.op("dve", lambda e: e.tensor_scalar(out=qTB[64:128, qs, :], in0=PSB[TB][64:128, 0:512], scalar1=gq[64:128, :], scalar2=None, op0=ALU.mult),
              reads=[bk(TB), "vecs"], writes=["qTB%d" % qs])
        fw.op("dve", lambda e: e.tensor_scalar(out=kT[:, (t % 4) * 128:(t % 4 + 1) * 128], in0=PSB[TB][:, 512:640], scalar1=gk, scalar2=None, op0=ALU.mult),
              reads=[bk(TB), "vecs"], writes=[("kT", t % 4)], rtag={bk(TB): ("T3", t)})
        yield

    SB = DBG_SB
    OB = DBG_OB

    def stage_T4(t):
        qs = t % 2
        ys = t % 8
        blocks = [(t, 0)] + ([(t - 1, 1)] if t > 0 else [])
        si = 0
        allpts = []
        for kv in range(2):
            qT = qTA if kv == 0 else qTB
            qkey = ("qTA%d" if kv == 0 else "qTB%d") % qs
            pts = []
            for (blk, which) in blocks:
                bank = SB[si % 2]
                pslot = si % 4
                si += 1
                fw.op("pe", lambda e, bank=bank, blk=blk, qT=qT: e.matmul(out=PS[bank], lhsT=kT[:, (blk % 4) * 128:(blk % 4 + 1) * 128], rhs=qT[:, qs, :], start=True, stop=False),
                      reads=[("kT", blk % 4), qkey], writes=[bk(bank)], sig=False)
                fw.op("pe", lambda e, bank=bank, which=which, kv=kv: e.matmul(out=PS[bank], lhsT=ident_b, rhs=abias[:, kv * 2 + which, :], start=False, stop=True),
                      reads=["ident_b", "abias"], writes=[bk(bank)], sig=True)
                fw.op("act", lambda e, bank=bank, pslot=pslot: e.activation(out=PT[:, pslot, :], in_=PS[bank], func=AF.Exp),
                      reads=[bk(bank)], writes=[("PT", pslot)])
                pts.append((pslot, blk))
                yield
            allpts.append(pts)
        for kv in range(2):
            pts = allpts[kv]
            for g in range(4):
                h = kv * 4 + g
                ob = OB[kv]
                for pi, (pslot, blk) in enumerate(pts):
                    fw.op("pe", lambda e, ob=ob, g=g, pslot=pslot, blk=blk, pi=pi, kv=kv, pts=pts: e.matmul(
                        out=PS[ob][:, g * 65:(g + 1) * 65], lhsT=PT[:, pslot, g * 128:(g + 1) * 128], rhs=vaug[:, blk % 4, kv, :],
                        start=(pi == 0), stop=(pi == len(pts) - 1)),
                        reads=[("PT", pslot), ("vaug", blk % 4)], writes=[bk(ob)], sig=(g == 3 and pi == len(pts) - 1))
            yield
        for kv in range(2):
            ov = PS[OB[kv]][:, 0:260].rearrange("p (h d) -> p h d", h=4)
            fw.op("dve", lambda e, ov=ov, kv=kv: e.tensor_tensor(out=den[:, kv * 4:(kv + 1) * 4], in0=ov[:, :, 64], in1=es[:, kv * 4:(kv + 1) * 4], op=ALU.add),
                  reads=[bk(OB[kv]), "es"], writes=["den"])
        fw.op("dve", lambda e: e.reciprocal(out=den, in_=den), reads=["den"], writes=["den"])
        yield
        for kv in range(2):
            ov = PS[OB[kv]][:, 0:260].rearrange("p (h d) -> p h d", h=4)
            fw.op("dve", lambda e, ov=ov, kv=kv: e.tensor_tensor(
                out=on[:, kv * 256:(kv + 1) * 256].rearrange("p (h d) -> p h d", h=4), in0=ov[:, :, 0:64],
                in1=den[:, kv * 4:(kv + 1) * 4].unsqueeze(2).to_broadcast([128, 4, 64]), op=ALU.mult),
                reads=[bk(OB[kv]), "den"], writes=["on"])
        yield
        fw.op("act", lambda e: e.activation(out=sqq, in_=on, func=AF.Square, accum_out=ssa), reads=["on"], writes=["sqq", "ssa"])
        rstd_act(ra, ssa, 512, 0.0, ["ssa"], ["ra"])
        fw.op("dve", lambda e: e.tensor_scalar(out=ya, in0=on, scalar1=ra, scalar2=None, op0=ALU.mult), reads=["on", "ra"], writes=["ya"])
        yield
        for i in range(4):
            fw.op("pe", lambda e, i=i: e.transpose(out=PSB[TB][:, i * 128:(i + 1) * 128], in_=ya[:, i * 128:(i + 1) * 128], identity=ident_b),
                  reads=["ya", "ident_b"], writes=[bk(TB)], sig=(i == 3), wtag={bk(TB): ("T4", t)})
        for i in range(4):
            evac_affine(yA[:, ys, i, :], PSB[TB][:, i * 128:(i + 1) * 128], vecs[:, 64 + i:65 + i], None, [bk(TB), "vecs"], [("yA", ys)], wtag={("yA", ys): t}, rtag={bk(TB): ("T4", t)})
        yield

    FB = (6, 7)

    def stage_G1(T0, GT):
        hcol = (T0 % HR) * 128
        NG_ = GT * 128
        hkeys = [("hT", (T0 + i) % HR) for i in range(GT)]

        def fm_proj(m, bank):
            for k in range(8):
                fw.op("pe", lambda e, k=k: e.matmul(out=PS[bank][:, 0:NG_], lhsT=win_fm[:, k, m * 128:(m + 1) * 128], rhs=hT[:, k, hcol:hcol + NG_],
                                                    start=(k == 0), stop=(k == 7)),
                      reads=hkeys + ["win_fm"], writes=[bk(bank)], sig=(k == 7), rtag={("hT", (T0 + i) % HR): T0 + i for i in range(GT)})
        fm_proj(4, FB[0])
        fw.op("act", lambda e: e.activation(out=lrT_pad[0:16, 0:NG_], in_=PS[FB[0]][0:16, 0:NG_], func=AF.Copy), reads=[bk(FB[0])], writes=["lrT"])
        yield
        for c in range(2):
            bank = FB[(c + 1) % 2]
            fw.op("pe", lambda e, c=c, bank=bank: e.matmul(out=PS[bank][:, 0:NG_], lhsT=wgk2_pad[:, c * 128:(c + 1) * 128], rhs=lrT_pad[:, 0:NG_], start=True, stop=True),
                  reads=["wgk2", "lrT"], writes=[bk(bank)])
            fw.op("act", lambda e, c=c, bank=bank: e.activation(out=lbuf[:, c, 0:NG_], in_=PS[bank][:, 0:NG_], func=AF.Exp, scale=-1.0, bias=nbgk[:, c:c + 1]),
                  reads=[bk(bank), "nbgk"], writes=[("lbuf", c)])
            fw.op("act", lambda e, c=c: e.activation(out=lbuf[:, c, 0:NG_], in_=lbuf[:, c, 0:NG_], func=AF.Ln, bias=1.0), reads=[("lbuf", c)], writes=[("lbuf", c)])
            yield
            for j in range(GT):
                fw.op("dve", lambda e, c=c, j=j: e.tensor_tensor_scan(out=Bc[:, c, j * 128:(j + 1) * 128], data0=ones_f, data1=lbuf[:, c, j * 128:(j + 1) * 128],
                                                                      initial=0.0, op0=ALU.mult, op1=ALU.add),
                      reads=[("lbuf", c), "ones_f"], writes=[("Bc", c)])
            yield
            fw.op("act", lambda e, c=c: e.activation(out=lbuf[:, c, 0:NG_], in_=Bc[:, c, 0:NG_], func=AF.Exp, scale=-1.0 / 16, bias=-LN8), reads=[("Bc", c)], writes=[("lbuf", c)])
            fw.op("act", lambda e, c=c: e.activation(out=elast[:, c, 0:GT], in_=Bc[:, c, 0:NG_].rearrange("p (j i) -> p j i", j=GT)[:, :, 127], func=AF.Exp, scale=-1.0 / 16),
                  reads=[("Bc", c)], writes=[("elast", c)])
            fw.op("act", lambda e, c=c: e.activation(out=Bc[:, c, 0:NG_], in_=Bc[:, c, 0:NG_], func=AF.Exp, scale=1.0 / 16), reads=[("Bc", c)], writes=[("Bc", c)])
            yield
        for c in range(2):
            bq = FB[0]
            fm_proj(c, bq)
            for hh in range(2):
                fw.op("dve", lambda e, c=c, hh=hh: e.tensor_tensor(out=qd_pad[hh * 64:(hh + 1) * 64, 2 * c + hh, 0:NG_], in0=PS[bq][hh * 64:(hh + 1) * 64, 0:NG_],
                                                                   in1=lbuf[hh * 64:(hh + 1) * 64, c, 0:NG_], op=ALU.mult),
                      reads=[bk(bq), ("lbuf", c)], writes=["qd_pad"])
            yield
            bkk = FB[1]
            fm_proj(2 + c, bkk)
            fw.op("dve", lambda e, c=c: e.tensor_tensor(out=kdT[:, c, 0:NG_], in0=PS[bkk][:, 0:NG_], in1=Bc[:, c, 0:NG_], op=ALU.mult),
                  reads=[bk(bkk), ("Bc", c)], writes=[("kdT", c)])
            yield
            for j in range(GT):
                fw.op("dve", lambda e, c=c, j=j: e.tensor_scalar(out=kdecT[:, c, j * 128:(j + 1) * 128], in0=kdT[:, c, j * 128:(j + 1) * 128],
                                                                 scalar1=elast[:, c, j:j + 1], scalar2=None, op0=ALU.mult),
                      reads=[("kdT", c), ("elast", c)], writes=[("kdecT", c)])
            yield
        for j in range(GT):
            for c in range(2):
                fw.op("pe", lambda e, c=c, j=j: e.transpose(out=PSB[6][:, (j * 2 + c) * 128:(j * 2 + c + 1) * 128], in_=kdecT[:, c, j * 128:(j + 1) * 128], identity=ident_b),
                      reads=[("kdecT", c), "ident_b"], writes=[bk(6)], sig=(j == GT - 1 and c == 1))
        fw.op("act", lambda e: e.activation(out=kdec[:, 0:GT, :].rearrange("p j c -> p (j c)"), in_=PSB[6][:, 0:GT * 256], func=AF.Copy), reads=[bk(6)], writes=["kdec"])
        yield

    def stage_B0(t):
        hs = t % HR
        vs = t % 2
        for bank, c0, n in ((VGB, 768, 512), (OGB, 1280, 512)):
            for k in range(8):
                fw.op("pe", lambda e, bank=bank, c0=c0, n=n, k=k: e.matmul(
                    out=PS[bank][:, 0:n], lhsT=hT[:, k, hs * 128:(hs + 1) * 128], rhs=win_tm[:, k, c0:c0 + n],
                    start=(k == 0), stop=(k == 7)),
                    reads=[("hT", hs), "win_tmB"], writes=[bk(bank)], sig=(k == 7), rtag={("hT", hs): t})
            yield
        fw.op("act", lambda e: e.activation(out=vg[:, vs, :], in_=PS[VGB], func=AF.Copy), reads=[bk(VGB)], writes=[("vg", vs)])
        yield
        fw.op("act", lambda e: e.activation(out=esc, in_=PS[OGB], func=AF.Exp, scale=-1.0), reads=[bk(OGB)], writes=["t1"])
        fw.op("act", lambda e: e.activation(out=esc, in_=esc, func=AF.Ln, bias=1.0), reads=["t1"], writes=["t1"])
        fw.op("act", lambda e: e.activation(out=esc, in_=esc, func=AF.Exp, scale=-1.0), reads=["t1"], writes=["t1"])
        fw.op("dve", lambda e: e.tensor_tensor(out=sog[:, vs, :], in0=PS[OGB], in1=esc, op=ALU.mult), reads=[bk(OGB), "t1"], writes=[("sog", vs)])
        yield

    AB, OGB2, UB = 6, 7, 6

    def stage_G2(t, j):
        vs = t % 2
        ys = t % 8
        for h in range(4):
            c = h // 2
            fw.op("pe", lambda e, h=h, c=c: e.matmul(out=PS[AB][:, h * 128:(h + 1) * 128], lhsT=kdT[:, c, j * 128:(j + 1) * 128],
                                                     rhs=qd_pad[:, h, j * 128:(j + 1) * 128], start=True, stop=True),
                  reads=[("kdT", c), "qd_pad"], writes=[bk(AB)], sig=(h == 3))
        yield
        fw.op("dve", lambda e: e.tensor_tensor(out=AT, in0=PS[AB].rearrange("p (h i) -> p h i", h=4), in1=gmask.unsqueeze(1).to_broadcast([128, 4, 128]), op=ALU.mult),
              reads=[bk(AB), "gmask"], writes=["AT"])
        yield
        for h in range(4):
            c = h // 2
            fw.op("pe", lambda e, h=h: e.matmul(out=PS[OGB2][:, h * 128:(h + 1) * 128], lhsT=AT[:, h, :], rhs=vg[:, vs, h * 128:(h + 1) * 128], start=True, stop=False),
                  reads=["AT", ("vg", vs)], writes=[bk(OGB2)], sig=False)
            fw.op("pe", lambda e, h=h, c=c: e.matmul(out=PS[OGB2][:, h * 128:(h + 1) * 128], lhsT=qd_pad[:, h, j * 128:(j + 1) * 128], rhs=Sbf[:, c, :], start=False, stop=True),
                  reads=["qd_pad", "Sbf"], writes=[bk(OGB2)], sig=(h == 3))
        yield
        for c in range(2):
            fw.op("pe", lambda e, c=c: e.matmul(out=PS[UB][:, c * 256:(c + 1) * 256], lhsT=kdec[:, j, c * 128:(c + 1) * 128], rhs=vg[:, vs, c * 256:(c + 1) * 256], start=True, stop=True),
                  reads=["kdec", ("vg", vs)], writes=[bk(UB)], sig=(c == 1))
        yield
        for c in range(2):
            for hh in range(2):
                fw.op("dve", lambda e, c=c, hh=hh: e.scalar_tensor_tensor(
                    out=S[hh * 64:(hh + 1) * 64, c, :], in0=S[hh * 64:(hh + 1) * 64, c, :], scalar=elast[hh * 64:(hh + 1) * 64, c, j:j + 1],
                    in1=PS[UB][hh * 64:(hh + 1) * 64, c * 256 + hh * 128:c * 256 + (hh + 1) * 128], op0=ALU.mult, op1=ALU.add),
                    reads=["S", ("elast", c), bk(UB)], writes=["S"])
        fw.op("act", lambda e: e.activation(out=Sbf, in_=S, func=AF.Copy), reads=["S"], writes=["Sbf"])
        yield
        fw.op("act", lambda e: e.activation(out=sqB, in_=PS[OGB2], func=AF.Square), reads=[bk(OGB2)], writes=["sqB"])
        fw.op("dve", lambda e: e.tensor_reduce(out=ssg, in_=sqB.rearrange("p (h d) -> p h d", h=4), axis=AX.X, op=ALU.add), reads=["sqB"], writes=["ssg"])
        rstd_act(rg, ssg, 128, 0.0, ["ssg"], ["rg"])
        fw.op("dve", lambda e: e.tensor_tensor(out=t1.rearrange("p (h d) -> p h d", h=4), in0=PS[OGB2].rearrange("p (h d) -> p h d", h=4),
                                               in1=rg.unsqueeze(2).to_broadcast([128, 4, 128]), op=ALU.mult),
              reads=[bk(OGB2), "rg"], writes=["t1"])
        yield
        fw.op("dve", lambda e: e.tensor_tensor(out=yg, in0=t1, in1=sog[:, vs, :], op=ALU.mult), reads=["t1", ("sog", vs)], writes=["yg"])
        yield
        for i in range(4):
            fw.op("pe", lambda e, i=i: e.transpose(out=PSB[6][:, i * 128:(i + 1) * 128], in_=yg[:, i * 128:(i + 1) * 128], identity=ident_b),
                  reads=["yg", "ident_b"], writes=[bk(6)], sig=(i == 3))
        yield
        fw.op("act", lambda e: e.activation(out=yG.rearrange("p a b -> p (a b)"), in_=PSB[6][:, 0:512], func=AF.Copy, scale=vecs[:, 68:69]),
              reads=[bk(6), "vecs"], writes=["yG"], wtag={"yG": t})
        yield

    WB = (6, 7)

    def stage_W(t):
        ys = t % 8
        for dh in range(2):
            for k in range(8):
                fw.op("pe", lambda e, dh=dh, k=k: e.matmul(out=PS[WB[dh]], lhsT=(yA[:, ys, k, :] if k < 4 else yG[:, k - 4, :]), rhs=wout_sb[:, k, dh * 512:(dh + 1) * 512], start=(k == 0), stop=(k == 7)),
                      reads=[("yA", ys), "yG", "wout"], writes=[bk(WB[dh])], sig=(k == 7), rtag={("yA", ys): t, "yG": t})
            fw.op("dve", lambda e, dh=dh: e.tensor_tensor(out=X1[:, t, dh * 512:(dh + 1) * 512], in0=PS[WB[dh]], in1=X1[:, t, dh * 512:(dh + 1) * 512], op=ALU.add),
                  reads=[bk(WB[dh]), ("X1", t)], writes=[("X1", t)])
            yield

    def chain(*gens):
        for g_ in gens:
            yield from g_

    def run_streams(streams):
        streams = [s for s in streams if s is not None]
        if SEQ_STREAMS:
            for s in streams:
                for _ in s:
                    pass
            return
        if not GREEDY:
            while streams:
                nxt = []
                for s in streams:
                    try:
                        next(s)
                        nxt.append(s)
                    except StopIteration:
                        pass
                streams = nxt
            return
        ready = [0.0] * len(streams)
        alive = list(range(len(streams)))
        while alive:
            i = min(alive, key=lambda j: ready[j])
            fw.cur_stream_end = 0.0
            try:
                next(streams[i])
                ready[i] = max(ready[i], fw.cur_stream_end)
            except StopIteration:
                alive.remove(i)

    def interleave(a, b):
        gens = [a, b]
        while gens:
            nxt = []
            for g_ in gens:
                try:
                    next(g_)
                    nxt.append(g_)
                    yield
                except StopIteration:
                    pass
            gens = nxt

    def stream_A(g):
        ts = list(range(GROUPS[g][0], GROUPS[g][0] + GROUPS[g][1]))
        parts = [stage_T1(ts[0]), stage_T2(ts[0])]
        for i, t in enumerate(ts):
            parts.append(stage_T3(t))
            if i + 1 < len(ts) and PIPE_A:
                parts.append(interleave(stage_T4(t), chain(stage_T1(ts[i + 1]), stage_T2(ts[i + 1]))))
            else:
                parts.append(stage_T4(t))
                if i + 1 < len(ts):
                    parts.append(stage_T1(ts[i + 1]))
                    parts.append(stage_T2(ts[i + 1]))
        return chain(*parts)

    def stream_B(g):
        T0, n_ = GROUPS[g]
        return chain(stage_G1(T0, n_), *[chain(stage_B0(t), stage_G2(t, t - T0), stage_W(t)) for t in range(T0, T0 + n_)])

    def early_exit():
        fw.barrier()
        for g_ in range(4):
            fw.dma("sp", out_v[:, 4 * g_:4 * g_ + 4, :], X1[:, 4 * g_:4 * g_ + 4, :], reads=[("X1", t) for t in range(4 * g_, 4 * g_ + 4)], writes=[("out", g_)])
        fw.barrier()
        return nc, fw, None
    if DBG_STAGE == 10:
        return early_exit()
    run_streams([stream_A(0) if 'A0' not in DBG_SKIP else None, stream_M1() if 'M1' not in DBG_SKIP else None])
    if DBG_STAGE == 11:
        return early_exit()
    m2 = stream_M2()
    NGRP = len(GROUPS)

    class GStream:
        def __init__(self, make, first, n):
            self.make = make
            self.cur = first
            self.n = n
            self.gen = None
            self.ready = 0.0

        def finished(self):
            return self.cur >= self.n and self.gen is None

        def at_boundary(self):
            return self.gen is None

        def step(self):
            if self.gen is None:
                self.gen = self.make(self.cur)
            fw.cur_stream_end = 0.0
            try:
                next(self.gen)
                self.ready = max(self.ready, fw.cur_stream_end)
            except StopIteration:
                self.gen = None
                self.cur += 1

    class PlainStream(GStream):
        def __init__(self, gen):
            self.gen = gen
            self.ready = 0.0
            self.done = False

        def finished(self):
            return self.done

        def at_boundary(self):
            return False

        def step(self):
            fw.cur_stream_end = 0.0
            try:
                next(self.gen)
                self.ready = max(self.ready, fw.cur_stream_end)
            except StopIteration:
                self.done = True

    STA = GStream(stream_A, 1, NGRP)
    STB = GStream(stream_B, 0, NGRP)
    STM = PlainStream(m2) if 'M2' not in DBG_SKIP else None
    while True:
        cands = []
        if not STA.finished() and not (STA.at_boundary() and (STA.cur >= NGRP or STA.cur - STB.cur > LEAD)):
            cands.append(STA)
        if not STB.finished() and not (STB.at_boundary() and not (STB.cur < STA.cur)):
            cands.append(STB)
        if STM is not None and not STM.finished():
            cands.append(STM)
        if not cands:
            assert STA.finished() and STB.finished(), (STA.cur, STB.cur)
            break
        s_ = min(cands, key=lambda s: s.ready - (PRIO_B if s is STB else 0.0)) if GREEDY else cands[0]
        s_.step()

    if stage == 1:
        fw.barrier()
        for g in range(4):
            fw.dma("sp", out_v[:, 4 * g:4 * g + 4, :], X1[:, 4 * g:4 * g + 4, :], reads=[("X1", t) for t in range(4 * g, 4 * g + 4)], writes=[("out", g)])
        fw.barrier()
        return nc, fw, None

    fw.barrier()
    reg.reset()
    h2T = reg.get([8, T], BF16)
    wb = [dict(w1=reg.get([EB, 8, 256], BF16), w3=reg.get([EB, 8, 256], BF16), w2=reg.get([EB, 2, 1024], BF16)) for _ in range(2)]
    xn2 = reg.get([2, 1024], F32)
    xT32 = reg.get([2, 1024], F32)
    wr_sb = reg.get([8, 20], F32)
    shrep = reg.get([128], F32)
    cb_sb = reg.get([20], F32)
    L = reg.get([NT, 20], F32)
    g2b = reg.get([1024], F32)
    comb_pad = reg.get([NT, 128], BF16)
    cT_sb = reg.get([T], BF16)
    Cb = reg.get([2, 512], BF16)
    s_sb = reg.get([2, 512], BF16)
    gc_sb = reg.get([2, 512], BF16)
    hid = reg.get([2, EB * 2, 512], BF16)
    sq2 = reg.get([1024], BF16)
    gmax = reg.get([NT], F32)
    goh = reg.get([NT, 4], F32)
    gex = reg.get([NT, 4], F32)
    pg = reg.get([NT], F32)
    tmp16 = reg.get([NT, 16], F32)
    esel = reg.get([NT, 4], F32)
    esel2 = reg.get([NT, 4], F32)
    m1 = reg.get([NT], F32)
    m2 = reg.get([NT], F32)
    oh1 = reg.get([NT, 4], F32)
    oh2 = reg.get([NT, 4], F32)
    rr = reg.get([NT], F32)
    wa = reg.get([NT], F32)
    wb2 = reg.get([NT], F32)
    wsel = reg.get([NT, 4], F32)
    print("phase2 region bytes", reg.off, "of", reg.n)

    def load_expert_batch(bi):
        buf = wb[bi % 2]
        for j in range(EB):
            e_ = bi * EB + j
            fw.dma("pool", buf["w1"][:, j, :, :], w1_d[e_].rearrange("(k p) f -> p k f", p=128), writes=[("w1", bi % 2, j)])
            fw.dma("pool", buf["w3"][:, j, :, :], w3_d[e_].rearrange("(k p) f -> p k f", p=128), writes=[("w3", bi % 2, j)])
            fw.dma("pool", buf["w2"][:, j, :, :], w2_d[e_].rearrange("(k p) d -> p k d", p=128), writes=[("w2", bi % 2, j)])

    fw.dma("sp", wr_sb, wr_d.rearrange("(k p) n -> p k n", p=128), writes=["wr"])
    load_expert_batch(0)
    load_expert_batch(1)

    for k in range(8):
        fw.op("dve", lambda e, k=k: e.tensor_scalar(out=shrep, in0=ident_f, scalar1=mod[:, 40 + k:41 + k], scalar2=None, op0=ALU.mult),
              reads=["ident_f", ("mod", 5)], writes=["shrep"])
        fw.op("pe", lambda e, k=k: e.matmul(out=PS[k // 4][:, (k % 4) * 128:(k % 4 + 1) * 128], lhsT=ones_f, rhs=shrep, start=True, stop=True),
              reads=["ones_f", "shrep"], writes=[bk(k // 4)])
    for hh in range(2):
        fw.op("act", lambda e, hh=hh: e.activation(out=g2b[:, hh * 512:(hh + 1) * 512], in_=PS[hh], func=AF.Copy), reads=[bk(hh)], writes=["g2b"])
    for k in range(8):
        fw.op("dve", lambda e, k=k: e.tensor_copy(out=shrep, in_=mod[:, 24 + k:25 + k].to_broadcast([128, 128])),
              reads=[("mod", 3)], writes=["shrep"])
        fw.op("pe", lambda e, k=k: e.matmul(out=PS[2][:, 0:20], lhsT=shrep, rhs=wr_sb[:, k, :], start=(k == 0), stop=(k == 7)),
              reads=["shrep", "wr"], writes=[bk(2)])
    fw.op("dve", lambda e: e.tensor_tensor(out=cb_sb, in0=PS[2][:, 0:20], in1=brg, op=ALU.add), reads=[bk(2), "brg"], writes=["cb"])
    for k in range(8):
        fw.op("dve", lambda e, k=k: e.tensor_scalar(out=wr_sb[:, k, :], in0=wr_sb[:, k, :], scalar1=gs2[:, k:k + 1], scalar2=None, op0=ALU.mult),
              reads=["wr", "gs2", bk(2)], writes=["wr"])
    fw.op("dve", lambda e: e.memset(comb_pad, 0.0), writes=["comb_pad"])
    fw.op("dve", lambda e: e.memset(cT_sb, 0.0), writes=["cT"])

    def stage_N(t):
        p_ = t % 2
        xs = p_
        b0_, b1_, rb = 2 * p_, 2 * p_ + 1, 4 + p_
        tb = (b0_, b1_)
        fw.op("act", lambda e: e.activation(out=sq2, in_=X1[:, t, :], func=AF.Square, accum_out=ssx[:, t:t + 1]),
              reads=[("X1", t)], writes=["sq2", ("ssx", t)])
        rstd_act(rsx[:, t:t + 1], ssx[:, t:t + 1], D, 0.0, [("ssx", t)], [("rsx", t)])
        fw.op("dve", lambda e: e.tensor_scalar(out=xn2[:, xs, :], in0=X1[:, t, :], scalar1=rsx[:, t:t + 1], scalar2=None, op0=ALU.mult),
              reads=[("X1", t), ("rsx", t)], writes=[("xn2", xs)])
        yield
        for k in range(8):
            fw.op("pe", lambda e, k=k: e.transpose(out=PS[tb[k // 4]][:, (k % 4) * 128:(k % 4 + 1) * 128], in_=xn2[:, xs, k * 128:(k + 1) * 128], identity=ident_f),
                  reads=[("xn2", xs), "ident_f"], writes=[bk(tb[k // 4])], sig=(k % 4 == 3))
        yield
        for hh in range(2):
            fw.op("dve", lambda e, hh=hh: e.tensor_copy(out=xT32[:, xs, hh * 512:(hh + 1) * 512], in_=PS[tb[hh]]), reads=[bk(tb[hh])], writes=[("xT32", xs)])
        yield
        for k in range(8):
            evac_affine(h2T[:, k, t * 128:(t + 1) * 128], PS[tb[k // 4]][:, (k % 4) * 128:(k % 4 + 1) * 128], gs2[:, k:k + 1], mod[:, 24 + k:25 + k],
                        [bk(tb[k // 4]), "gs2", ("mod", 3)], [("h2T", t // 4)])
        yield
        for k in range(8):
            fw.op("pe", lambda e, k=k: e.matmul(out=PS[rb][:, 0:20], lhsT=xT32[:, xs, k * 128:(k + 1) * 128], rhs=wr_sb[:, k, :], start=(k == 0), stop=(k == 7)),
                  reads=[("xT32", xs), "wr"], writes=[bk(rb)], sig=(k == 7))
        yield
        fw.op("dve", lambda e: e.tensor_tensor(out=L[:, t, :], in0=PS[rb][:, 0:20], in1=cb_sb, op=ALU.add), reads=[bk(rb), "cb"], writes=["L"])
        yield

    run_streams([chain(*[stage_N(t) for t in range(0, NT, 2)]), chain(*[stage_N(t) for t in range(1, NT, 2)])])

    def dv(fn, reads, writes):
        fw.op("dve", fn, reads=reads, writes=writes)
    gl = L[:, :, 0:4]
    el = L[:, :, 4:20].rearrange("p t (g i) -> p t g i", g=4)
    dv(lambda e: e.tensor_reduce(out=gmax, in_=gl, axis=AX.X, op=ALU.max), ["L"], ["gmax"])
    dv(lambda e: e.tensor_tensor(out=goh, in0=gl, in1=gmax.unsqueeze(2).to_broadcast([128, NT, 4]), op=ALU.is_equal), ["L", "gmax"], ["goh"])
    dv(lambda e: e.tensor_tensor(out=gex, in0=gl, in1=gmax.unsqueeze(2).to_broadcast([128, NT, 4]), op=ALU.subtract), ["L", "gmax"], ["gex"])
    fw.op("act", lambda e: e.activation(out=gex, in_=gex, func=AF.Exp), reads=["gex"], writes=["gex"])
    dv(lambda e: e.tensor_reduce(out=pg, in_=gex, axis=AX.X, op=ALU.add), ["gex"], ["pg"])
    dv(lambda e: e.reciprocal(out=pg, in_=pg), ["pg"], ["pg"])
    dv(lambda e: e.tensor_tensor(out=tmp16.rearrange("p t (g i) -> p t g i", g=4), in0=el, in1=goh.unsqueeze(3).to_broadcast([128, NT, 4, 4]), op=ALU.mult),
       ["L", "goh"], ["tmp16"])
    dv(lambda e: e.tensor_reduce(out=esel, in_=tmp16.rearrange("p t (g i) -> p t i g", g=4), axis=AX.X, op=ALU.add), ["tmp16"], ["esel"])
    dv(lambda e: e.tensor_reduce(out=m1, in_=esel, axis=AX.X, op=ALU.max), ["esel"], ["m1"])
    dv(lambda e: e.tensor_tensor(out=oh1, in0=esel, in1=m1.unsqueeze(2).to_broadcast([128, NT, 4]), op=ALU.is_equal), ["esel", "m1"], ["oh1"])
    dv(lambda e: e.scalar_tensor_tensor(out=esel2, in0=oh1, scalar=-1e30, in1=esel, op0=ALU.mult, op1=ALU.add), ["oh1", "esel"], ["esel2"])
    dv(lambda e: e.tensor_reduce(out=m2, in_=esel2, axis=AX.X, op=ALU.max), ["esel2"], ["m2"])
    dv(lambda e: e.tensor_tensor(out=oh2, in0=esel2, in1=m2.unsqueeze(2).to_broadcast([128, NT, 4]), op=ALU.is_equal), ["esel2", "m2"], ["oh2"])
    dv(lambda e: e.tensor_tensor(out=rr, in0=m2, in1=m1, op=ALU.subtract), ["m1", "m2"], ["rr"])
    fw.op("act", lambda e: e.activation(out=rr, in_=rr, func=AF.Exp), reads=["rr"], writes=["rr"])
    dv(lambda e: e.tensor_scalar_add(out=wa, in0=rr, scalar1=1.0), ["rr"], ["wa"])
    dv(lambda e: e.reciprocal(out=wa, in_=wa), ["wa"], ["wa"])
    dv(lambda e: e.tensor_tensor(out=wa, in0=wa, in1=pg, op=ALU.mult), ["wa", "pg"], ["wa"])
    dv(lambda e: e.tensor_tensor(out=wb2, in0=wa, in1=rr, op=ALU.mult), ["wa", "rr"], ["wb2"])
    dv(lambda e: e.tensor_tensor(out=wsel, in0=oh1, in1=wa.unsqueeze(2).to_broadcast([128, NT, 4]), op=ALU.mult), ["oh1", "wa"], ["wsel"])
    dv(lambda e: e.tensor_tensor(out=oh2, in0=oh2, in1=wb2.unsqueeze(2).to_broadcast([128, NT, 4]), op=ALU.mult), ["oh2", "wb2"], ["oh2"])
    dv(lambda e: e.tensor_tensor(out=wsel, in0=wsel, in1=oh2, op=ALU.add), ["wsel", "oh2"], ["wsel"])
    dv(lambda e: e.tensor_tensor(out=comb_pad[:, :, 0:16].rearrange("p t (g i) -> p t g i", g=4), in0=goh.unsqueeze(3).to_broadcast([128, NT, 4, 4]),
                                 in1=wsel.unsqueeze(2).to_broadcast([128, NT, 4, 4]), op=ALU.mult), ["goh", "wsel"], ["comb_pad"])
    for half in range(2):
        for i in range(8):
            t = half * 8 + i
            fw.op("pe", lambda e, t=t, i=i: e.transpose(out=PSB[3][:, i * 128:(i + 1) * 128], in_=comb_pad[:, t, :], identity=ident_b),
                  reads=["comb_pad", "ident_b"], writes=[bk(3)], sig=(i == 7))
        fw.op("act", lambda e, half=half: e.activation(out=cT_sb[0:16, half * 1024:(half + 1) * 1024], in_=PSB[3][0:16, :], func=AF.Copy), reads=[bk(3)], writes=["cT"])

    NB = NEXP // EB
    AG = (0, 1, 2, 3)
    CBK = 4
    YB = (5, 6, 7)
    yi = [0]
    ui = [0]
    def moe_fold(bi):
        buf = wb[bi % 2]
        for j in range(EB):
            for fc in range(2):
                fw.op("dve", lambda e, j=j, fc=fc: e.tensor_tensor(out=buf["w2"][:, j, fc, :], in0=buf["w2"][:, j, fc, :], in1=g2b, op=ALU.mult),
                      reads=[("w2", bi % 2, j), "g2b"], writes=[("w2", bi % 2, j)])

    def moe_p1(bi, tg):
        buf = wb[bi % 2]
        hs = tg % 2
        for j in range(EB):
            e_ = bi * EB + j
            fw.op("pe", lambda e, e_=e_: e.matmul(out=PS[CBK], lhsT=sel[:, e_, :], rhs=cT_sb[:, tg * 512:(tg + 1) * 512], start=True, stop=True),
                  reads=["sel", "cT"], writes=[bk(CBK)])
            cs = ui[0] % 2
            fw.op("act", lambda e, cs=cs: e.activation(out=Cb[:, cs, :], in_=PS[CBK], func=AF.Copy), reads=[bk(CBK)], writes=[("Cb", cs)])
            for fc in range(2):
                u = ui[0] % 2
                ab, gb = AG[2 * u], AG[2 * u + 1]
                ui[0] += 1
                for (bank, wname) in ((ab, "w1"), (gb, "w3")):
                    for k in range(8):
                        fw.op("pe", lambda e, bank=bank, wname=wname, j=j, fc=fc, k=k: e.matmul(
                            out=PS[bank], lhsT=buf[wname][:, j, k, fc * 128:(fc + 1) * 128], rhs=h2T[:, k, tg * 512:(tg + 1) * 512],
                            start=(k == 0), stop=(k == 7)),
                            reads=[(wname, bi % 2, j), ("h2T", tg)], writes=[bk(bank)], sig=(k == 7))
                fw.op("act", lambda e, ab=ab, u=u: e.activation(out=s_sb[:, u, :], in_=PS[ab], func=AF.Silu), reads=[bk(ab)], writes=[("s", u)])
                fw.op("dve", lambda e, gb=gb, u=u, cs=cs: e.tensor_tensor(out=gc_sb[:, u, :], in0=PS[gb], in1=Cb[:, cs, :], op=ALU.mult),
                      reads=[bk(gb), ("Cb", cs)], writes=[("gc", u)])
                fw.op("dve", lambda e, u=u, j=j, fc=fc: e.tensor_tensor(out=hid[:, hs, j * 2 + fc, :], in0=s_sb[:, u, :], in1=gc_sb[:, u, :], op=ALU.mult),
                      reads=[("s", u), ("gc", u)], writes=[("hid", hs)], wtag={("hid", hs): (bi, tg)})

    def moe_p2(bi, tg):
        buf = wb[bi % 2]
        hs = tg % 2
        for tt in range(4):
            t = tg * 4 + tt
            for dh in range(2):
                yb = YB[yi[0] % 3]
                yi[0] += 1
                n = EB * 2
                for q in range(n):
                    j, fc = q // 2, q % 2
                    fw.op("pe", lambda e, yb=yb, q=q, j=j, fc=fc, tt=tt, dh=dh: e.matmul(
                        out=PS[yb], lhsT=hid[:, hs, q, tt * 128:(tt + 1) * 128], rhs=buf["w2"][:, j, fc, dh * 512:(dh + 1) * 512],
                        start=(q == 0), stop=(q == n - 1)),
                        reads=[("hid", hs), ("w2", bi % 2, j)], writes=[bk(yb)], sig=(q == n - 1), rtag={("hid", hs): (bi, tg)})
                fw.op("dve", lambda e, yb=yb, t=t, dh=dh: e.tensor_tensor(out=X1[:, t, dh * 512:(dh + 1) * 512], in0=PS[yb], in1=X1[:, t, dh * 512:(dh + 1) * 512], op=ALU.add),
                      reads=[bk(yb), ("X1", t)], writes=[("X1", t)])
        if bi == NB - 1:
            fw.dma("sp", out_v[:, 4 * tg:4 * tg + 4, :], X1[:, 4 * tg:4 * tg + 4, :], reads=[("X1", t) for t in range(4 * tg, 4 * tg + 4)], writes=[("out", tg)])
        if tg == 3 and bi + 2 < NB:
            load_expert_batch(bi + 2)

    prev_ = None
    for bi in range(NB):
        for tg in range(4):
            if tg == 0:
                moe_fold(bi)
            moe_p1(bi, tg)
            if prev_ is not None and MOE_PIPE:
                moe_p2(*prev_)
            if MOE_PIPE:
                prev_ = (bi, tg)
            else:
                moe_p2(bi, tg)
    if prev_ is not None:
        moe_p2(*prev_)
    fw.barrier()
    print("ops", fw.nops, "sem counts", fw.cnt)
    return nc, fw, None


def host_inputs(inputs, b):
    f = np.float32
    g = lambda k: np.asarray(inputs[k], dtype=f)
    P = [0, 4, 1, 5, 2, 6, 3, 7]
    w_in = g("w_in")[0]
    qcols = np.concatenate([np.arange(h * 64, (h + 1) * 64) for h in P])
    w_in_tm = np.concatenate([w_in[:, qcols], w_in[:, 512:768], w_in[:, 1280:1792], w_in[:, 1808:2320]], axis=1)
    w_in_fm = np.concatenate([w_in[:, 768:1280], w_in[:, 1792:1920]], axis=1)
    col = lambda v: np.ascontiguousarray(v.reshape(-1, 128).T)
    vecs = np.concatenate([
        col(g("b_ada")[0]), col(g("g_norm1")[0]), col(g("g_norm2")[0]), col(g("g_att_out")[0]), col(g("g_gla_out")[0]),
        col(g("b_gk")[0]), col(np.tile(g("q_norm")[0], 2)), col(np.tile(g("k_norm")[0], 2)), col(g("c")[b]),
    ], axis=1)
    assert vecs.shape == (128, NV)
    sinks_b = np.ascontiguousarray(np.broadcast_to(g("sinks")[0][None, :], (128, 8)))
    brg = np.ascontiguousarray(np.broadcast_to(np.concatenate([g("b_group")[0], g("b_router")[0]])[None, :], (128, 20)))
    slopes = np.exp2(-8.0 * np.arange(1, 9) / 8).astype(f)
    kj = np.arange(128)[:, None]
    qi = np.arange(128)[None, :]
    abias = np.zeros((128, 4, 512), f)
    for kv in range(2):
        for gg in range(4):
            h = kv * 4 + gg
            dcur = (qi - kj).astype(f)
            abias[:, kv * 2 + 0, gg * 128:(gg + 1) * 128] = np.where(dcur >= 0, -slopes[h] * dcur, -30000.0)
            dprev = (qi - kj + 128).astype(f)
            abias[:, kv * 2 + 1, gg * 128:(gg + 1) * 128] = np.where(dprev < 128, -slopes[h] * dprev, -30000.0)
    gmask = (kj <= qi).astype(f)
    sel = np.zeros((128, 16, 128), f)
    for e in range(16):
        sel[e, e, :] = 1.0
    w_rt = np.concatenate([g("w_group")[0], g("w_router")[0]], axis=1)
    return {
        "x": np.ascontiguousarray(g("x")[b]), "vecs": vecs, "sinks_b": sinks_b, "brg": brg,
        "ident": np.eye(128, dtype=f), "abias": abias, "gmask": gmask, "sel": sel,
        "w_ada": g("w_ada")[0], "w_in_tm": np.ascontiguousarray(w_in_tm), "w_in_fm": np.ascontiguousarray(w_in_fm),
        "w_gk2": g("w_gk2")[0], "w_out": g("w_out")[0], "w_rt": np.ascontiguousarray(w_rt),
        "w1": g("w1")[0], "w3": g("w3")[0], "w2": g("w2")[0],
    }


def kernel(**inputs):
    nc, fw, _ = build_nc()
    in_maps = [host_inputs(inputs, b) for b in range(8)]
    res = run_bass_kernel_spmd(nc, in_maps, core_ids=list(range(8)))
    return np.stack([r["out"] for r in res.results], axis=0).astype(np.float32)
```
